# Optimizing a Trainium2 kernel written in Bass

```python
import jax, jax.numpy as jnp
from jax import lax
import numpy as np

D_MODEL = 1024
BATCH = 32
SEQ = 256
DEPTH = 2
DEC_BATCH = 8
DEC_SEQ = 1024
PAST_LEN = 512

GRID_W = 64
N_EVEN = (DEPTH + 1) // 2
N_ODD = DEPTH // 2
RMS_EPS = 1e-6
N_MOD = 6

HG_HEADS = 4
HG_DK = 128
HG_DV = 128
HG_KW = HG_HEADS * HG_DK
HG_VW = HG_HEADS * HG_DV
HG_CHUNK = 16

ML_HEADS = 4
ML_DK = 128
ML_DV = 128
ML_KW = ML_HEADS * ML_DK
ML_VW = ML_HEADS * ML_DV
ML_CHUNK = 64
ML_N_GATES = 4 * ML_HEADS
ML_FGATE_BIAS = 3.0

SHORT_CONV = 3

EVEN_SPLIT = (HG_KW, HG_VW, HG_VW, HG_KW, HG_KW, ML_KW, ML_KW, ML_VW, ML_VW, ML_N_GATES)
EVEN_PROJ = 3 * HG_KW + 2 * HG_VW + 2 * ML_KW + 2 * ML_VW + ML_N_GATES
MIX_OUT = HG_VW + ML_VW

HY_ORDER = 2
HY_BANDS = 16
HY_EMB = 1 + 2 * HY_BANDS
HY_HIDDEN = 64
HY_FILT = 2 * HY_ORDER * D_MODEL
HY_MIN_DECAY = 3.07
HY_MAX_DECAY = 15.35

N_EXPERTS = 32
TOP_K = 4
D_FF = D_MODEL
SWIGLU_LIMIT = 7.0
SWIGLU_ALPHA = 1.702

kernel_name = 'hybrid_prefix_diffusion_step'


def rmsnorm(x, g):
    xf = x.astype(jnp.float32)
    y = xf * lax.rsqrt(jnp.mean(xf * xf, axis=-1, keepdims=True) + RMS_EPS)
    return (y * g.astype(jnp.float32)).astype(x.dtype)


def head_rms(x):
    return x * lax.rsqrt(jnp.mean(x * x, axis=-1, keepdims=True) + RMS_EPS)


def split_heads(x, n):
    b, l, w = x.shape
    return x.reshape(b, l, n, w // n).transpose(0, 2, 1, 3).astype(jnp.float32)


def merge_heads(x):
    b, h, l, d = x.shape
    return x.transpose(0, 2, 1, 3).reshape(b, l, h * d)


def rev(x):
    return jnp.flip(x, axis=2)


def short_conv(x, w):
    k = w.shape[0]
    pad = k // 2
    L = x.shape[1]
    xp = jnp.pad(x, ((0, 0), (pad, pad), (0, 0)))
    y = xp[:, 0:L] * w[0]
    for j in range(1, k):
        y = y + xp[:, j:j + L] * w[j]
    return y


def grid_positions(n_tok, d):
    f32 = jnp.float32
    rows = n_tok // GRID_W
    r, col = jnp.meshgrid(jnp.arange(rows, dtype=f32), jnp.arange(GRID_W, dtype=f32), indexing='ij')
    r = r.reshape(-1)
    col = col.reshape(-1)
    quarter = d // 4
    inv = 1.0 / (10000.0 ** (jnp.arange(quarter, dtype=f32) / quarter))
    ar = r[:, None] * inv[None]
    ac = col[:, None] * inv[None]
    return jnp.concatenate([jnp.sin(ar), jnp.cos(ar), jnp.sin(ac), jnp.cos(ac)], axis=-1)


def gla_chunked(q, k, v, log_f, s0):
    f32 = jnp.float32
    bsz, nh, L, dk = q.shape
    dv = v.shape[-1]
    C = HG_CHUNK
    n = L // C
    q = q.reshape(bsz, nh, n, C, dk)
    k = k.reshape(bsz, nh, n, C, dk)
    v = v.reshape(bsz, nh, n, C, dv)
    b = jnp.cumsum(log_f.reshape(bsz, nh, n, C, dk), axis=3)
    causal = jnp.tril(jnp.ones((C, C), dtype=bool))[:, :, None]
    diff = b[:, :, :, :, None, :] - b[:, :, :, None, :, :]
    decay = jnp.exp(jnp.where(causal, diff, -jnp.inf))
    attn = jnp.einsum('bhntd,bhnsd,bhntsd->bhnts', q, k, decay)
    intra = jnp.einsum('bhnts,bhnsv->bhntv', attn, v)
    b_last = b[:, :, :, -1:, :]
    q_in = q * jnp.exp(b)
    k_out = k * jnp.exp(b_last - b)
    chunk_decay = jnp.exp(b_last[:, :, :, 0, :])

    def step(s, xs):
        qc, kc, vc, dc = xs
        o = jnp.einsum('bhtd,bhdv->bhtv', qc, s)
        s = dc[..., None] * s + jnp.einsum('bhsd,bhsv->bhdv', kc, vc)
        return s, o

    xs = tuple(jnp.moveaxis(a, 2, 0) for a in (q_in, k_out, v, chunk_decay))
    s_fin, inter = lax.scan(step, s0.astype(f32), xs)
    out = intra + jnp.moveaxis(inter, 0, 2)
    return out.reshape(bsz, nh, L, dv), s_fin


def mlstm_chunked(q, k, v, ig, log_f, c0, n0, m0):
    f32 = jnp.float32
    bsz, nh, L, dk = q.shape
    dv = v.shape[-1]
    C = ML_CHUNK
    n = L // C
    q = q.reshape(bsz, nh, n, C, dk)
    k = k.reshape(bsz, nh, n, C, dk)
    v = v.reshape(bsz, nh, n, C, dv)
    ig = ig.reshape(bsz, nh, n, C)
    b = jnp.cumsum(log_f.reshape(bsz, nh, n, C), axis=3)
    causal = jnp.tril(jnp.ones((C, C), dtype=bool))
    dmat = jnp.where(causal, b[..., :, None] - b[..., None, :] + ig[..., None, :], -jnp.inf)
    qk = jnp.einsum('bhntd,bhnsd->bhnts', q, k)

    def step(carry, xs):
        c_prev, n_prev, m_prev = carry
        qc, kc, vc, bc, igc, qkc, dc = xs
        m_t = jnp.maximum(bc + m_prev[..., None], jnp.max(dc, axis=-1))
        p = jnp.exp(dc - m_t[..., None]) * qkc
        inter = jnp.exp(bc + m_prev[..., None] - m_t)
        num = inter[..., None] * jnp.einsum('bhtd,bhdv->bhtv', qc, c_prev) + jnp.einsum('bhts,bhsv->bhtv', p, vc)
        den = inter * jnp.einsum('bhtd,bhd->bht', qc, n_prev) + jnp.sum(p, axis=-1)
        h = num / jnp.maximum(jnp.abs(den), jnp.exp(-m_t))[..., None]
        m_new = m_t[..., -1]
        w = jnp.exp(bc[..., -1:] - bc + igc - m_new[..., None])
        dec = jnp.exp(bc[..., -1] + m_prev - m_new)
        c_new = dec[..., None, None] * c_prev + jnp.einsum('bhs,bhsd,bhsv->bhdv', w, kc, vc)
        n_new = dec[..., None] * n_prev + jnp.einsum('bhs,bhsd->bhd', w, kc)
        return (c_new, n_new, m_new), h

    xs = tuple(jnp.moveaxis(a, 2, 0) for a in (q, k, v, b, ig, qk, dmat))
    carry0 = (c0.astype(f32), n0.astype(f32), m0.astype(f32))
    (c_f, n_f, m_f), h = lax.scan(step, carry0, xs)
    return jnp.moveaxis(h, 0, 2).reshape(bsz, nh, L, dv), (c_f, n_f, m_f)


def even_mixer(h, w_in, gate_b, conv_w, lb, w_out, hg_s0, ml_c0, ml_n0, ml_m0):
    f32 = jnp.float32
    bsz, L, _ = h.shape
    p = h @ w_in
    cuts = np.cumsum(EVEN_SPLIT)[:-1].tolist()
    hq, hi, hg, hff, hfb, mq, mk, mv, mo, mg = jnp.split(p, cuts, axis=-1)
    lbh = lb.astype(f32).reshape(HG_HEADS, 1, HG_DK)
    q = split_heads(hq, HG_HEADS)
    iv = split_heads(hi, HG_HEADS)
    f_f = lbh + (1.0 - lbh) * jax.nn.sigmoid(split_heads(hff, HG_HEADS))
    f_b = lbh + (1.0 - lbh) * jax.nn.sigmoid(split_heads(hfb, HG_HEADS))
    o_f, s_f = gla_chunked(q, 1.0 - f_f, iv, jnp.log(f_f), hg_s0[:, 0])
    o_b, s_b = gla_chunked(rev(q), rev(1.0 - f_b), rev(iv), rev(jnp.log(f_b)), hg_s0[:, 1])
    o_hg = merge_heads(head_rms(o_f + rev(o_b))) * jax.nn.silu(hg.astype(f32))
    qk = jax.nn.silu(short_conv(jnp.concatenate([mq, mk], axis=-1), conv_w))
    mq_c, mk_c = jnp.split(qk, 2, axis=-1)
    q2 = split_heads(mq_c, ML_HEADS)
    k2 = split_heads(mk_c, ML_HEADS) * (ML_DK ** -0.5)
    v2 = split_heads(mv, ML_HEADS)
    g = (mg + gate_b).astype(f32).reshape(bsz, L, 4, ML_HEADS).transpose(2, 0, 3, 1)
    ig_f, lf_f = g[0], jax.nn.log_sigmoid(g[1])
    ig_b, lf_b = g[2], jax.nn.log_sigmoid(g[3])
    h_f, (c_f, n_f, m_f) = mlstm_chunked(q2, k2, v2, ig_f, lf_f, ml_c0[:, 0], ml_n0[:, 0], ml_m0[:, 0])
    h_b, (c_b, n_b, m_b) = mlstm_chunked(rev(q2), rev(k2), rev(v2), rev(ig_b), rev(lf_b),
                                         ml_c0[:, 1], ml_n0[:, 1], ml_m0[:, 1])
    o_ml = merge_heads(head_rms(h_f + rev(h_b))) * jax.nn.sigmoid(mo.astype(f32))
    y = jnp.concatenate([o_hg, o_ml], axis=-1).astype(h.dtype) @ w_out
    states = (jnp.stack([s_f, s_b], axis=1), jnp.stack([c_f, c_b], axis=1),
              jnp.stack([n_f, n_b], axis=1), jnp.stack([m_f, m_b], axis=1))
    return y, states


def hyena_filters(L, w1, b1, w2, b2, w3, b3, freq, log_rate):
    f32 = jnp.float32
    t = jnp.arange(L, dtype=f32)
    t_norm = t / (L - 1)
    bands = jnp.linspace(1e-4, HY_BANDS - 1, HY_BANDS, dtype=f32)
    ang = (2.0 * np.pi / L) * t[:, None] * bands[None, :]
    z = jnp.concatenate([t_norm[:, None], jnp.cos(ang), jnp.sin(ang)], axis=-1)
    a = jnp.sin(freq[0].astype(f32) * (z @ w1.astype(f32) + b1.astype(f32)))
    a = jnp.sin(freq[1].astype(f32) * (a @ w2.astype(f32) + b2.astype(f32)))
    filt = (a @ w3.astype(f32) + b3.astype(f32)) * jnp.exp(-t_norm[:, None] * jnp.exp(log_rate.astype(f32)))
    filt = filt.reshape(L, 2, HY_ORDER, D_MODEL)
    return filt * lax.rsqrt(jnp.sum(filt * filt, axis=(0, 1), keepdims=True))


def long_conv_bidir(z, h_fwd, h_bwd):
    L = z.shape[1]
    kern = jnp.concatenate([h_fwd, jnp.zeros_like(h_fwd[:1]), h_bwd[:0:-1]], axis=0)
    zf = jnp.fft.rfft(z.astype(jnp.float32), n=2 * L, axis=1)
    kf = jnp.fft.rfft(kern, axis=0)
    return jnp.fft.irfft(zf * kf[None], n=2 * L, axis=1)[:, :L]


def hyena_mixer(h, w_in, conv_w, w1, b1, w2, b2, w3, b3, freq, log_rate, bias, w_out):
    L = h.shape[1]
    u = short_conv(h @ w_in, conv_w).astype(jnp.float32)
    v, x1, x2 = jnp.split(u, 3, axis=-1)
    filt = hyena_filters(L, w1, b1, w2, b2, w3, b3, freq, log_rate)
    z = v
    for o, gate in enumerate((x1, x2)):
        z = gate * (long_conv_bidir(z, filt[:, 0, o], filt[:, 1, o]) + z * bias[o].astype(jnp.float32))
    return z.astype(h.dtype) @ w_out


def moe(h, w_router, b_router, w_gu, b_gu, w_down, b_down):
    bsz, L, d = h.shape
    x = h.reshape(-1, d)
    logits = (x @ w_router + b_router).astype(jnp.float32)
    vals, idx = lax.top_k(logits, TOP_K)
    wts = jax.nn.softmax(vals, axis=-1)
    gates = jnp.sum(jax.nn.one_hot(idx, N_EXPERTS, dtype=jnp.float32) * wts[..., None], axis=1)
    y = jnp.zeros(x.shape, jnp.float32)
    for e in range(N_EXPERTS):
        gu = x @ w_gu[e] + b_gu[e]
        gl, up = gu[:, 0::2], gu[:, 1::2]
        gl = jnp.minimum(gl, SWIGLU_LIMIT)
        up = jnp.clip(up, -SWIGLU_LIMIT, SWIGLU_LIMIT)
        act = (up + 1.0) * gl * jax.nn.sigmoid(SWIGLU_ALPHA * gl)
        y = y + gates[:, e:e + 1] * (act @ w_down[e] + b_down[e])
    return y.astype(h.dtype).reshape(bsz, L, d)


def trunk(x, cond, st_hg, st_c, st_n, st_m, P):
    lb_all = jnp.cumsum(jax.nn.softmax(P['hg_lb'].astype(jnp.float32), axis=0), axis=0)
    new_states = []
    for l in range(DEPTH):
        mod = jax.nn.silu(cond) @ P['w_mod'][l] + P['b_mod'][l]
        sh1, sc1, g1, sh2, sc2, g2 = jnp.split(mod[:, None, :], N_MOD, axis=-1)
        hdn = (rmsnorm(x, P['norm_g'][l, 0]) * (1.0 + sc1) + sh1).astype(x.dtype)
        if l % 2 == 0:
            e = l // 2
            y, st = even_mixer(hdn, P['ev_w_in'][e], P['ev_gate_b'][e], P['ev_conv'][e], lb_all[l],
                               P['ev_w_out'][e], st_hg[:, e], st_c[:, e], st_n[:, e], st_m[:, e])
            new_states.append(st)
        else:
            o = l // 2
            y = hyena_mixer(hdn, P['hy_w_in'][o], P['hy_conv'][o], P['hy_w1'][o], P['hy_b1'][o],
                            P['hy_w2'][o], P['hy_b2'][o], P['hy_w3'][o], P['hy_b3'][o],
                            P['hy_freq'][o], P['hy_log_rate'][o], P['hy_bias'][o], P['hy_w_out'][o])
        x = x + (g1 * y).astype(x.dtype)
        hdn = (rmsnorm(x, P['norm_g'][l, 1]) * (1.0 + sc2) + sh2).astype(x.dtype)
        y = moe(hdn, P['w_router'][l], P['b_router'][l], P['w_gu'][l], P['b_gu'][l], P['w_down'][l], P['b_down'][l])
        x = x + (g2 * y).astype(x.dtype)
    x = rmsnorm(x, P['final_g'])
    stacked = tuple(jnp.stack([s[i] for s in new_states], axis=1) for i in range(4))
    return x, stacked


def setup_inputs(seed: int = 0) -> dict:
    key = jax.random.key(seed)
    ks = iter(jax.random.split(key, 48))
    f32 = jnp.float32
    D = D_MODEL

    def nrm(shape, scale):
        return scale * jax.random.normal(next(ks), shape, f32)

    gate_base = jnp.repeat(jnp.array([0.0, ML_FGATE_BIAS, 0.0, ML_FGATE_BIAS], f32), ML_HEADS)
    rate_base = jnp.log(jnp.linspace(HY_MIN_DECAY, HY_MAX_DECAY, D, dtype=f32))
    return {
        'x_prompt': nrm((BATCH, SEQ, D), 1.0),
        'x_sample': nrm((DEC_BATCH, DEC_SEQ, D), 1.0),
        'state_hgrn': nrm((DEC_BATCH, N_EVEN, 2, HG_HEADS, HG_DK, HG_DV), 0.5),
        'state_mlstm_c': nrm((DEC_BATCH, N_EVEN, 2, ML_HEADS, ML_DK, ML_DV), 0.5),
        'state_mlstm_n': nrm((DEC_BATCH, N_EVEN, 2, ML_HEADS, ML_DK), 0.5),
        'state_mlstm_m': nrm((DEC_BATCH, N_EVEN, 2, ML_HEADS), 1.0),
        'c': nrm((DEC_BATCH, D), 1.0),
        'c_ctx': nrm((D,), 1.0),
        'norm_g': 1.0 + nrm((DEPTH, 2, D), 0.02),
        'final_g': 1.0 + nrm((D,), 0.02),
        'w_mod': nrm((DEPTH, D, N_MOD * D), 0.5 * D ** -0.5),
        'b_mod': nrm((DEPTH, N_MOD * D), 0.02),
        'ev_w_in': nrm((N_EVEN, D, EVEN_PROJ), D ** -0.5),
        'ev_gate_b': jnp.tile(gate_base, (N_EVEN, 1)) + nrm((N_EVEN, ML_N_GATES), 0.1),
        'ev_conv': nrm((N_EVEN, SHORT_CONV, 2 * ML_KW), 0.5),
        'hg_lb': nrm((DEPTH + 1, HG_KW), 0.1),
        'ev_w_out': nrm((N_EVEN, MIX_OUT, D), MIX_OUT ** -0.5),
        'hy_w_in': nrm((N_ODD, D, 3 * D), D ** -0.5),
        'hy_conv': nrm((N_ODD, SHORT_CONV, 3 * D), 0.5),
        'hy_w1': nrm((N_ODD, HY_EMB, HY_HIDDEN), HY_EMB ** -0.5),
        'hy_b1': nrm((N_ODD, HY_HIDDEN), 0.02),
        'hy_w2': nrm((N_ODD, HY_HIDDEN, HY_HIDDEN), HY_HIDDEN ** -0.5),
        'hy_b2': nrm((N_ODD, HY_HIDDEN), 0.02),
        'hy_w3': nrm((N_ODD, HY_HIDDEN, HY_FILT), HY_HIDDEN ** -0.5),
        'hy_b3': nrm((N_ODD, HY_FILT), 0.02),
        'hy_freq': 1.0 + nrm((N_ODD, 2, HY_HIDDEN), 0.1),
        'hy_log_rate': jnp.tile(rate_base, (N_ODD, 2 * HY_ORDER)) + nrm((N_ODD, HY_FILT), 0.05),
        'hy_bias': nrm((N_ODD, HY_ORDER, D), 0.5),
        'hy_w_out': nrm((N_ODD, D, D), D ** -0.5),
        'w_router': nrm((DEPTH, D, N_EXPERTS), D ** -0.5),
        'b_router': nrm((DEPTH, N_EXPERTS), 0.01),
        'w_gu': nrm((DEPTH, N_EXPERTS, D, 2 * D_FF), D ** -0.5),
        'b_gu': nrm((DEPTH, N_EXPERTS, 2 * D_FF), 0.02),
        'w_down': nrm((DEPTH, N_EXPERTS, D_FF, D), D_FF ** -0.5),
        'b_down': nrm((DEPTH, N_EXPERTS, D), 0.02),
    }


def reference(x_prompt, x_sample, state_hgrn, state_mlstm_c, state_mlstm_n, state_mlstm_m, c, c_ctx,
              norm_g, final_g, w_mod, b_mod, ev_w_in, ev_gate_b, ev_conv, hg_lb, ev_w_out,
              hy_w_in, hy_conv, hy_w1, hy_b1, hy_w2, hy_b2, hy_w3, hy_b3, hy_freq, hy_log_rate, hy_bias, hy_w_out,
              w_router, b_router, w_gu, b_gu, w_down, b_down):
    f32 = jnp.float32
    P = {
        'norm_g': norm_g, 'final_g': final_g, 'w_mod': w_mod, 'b_mod': b_mod,
        'ev_w_in': ev_w_in, 'ev_gate_b': ev_gate_b, 'ev_conv': ev_conv, 'hg_lb': hg_lb, 'ev_w_out': ev_w_out,
        'hy_w_in': hy_w_in, 'hy_conv': hy_conv, 'hy_w1': hy_w1, 'hy_b1': hy_b1, 'hy_w2': hy_w2, 'hy_b2': hy_b2,
        'hy_w3': hy_w3, 'hy_b3': hy_b3, 'hy_freq': hy_freq, 'hy_log_rate': hy_log_rate, 'hy_bias': hy_bias,
        'hy_w_out': hy_w_out, 'w_router': w_router, 'b_router': b_router, 'w_gu': w_gu, 'b_gu': b_gu,
        'w_down': w_down, 'b_down': b_down,
    }
    bp = x_prompt.shape[0]
    z_hg = jnp.zeros((bp, N_EVEN, 2, HG_HEADS, HG_DK, HG_DV), f32)
    z_c = jnp.zeros((bp, N_EVEN, 2, ML_HEADS, ML_DK, ML_DV), f32)
    z_n = jnp.zeros((bp, N_EVEN, 2, ML_HEADS, ML_DK), f32)
    z_m = jnp.zeros((bp, N_EVEN, 2, ML_HEADS), f32)
    y_prompt, (new_hgrn, new_mlstm_c, new_mlstm_n, new_mlstm_m) = trunk(
        x_prompt, c_ctx[None, :], z_hg, z_c, z_n, z_m, P)
    pos = grid_positions(x_sample.shape[1], D_MODEL).astype(x_sample.dtype)
    y_sample, _ = trunk(x_sample + pos[None], c, state_hgrn, state_mlstm_c, state_mlstm_n, state_mlstm_m, P)
    return (y_prompt, y_sample, new_hgrn, new_mlstm_c, new_mlstm_n, new_mlstm_m)
```

```python
import numpy as np
from contextlib import ExitStack
import concourse.bass as bass
import concourse.mybir as mybir
from concourse.bass_utils import run_bass_kernel_spmd

F32 = mybir.dt.float32
BF16 = mybir.dt.bfloat16
I32 = mybir.dt.int32
U8 = mybir.dt.uint8
ALU = mybir.AluOpType
AF = mybir.ActivationFunctionType
AX = mybir.AxisListType

NCORES = 8
D = 1024
NT = 2048
NE = 32
EPS = 1e-6


class Buf:
    __slots__ = ("name", "w", "r", "sem")

    def __init__(self, name):
        self.name = name
        self.w = None
        self.r = {}
        self.sem = None


class Sched:
    ENG = ("pe", "act", "dve", "pool", "sp")

    def __init__(self, nc, stack):
        self.nc = nc
        self.stack = stack
        self.eng = {"pe": nc.tensor, "act": nc.scalar, "dve": nc.vector, "pool": nc.gpsimd, "sp": nc.sync}
        self.sems = {}
        self.cnt = {}
        self.seen = {}
        for e in self.ENG:
            self.sems[e] = stack.enter_context(nc.semaphore("s_" + e))
            self.cnt[e] = 0
            self.seen[e] = {}
        self.dma_cnt = {}
        self.dma_free = {}
        self.nsem = 0
        self.n_inst = 0
        self.n_wait = 0
        self.phase_bufs = []

    def buf(self, name):
        b = Buf(name)
        self.phase_bufs.append(b)
        return b

    def _dma_sem(self, buf, q):
        kind = "sw" if q == "pool" else "hw"
        if buf.sem is None:
            buf.sem = {}
        if kind not in buf.sem:
            free = self.dma_free.setdefault(kind, [])
            if free:
                buf.sem[kind] = free.pop()
            else:
                self.nsem += 1
                key = "d%d" % self.nsem
                self.sems[key] = self.stack.enter_context(self.nc.semaphore("sd%s_%d" % (kind, self.nsem)))
                self.dma_cnt[key] = 0
                buf.sem[kind] = key
        return buf.sem[kind]

    def _waits(self, e, r, w):
        deps = {}

        def add(k, v):
            if deps.get(k, 0) < v:
                deps[k] = v
        for b in r:
            if b.w is not None:
                add(*b.w)
        for b in w:
            if b.w is not None:
                add(*b.w)
            for k, v in b.r.items():
                add(k, v)
        seen = self.seen[e]
        for k, v in deps.items():
            if k == e and e == "pe":
                continue
            if seen.get(k, 0) >= v:
                continue
            if k == e and v > self.cnt[e]:
                raise RuntimeError("self-wait on future event %s %d" % (e, v))
            self.eng[e].wait_ge(self.sems[k], v)
            self.n_wait += 1
            seen[k] = v

    def op(self, e, fn, r=(), w=(), inc=True):
        self._waits(e, r, w)
        inst = fn(self.eng[e])
        self.n_inst += 1
        if inc:
            inst.then_inc(self.sems[e], 1)
            self.cnt[e] += 1
            ev = (e, self.cnt[e])
        else:
            ev = (e, self.cnt[e] + 1)
        for b in r:
            if b.r.get(ev[0], 0) < ev[1]:
                b.r[ev[0]] = ev[1]
        for b in w:
            b.w = ev
            b.r = {}
        return inst

    def dma(self, q, out, in_, r=(), w=(), **kw):
        assert len(w) == 1
        self._waits(q, r, w)
        b = w[0]
        key = self._dma_sem(b, q)
        inst = self.eng[q].dma_start(out=out, in_=in_, **kw)
        inst.then_inc(self.sems[key], 16)
        self.n_inst += 1
        self.dma_cnt[key] += 16
        ev = (key, self.dma_cnt[key])
        for x in r:
            if x.r.get(key, 0) < ev[1]:
                x.r[key] = ev[1]
        b.w = ev
        b.r = {}
        return inst

    def barrier(self, extra=()):
        evs = {}
        for e in self.ENG:
            if self.cnt[e] > 0:
                evs[e] = self.cnt[e]
        for k, v in self.dma_cnt.items():
            if v > 0:
                evs[k] = v
        for e in self.ENG:
            seen = self.seen[e]
            for k, v in evs.items():
                if k == e:
                    continue
                if seen.get(k, 0) >= v:
                    continue
                self.eng[e].wait_ge(self.sems[k], v)
                self.n_wait += 1
                seen[k] = v

    def end_phase(self, keep=()):
        self.barrier()
        keepset = set(id(b) for b in keep)
        rest = []
        for b in self.phase_bufs:
            if id(b) in keepset:
                rest.append(b)
                continue
            if b.sem is not None:
                for kind, key in b.sem.items():
                    self.dma_free.setdefault(kind, []).append(key)
                b.sem = None
            b.w = None
            b.r = {}
        self.phase_bufs = rest


class GroupInfo:
    def __init__(self, gi, nseq, L, tok0, col0):
        self.gi = gi
        self.nseq = nseq
        self.L = L
        self.tok0 = tok0
        self.col0 = col0
        self.T = nseq * L
        self.cond = gi

    def col(self, j, i):
        return self.col0 + j * (self.L + 2) + 1 + i

    def lcol(self, lt):
        return self.col(lt // self.L, lt % self.L)

    def ttiles(self):
        out = []
        n = min(self.L, 512)
        for j in range(self.nseq):
            for i0 in range(0, self.L, n):
                out.append((j * self.L + i0, n, self.col(j, i0)))
        return out


GROUPS = [GroupInfo(0, 4, 256, 0, 0), GroupInfo(1, 1, 1024, 1024, 4 * 258)]
HTW = 4 * 258 + 1026


IN_SHAPES = {
    "x_in": ([16, 128, 1024], F32),
    "pos": ([8, 128, 1024], F32),
    "cond_l": ([128, 8, 2], F32),
    "w_mod": ([2, 1024, 6144], F32),
    "b_mod": ([2, 6144], F32),
    "norm_g": ([2, 2, 1024], F32),
    "final_g": ([1, 1024], F32),
    "w_router": ([2, 1024, 32], F32),
    "b_router": ([2, 32], F32),
    "gu_l": ([2, 32, 8, 128, 2 * 8 * 128], F32),
    "wd_l": ([2, 32, 2, 128, 8 * 512], F32),
    "bgu_l": ([2, 128, 32 * 16], F32),
    "b_down": ([2, 32, 1024], F32),
    "ev_w_in": ([1024, 4624], F32),
    "ev_gate_b": ([16], F32),
    "ev_conv": ([3, 1024], F32),
    "hg_lb": ([3, 512], F32),
    "ev_w_out": ([1024, 1024], F32),
    "st_hg": ([2, 4, 128, 128], F32),
    "st_c": ([2, 4, 128, 128], F32),
    "st_n": ([2, 4, 128], F32),
    "st_m": ([2, 4], F32),
    "hy_w_in": ([1024, 3072], F32),
    "hy_conv": ([3, 3072], F32),
    "hy_w1": ([33, 64], F32),
    "hy_b1": ([64], F32),
    "hy_w2": ([64, 64], F32),
    "hy_b2": ([64], F32),
    "hy_w3": ([64, 4096], F32),
    "hy_b3": ([1, 4096], F32),
    "hy_freq": ([2, 64], F32),
    "hy_log_rate": ([1, 4096], F32),
    "hy_bias": ([2, 1024], F32),
    "hy_w_out": ([1024, 1024], F32),
    "zfeat0": ([33, 256], F32),
    "zfeat1": ([33, 1024], F32),
    "tn0": ([128, 2], F32),
    "tn1": ([128, 8], F32),
    "dftc0": ([2, 128, 2, 128], BF16),
    "dfts0": ([2, 128, 2, 128], BF16),
    "dftc1": ([8, 128, 8, 128], BF16),
    "dfts1": ([8, 128, 8, 128], BF16),
    "idftc0": ([2, 128, 2, 128], BF16),
    "idfts0": ([2, 128, 2, 128], BF16),
    "idftc1": ([8, 128, 8, 128], BF16),
    "idfts1": ([8, 128, 8, 128], BF16),
}


class LazyDram(dict):
    def __init__(self, nc):
        super().__init__()
        self.nc = nc

    def __missing__(self, name):
        sh, dt = IN_SHAPES[name]
        t = self.nc.dram_tensor(name, list(sh), dt, kind="ExternalInput").ap()
        self[name] = t
        return t


class Builder:
    def __init__(self, plan):
        self.plan = plan
        self.nc = bass.Bass("TRN2", target_bir_lowering=False)
        self.stack = ExitStack()
        self.S = Sched(self.nc, self.stack)
        self.dram = LazyDram(self.nc)
        self.outs = []

    def din(self, name, shape, dt=F32):
        t = self.nc.dram_tensor(name, list(shape), dt, kind="ExternalInput").ap()
        self.dram[name] = t
        return t

    def dout(self, name, shape, dt=F32):
        t = self.nc.dram_tensor(name, list(shape), dt, kind="ExternalOutput").ap()
        self.dram[name] = t
        return t

    def sb(self, st, name, shape, dt):
        self._uid = getattr(self, "_uid", 0) + 1
        name = "%s_u%d" % (name, self._uid)
        t = st.enter_context(self.nc.sbuf_tensor(name, list(shape), dt))
        return t, self.S.buf(name)

    def out_dma(self, dst, src_ap, src_bufs, q="sp"):
        if len(self.outs) < 12:
            self.outs.append(Buf("out%d" % len(self.outs)))
        self._oc = getattr(self, "_oc", 0) + 1
        b = self.outs[self._oc % len(self.outs)]
        self.S.dma(q, dst, src_ap, r=src_bufs, w=[b])

    def mm(self, ps_ap, ps_buf, pairs, rbufs, start=True, stop=True):
        n = len(pairs)
        for i, (l, r) in enumerate(pairs):
            self.S.op("pe", lambda e, l=l, r=r, i=i: e.matmul(ps_ap, lhsT=l, rhs=r, start=(start and i == 0), stop=(stop and i == n - 1)),
                      r=rbufs, w=[ps_buf], inc=(i == n - 1))

    def build(self):
        nc, S = self.nc, self.S
        st = self.stack
        P = self.plan
        x_in = self.dram["x_in"]
        pos = self.dram["pos"]
        cond_l = self.dram["cond_l"]
        y_out = self.dout("y_out", [16, 128, 1024])
        self.dout("o_hg", [4, 2, 4, 128, 128])
        self.dout("o_c", [4, 2, 4, 128, 128])
        self.dout("o_n", [4, 2, 4, 128])
        self.dout("o_m", [4, 2, 4])
        if "dbg_h" in P:
            self.dout("dbg_h", [128, 8, HTW])
        if "dbg_x" in P:
            self.dout("dbg_x", [16, 128, 1024])

        self.x_tok, self.b_x = self.sb(st, "x_tok", [128, 16, 1024], F32)
        self.bx = [Buf("x%d" % i) for i in range(16)]
        self.hT, _ = self.sb(st, "hT", [128, 8, HTW], BF16)
        self.bh = [Buf("h0"), Buf("h1")]
        self.ident, self.b_ident = self.sb(st, "ident", [128, 128], F32)
        self.identb, self.b_identb = self.sb(st, "identb", [128, 128], BF16)
        self.onesb, self.b_onesb = self.sb(st, "onesb", [128, 128], BF16)
        self.eps_t, self.b_eps = self.sb(st, "eps", [128, 1], F32)
        self.gates, self.b_gates = self.sb(st, "gates", [128, 16, NE], F32)
        self.ps = []
        self.bps = []
        for i in range(8):
            t = st.enter_context(nc.psum_tensor("ps%d" % i, [128, 512], F32))
            self.ps.append(t)
            self.bps.append(Buf("ps%d" % i))

        with ExitStack() as ph:
            io, b_io = self.sb(ph, "io", [128, 128], I32)
            S.op("pool", lambda e: e.iota(io[:], pattern=[[1, 128]], base=0, channel_multiplier=-1), w=[b_io])
            S.op("dve", lambda e: e.tensor_scalar(out=self.ident[:], in0=io[:], scalar1=0, scalar2=None, op0=ALU.is_equal), r=[b_io], w=[self.b_ident])
            S.op("dve", lambda e: e.tensor_copy(out=self.identb[:], in_=self.ident[:]), r=[self.b_ident], w=[self.b_identb])
            S.op("pool", lambda e: e.memset(self.onesb[:], 1.0), w=[self.b_onesb])
            S.op("pool", lambda e: e.memset(self.eps_t[:], EPS), w=[self.b_eps])
            S.op("pool", lambda e: e.memset(self.hT[:], 0.0), w=self.bh)
            S.end_phase()
        if "load" in P:
            for tb in range(16):
                S.dma("sp", self.x_tok[:, tb, :], x_in[tb], w=[self.bx[tb]])
        self.ph_modrows()

        for step in P:
            if step == "load":
                self.ph_load(x_in, pos)
            elif step.startswith("norm"):
                _, l, n = step.split(":")
                self.ph_norm(int(l), int(n))
            elif step.startswith("moe"):
                sp = step.split(":")
                self.ph_moe(int(sp[1]), nex=(int(sp[2]) if len(sp) > 2 else NE))
            elif step == "even":
                self.ph_even()
            elif step == "hyena":
                self.ph_hyena()
            elif step == "final":
                self.ph_final(y_out)
            elif step == "dbg_h":
                with ExitStack() as ph:
                    for kc in range(8):
                        t32, b32 = self.sb(ph, "dbgh%d" % kc, [128, HTW], F32)
                        S.op("dve", lambda e: e.tensor_copy(out=t32[:], in_=self.hT[:, kc, :]), r=self.bh, w=[b32])
                        self.out_dma(self.dram["dbg_h"][:, kc, :], t32[:], [b32])
                    S.end_phase()
            elif step == "dbg_x":
                for tb in range(16):
                    self.out_dma(self.dram["dbg_x"][tb], self.x_tok[:, tb, :], [self.bx[tb]])
        S._waits("sp", self.outs, ())
        S.barrier()
        self.stack.close()
        return nc

    def ph_load(self, x_in, pos):
        S = self.S
        with ExitStack() as ph:
            pts = [self.sb(ph, "pos%d" % i, [128, 1024], F32) for i in range(2)]
            for i in range(8):
                pt, bp = pts[i % 2]
                S.dma("sp", pt[:], pos[i], w=[bp])
                tb = 8 + i
                S.op("dve", lambda e, pt=pt, tb=tb: e.tensor_tensor(out=self.x_tok[:, tb, :], in0=self.x_tok[:, tb, :], in1=pt[:], op=ALU.add),
                     r=[bp], w=[self.bx[tb]])
            S.end_phase(keep=())

    def ph_modrows(self):
        S = self.S
        self.modrow = self.nc.dram_tensor("modrow", [2, 2, 6144], F32).ap()
        self.b_modrow = [Buf("modrow0"), Buf("modrow1")]
        w_mod = self.dram["w_mod"]
        b_mod = self.dram["b_mod"]
        with ExitStack() as ph:
            c32, b_c32 = self.sb(ph, "c32", [128, 8, 2], F32)
            S.dma("sp", c32[:], self.dram["cond_l"], w=[b_c32])
            S.op("act", lambda e: e.activation(out=c32[:], in_=c32[:], func=AF.Silu), r=[b_c32], w=[b_c32])
            cs2, b_cs2 = self.sb(ph, "cs2", [128, 8, 2], BF16)
            S.op("dve", lambda e: e.tensor_copy(out=cs2[:], in_=c32[:]), r=[b_c32], w=[b_cs2])
            wts = [self.sb(ph, "modw%d" % i, [128, 8, 512], BF16) for i in range(3)]
            bts = [self.sb(ph, "modb%d" % i, [2, 512], F32) for i in range(3)]
            rows = [self.sb(ph, "modr%d" % i, [2, 512], F32) for i in range(3)]
            cnt = 0
            for l in range(2):
                for blk in range(12):
                    c0 = blk * 512
                    wt, bw = wts[cnt % 3]
                    bt, bb = bts[cnt % 3]
                    row, brow = rows[cnt % 3]
                    pb = cnt % 4
                    cnt += 1
                    S.dma("pool", wt[:], w_mod[l].rearrange("(k p) n -> p k n", p=128)[:, :, c0:c0 + 512], w=[bw])
                    S.dma("sp", bt[:], b_mod[l:l + 1, c0:c0 + 512].partition_broadcast(2), w=[bb])
                    self.mm(self.ps[pb][0:2, :], self.bps[pb], [(cs2[:, kc, :], wt[:, kc, :]) for kc in range(8)], [b_cs2, bw])
                    S.op("dve", lambda e, row=row, pb=pb, bt=bt: e.tensor_tensor(out=row[:], in0=self.ps[pb][0:2, :], in1=bt[:], op=ALU.add),
                         r=[self.bps[pb], bb], w=[brow])
                    S.dma("sp", self.modrow[l, :, c0:c0 + 512], row[:], r=[brow], w=[self.b_modrow[l]])
            S.end_phase(keep=())

    def mod_pieces(self, ph, l, pieces, names):
        S = self.S
        res = {}
        for pi, k in enumerate(pieces):
            for cond in range(2):
                t, b = self.sb(ph, "%s_%d" % (names[pi], cond), [128, 1024], F32)
                S.dma("sp", t[:], self.modrow[l, cond:cond + 1, k * 1024:(k + 1) * 1024].partition_broadcast(128), r=[self.b_modrow[l]], w=[b])
                res[(k, cond)] = (t, b)
        return res

    def ph_norm(self, l, n):
        S = self.S
        with ExitStack() as ph:
            mp = self.mod_pieces(ph, l, [3 * n, 3 * n + 1], ["sh", "sc"])
            g_bc, b_g = self.sb(ph, "g_bc", [128, 1024], F32)
            S.dma("sp", g_bc[:], self.dram["norm_g"][l, n:n + 1, :].partition_broadcast(128), w=[b_g])
            for cond in range(2):
                sc, bsc = mp[(3 * n + 1, cond)]
                S.op("dve", lambda e, sc=sc: e.scalar_tensor_tensor(out=sc[:], in0=sc[:], scalar=1.0, in1=g_bc[:], op0=ALU.add, op1=ALU.mult),
                     r=[bsc, b_g], w=[bsc])
            if n == 0:
                for g in GROUPS:
                    for j in range(g.nseq):
                        for cpad in (g.col(j, 0) - 1, g.col(j, g.L - 1) + 1):
                            S.op("pool", lambda e, cpad=cpad: e.memset(self.hT[:, :, cpad:cpad + 1], 0.0), w=[self.bh[g.gi]])
                self.norm_blocks(ph, lambda cond: mp[(3 * n + 1, cond)], lambda cond: mp[(3 * n, cond)], router=None)
            else:
                mp2 = self.mod_pieces(ph, l, [5], ["g2n"])
                self.norm_blocks(ph, lambda cond: mp[(3 * n + 1, cond)], lambda cond: mp[(3 * n, cond)], router=l, g2=[mp2[(5, c)] for c in range(2)])
            S.end_phase(keep=())

    def norm_blocks(self, ph, a_of, sh_of, router=None, final_out=None, g2=None):
        S = self.S
        junk, b_junk = self.sb(ph, "junk", [128, 1024], F32)
        tmps = [self.sb(ph, "ntmp%d" % i, [128, 1024], F32) for i in range(2)]
        xns = [self.sb(ph, "xn%d" % i, [128, 1024], F32) for i in range(2)]
        ss, b_ss = self.sb(ph, "ss", [128, 16], F32)
        if router is not None:
            wr, b_wr = self.sb(ph, "wr", [128, 8, 32], F32)
            S.dma("sp", wr[:], self.dram["w_router"][router].rearrange("(k p) n -> p k n", p=128), w=[b_wr])
            br, b_br = self.sb(ph, "br", [128, 32], F32)
            S.dma("sp", br[:], self.dram["b_router"][router:router + 1, :].partition_broadcast(128), w=[b_br])
            xT32s = [self.sb(ph, "xT32_%d" % i, [128, 8, 128], F32) for i in range(2)]
            lg, b_lg = self.sb(ph, "lg", [128, 32], F32)
            t8, b_t8 = self.sb(ph, "t8", [128, 8], F32)
            msk, b_msk = self.sb(ph, "msk", [128, 32], F32)
            ex, b_ex = self.sb(ph, "ex", [128, 32], F32)
            sm, b_sm = self.sb(ph, "sm", [128, 2], F32)
            bd32, b_bd32 = self.sb(ph, "bd32", [NE, 1024], F32)
            S.dma("sp", bd32[:], self.dram["b_down"][router], w=[b_bd32])
            gT, b_gT = self.sb(ph, "gT", [NE, 128], F32)
            btmp_t = [self.sb(ph, "bdt%d" % i, [128, 512], F32) for i in range(2)]
        for tb in range(16):
            gi = 0 if tb < 8 else 1
            g = GROUPS[gi]
            a, ba = a_of(gi)
            x = self.x_tok[:, tb, :]
            tmp, btmp = tmps[tb % 2]
            xn, bxn = xns[tb % 2]
            S.op("act", lambda e, x=x, tb=tb: e.activation(out=junk[:], in_=x, func=AF.Square, accum_out=ss[:, tb:tb + 1]),
                 r=[self.bx[tb]], w=[b_junk, b_ss])
            S.op("act", lambda e, tb=tb: e.activation(out=ss[:, tb:tb + 1], in_=ss[:, tb:tb + 1], func=AF.Sqrt, bias=self.eps_t[:], scale=1.0 / 1024.0),
                 r=[b_ss, self.b_eps], w=[b_ss])
            S.op("dve", lambda e, tb=tb: e.reciprocal(out=ss[:, tb:tb + 1], in_=ss[:, tb:tb + 1]), r=[b_ss], w=[b_ss])
            S.op("dve", lambda e, x=x, tb=tb, tmp=tmp, a=a: e.scalar_tensor_tensor(out=tmp[:], in0=x, scalar=ss[:, tb:tb + 1], in1=a[:], op0=ALU.mult, op1=ALU.mult),
                 r=[self.bx[tb], b_ss, ba], w=[btmp])
            if final_out is not None:
                self.out_dma(final_out[tb], tmp[:], [btmp])
                continue
            sh, bsh = sh_of(gi)
            S.op("dve", lambda e, xn=xn, tmp=tmp, sh=sh: e.tensor_tensor(out=xn[:], in0=tmp[:], in1=sh[:], op=ALU.add),
                 r=[btmp, bsh], w=[bxn])
            lt0 = tb * 128 - g.tok0
            col = g.lcol(lt0) if router is None else tb * 128
            for hb in range(2):
                pb = 2 * (tb % 2) + hb
                for q in range(4):
                    kc = hb * 4 + q
                    S.op("pe", lambda e, pb=pb, q=q, kc=kc, xn=xn: e.transpose(self.ps[pb][:, q * 128:(q + 1) * 128], xn[:, kc * 128:(kc + 1) * 128], self.ident[:]),
                         r=[bxn, self.b_ident], w=[self.bps[pb]], inc=(q == 3))
                src = self.ps[pb][:].rearrange("p (q c) -> p q c", c=128)
                if router is None:
                    S.op("act", lambda e, hb=hb, col=col, src=src: e.copy(out=self.hT[:, hb * 4:hb * 4 + 4, col:col + 128], in_=src),
                         r=[self.bps[pb]], w=[self.bh[gi]])
                else:
                    xT32, bxT = xT32s[tb % 2]
                    S.op("act", lambda e, hb=hb, src=src, xT32=xT32: e.copy(out=xT32[:, hb * 4:hb * 4 + 4, :], in_=src),
                         r=[self.bps[pb]], w=[bxT])
                    S.op("dve", lambda e, hb=hb, col=col, xT32=xT32: e.tensor_copy(out=self.hT[:, hb * 4:hb * 4 + 4, col:col + 128], in_=xT32[:, hb * 4:hb * 4 + 4, :]),
                         r=[bxT], w=[self.bh[gi]])
            if router is not None:
                xT32, bxT = xT32s[tb % 2]
                pb = 4 + (tb % 2)
                self.mm(self.ps[pb][:, 0:32], self.bps[pb], [(xT32[:, kc, :], wr[:, kc, :]) for kc in range(8)], [bxT, b_wr])
                S.op("dve", lambda e, pb=pb: e.tensor_tensor(out=lg[:], in0=self.ps[pb][:, 0:32], in1=br[:], op=ALU.add), r=[self.bps[pb], b_br], w=[b_lg])
                S.op("dve", lambda e: e.max(out=t8[:], in_=lg[:]), r=[b_lg], w=[b_t8])
                S.op("dve", lambda e: e.tensor_scalar(out=msk[:], in0=lg[:], scalar1=t8[:, 3:4], scalar2=None, op0=ALU.is_ge), r=[b_lg, b_t8], w=[b_msk])
                S.op("dve", lambda e: e.tensor_scalar(out=sm[:, 0:1], in0=t8[:, 0:1], scalar1=-1.0, scalar2=None, op0=ALU.mult), r=[b_t8], w=[b_sm])
                S.op("act", lambda e: e.activation(out=ex[:], in_=lg[:], func=AF.Exp, bias=sm[:, 0:1], scale=1.0), r=[b_lg, b_sm], w=[b_ex])
                S.op("dve", lambda e: e.tensor_tensor(out=ex[:], in0=ex[:], in1=msk[:], op=ALU.mult), r=[b_ex, b_msk], w=[b_ex])
                S.op("dve", lambda e: e.reduce_sum(out=sm[:, 1:2], in_=ex[:], axis=AX.X), r=[b_ex], w=[b_sm])
                S.op("dve", lambda e: e.reciprocal(out=sm[:, 1:2], in_=sm[:, 1:2]), r=[b_sm], w=[b_sm])
                S.op("dve", lambda e, tb=tb: e.tensor_scalar(out=self.gates[:, tb, :], in0=ex[:], scalar1=sm[:, 1:2], scalar2=None, op0=ALU.mult),
                     r=[b_ex, b_sm], w=[self.b_gates])
                S.op("pe", lambda e, tb=tb: e.transpose(self.ps[6][0:NE, 0:128], self.gates[:, tb, :], self.ident[:]), r=[self.b_gates, self.b_ident], w=[self.bps[6]])
                S.op("act", lambda e: e.copy(out=gT[:], in_=self.ps[6][0:NE, 0:128]), r=[self.bps[6]], w=[b_gT])
                for half in range(2):
                    pbb = 6 + half
                    self.mm(self.ps[pbb][:], self.bps[pbb], [(gT[:], bd32[:, half * 512:(half + 1) * 512])], [b_gT, b_bd32])
                    bt_, bbt_ = btmp_t[half]
                    g2t, bg2 = g2[gi]
                    S.op("dve", lambda e, bt_=bt_, pbb=pbb, g2t=g2t, half=half: e.tensor_tensor(out=bt_[:], in0=self.ps[pbb][:], in1=g2t[:, half * 512:(half + 1) * 512], op=ALU.mult),
                         r=[self.bps[pbb], bg2], w=[bbt_])
                    xs = self.x_tok[:, tb, half * 512:(half + 1) * 512]
                    S.op("pool", lambda e, xs=xs, bt_=bt_: e.tensor_tensor(out=xs, in0=xs, in1=bt_[:], op=ALU.add), r=[bbt_], w=[self.bx[tb]])

    def ph_final(self, y_out):
        S = self.S
        with ExitStack() as ph:
            g_bc, b_g = self.sb(ph, "fg_bc", [128, 1024], F32)
            S.dma("sp", g_bc[:], self.dram["final_g"].partition_broadcast(128), w=[b_g])
            self.norm_blocks(ph, lambda cond: (g_bc, b_g), None, final_out=y_out)
            S.end_phase(keep=())

    def ph_moe(self, l, nex=NE):
        S = self.S
        nc = self.nc
        with ExitStack() as ph:
            mp = self.mod_pieces(ph, l, [5], ["g2"])
            g2 = [mp[(5, c)] for c in range(2)]
            actT, _ = self.sb(ph, "actT", [128, 8, NT], BF16)
            b_act = [[S.buf("act%d_%d" % (c, tt)) for tt in range(4)] for c in range(8)]
            NGU, NWD = 4, 2
            gus = [self.sb(ph, "gu%d" % i, [128, 2, 8, 128], BF16) for i in range(NGU)]
            wds = [self.sb(ph, "wd%d" % i, [128, 8, 512], BF16) for i in range(NWD)]
            bgu, b_bgu = self.sb(ph, "bgu", [128, NE, 2, 8], F32)
            S.dma("sp", bgu[:], self.dram["bgu_l"][l].rearrange("p (e g c) -> p e g c", e=NE, g=2), w=[b_bgu])
            NTMP = 3
            Gt = [self.sb(ph, "Gt%d" % i, [128, 512], F32) for i in range(NTMP)]
            St = [self.sb(ph, "St%d" % i, [128, 512], BF16) for i in range(NTMP)]
            Ut = [self.sb(ph, "Ut%d" % i, [128, 512], F32) for i in range(NTMP)]
            Yt = [self.sb(ph, "Yt%d" % i, [128, 512], F32) for i in range(3)]
            gu_l = self.dram["gu_l"]
            wd_l = self.dram["wd_l"]
            b_down = self.dram["b_down"]
            ucnt = 0
            wcnt = 0
            ecnt = 0
            ycnt = 0
            deferred = []
            for ex in range(nex):
                for c in range(8):
                    gu, b_gu = gus[ucnt % NGU]
                    ucnt += 1
                    S.dma("pool", gu[:], gu_l[l, ex, c].rearrange("p (g k f) -> p g k f", g=2, k=8), w=[b_gu])
                    for tt in range(4):
                        gi = 0 if tt < 2 else 1
                        g = GROUPS[gi]
                        lt0 = tt * 512 - g.tok0
                        segs = [(0, 512, tt * 512)]
                        pg, pu = (ecnt % 2) * 2, (ecnt % 2) * 2 + 1
                        for which, pb in ((0, pg), (1, pu)):
                            for (o0, n, col) in segs:
                                self.mm(self.ps[pb][:, o0:o0 + n], self.bps[pb],
                                        [(gu[:, which, kc, :], self.hT[:, kc, col:col + n]) for kc in range(8)],
                                        [b_gu, self.bh[gi]])
                        G, bG = Gt[ecnt % NTMP]
                        Sg, bS = St[ecnt % NTMP]
                        U, bU = Ut[ecnt % NTMP]
                        ecnt += 1
                        S.op("dve", lambda e, G=G, pg=pg, ex=ex, c=c: e.tensor_scalar(out=G[:], in0=self.ps[pg][:], scalar1=bgu[:, ex, 0, c:c + 1], scalar2=7.0, op0=ALU.add, op1=ALU.min),
                             r=[self.bps[pg], b_bgu], w=[bG])
                        S.op("act", lambda e, U=U, pu=pu, ex=ex, c=c: e.activation(out=U[:], in_=self.ps[pu][:], func=AF.Identity, bias=bgu[:, ex, 1, c:c + 1], scale=1.0),
                             r=[self.bps[pu], b_bgu], w=[bU])
                        S.op("act", lambda e, G=G, Sg=Sg: e.activation(out=Sg[:], in_=G[:], func=AF.Sigmoid, scale=1.702), r=[bG], w=[bS])
                        S.op("dve", lambda e, U=U: e.tensor_scalar(out=U[:], in0=U[:], scalar1=7.0, scalar2=-7.0, op0=ALU.min, op1=ALU.max), r=[bU], w=[bU])
                        for fn in deferred:
                            fn()
                        deferred = []

                        def tail(G=G, bG=bG, Sg=Sg, bS=bS, U=U, bU=bU, c=c, tt=tt):
                            S.op("dve", lambda e: e.tensor_tensor(out=G[:], in0=G[:], in1=Sg[:], op=ALU.mult), r=[bG, bS], w=[bG])
                            S.op("dve", lambda e: e.scalar_tensor_tensor(out=actT[:, c, tt * 512:(tt + 1) * 512], in0=U[:], scalar=1.0, in1=G[:], op0=ALU.add, op1=ALU.mult),
                                 r=[bU, bG], w=[b_act[c][tt]])
                        deferred.append(tail)
                for fn in deferred:
                    fn()
                deferred = []
                for half in range(2):
                    wd, b_wd = wds[wcnt % NWD]
                    wcnt += 1
                    S.dma("pool", wd[:], wd_l[l, ex, half].rearrange("p (c n) -> p c n", c=8), w=[b_wd])
                    for tb in range(16):
                        gi = 0 if tb < 8 else 1
                        pb = 4 + (ycnt % 2)
                        tt = tb // 4
                        pairs = [(actT[:, c, tb * 128:(tb + 1) * 128], wd[:, c, :]) for c in range(8)]
                        self.mm(self.ps[pb][:], self.bps[pb], pairs, [b_wd] + [b_act[c][tt] for c in range(8)])
                        Y, bY = Yt[ycnt % 3]
                        ycnt += 1
                        S.op("act", lambda e, Y=Y, pb=pb, tb=tb, ex=ex: e.activation(out=Y[:], in_=self.ps[pb][:], func=AF.Copy, scale=self.gates[:, tb, ex:ex + 1]),
                             r=[self.bps[pb], self.b_gates], w=[bY])
                        g2t, bg2 = g2[gi]
                        S.op("dve", lambda e, Y=Y, g2t=g2t, half=half: e.tensor_tensor(out=Y[:], in0=Y[:], in1=g2t[:, half * 512:(half + 1) * 512], op=ALU.mult),
                             r=[bY, bg2], w=[bY])
                        xs = self.x_tok[:, tb, half * 512:(half + 1) * 512]
                        S.op("dve", lambda e, Y=Y, xs=xs: e.tensor_tensor(out=xs, in0=xs, in1=Y[:], op=ALU.add), r=[bY], w=[self.bx[tb]])
            S.end_phase(keep=())


    def proj_fm(self, g, taps, M, evac, banks=(6, 7), wbufs=()):
        for i, (lt0, n, col) in enumerate(g.ttiles()):
            pb = banks[i % len(banks)]
            pairs = []
            for (wt, shift) in taps:
                for kc in range(8):
                    pairs.append((wt(kc), self.hT[:, kc, col + shift:col + shift + n]))
            self.mm(self.ps[pb][0:M, 0:n], self.bps[pb], pairs, [self.bh[g.gi]] + list(wbufs))
            evac(self.ps[pb][0:M, 0:n], self.bps[pb], lt0, n)

    def proj_tm(self, g, taps, N, evac, banks=(6, 7), wbufs=()):
        for lb_ in range(g.T // 128):
            pb = banks[lb_ % len(banks)]
            col = g.lcol(lb_ * 128)
            pairs = []
            for (wt, shift) in taps:
                for kc in range(8):
                    pairs.append((self.hT[:, kc, col + shift:col + shift + 128], wt(kc)))
            self.mm(self.ps[pb][:, 0:N], self.bps[pb], pairs, [self.bh[g.gi]] + list(wbufs))
            evac(self.ps[pb][:, 0:N], self.bps[pb], lb_)

    def wload(self, st, name, src, c0, n, q="pool", dt=BF16):
        t, b = self.sb(st, name, [128, 8, n], dt)
        self.S.dma(q, t[:], src.rearrange("(k p) n -> p k n", p=128)[:, :, c0:c0 + n], w=[b])
        return t, b

    def head_rms_gate(self, hs, o, bo, gate, bgate, zdst, bz, T, scratch=None):
        S = self.S
        if scratch is None:
            sq, bsq = self.sb(hs, "hr_sq", [128, T], BF16)
            rs, brs = self.sb(hs, "hr_rs", [128, T], F32)
        else:
            (sq, bsq), (rs, brs) = scratch
        S.op("act", lambda e: e.activation(out=sq[:], in_=o[:], func=AF.Square), r=[bo], w=[bsq])
        for i in range(T // 512):
            pb = 6 + (i % 2)
            sl = slice(i * 512, (i + 1) * 512)
            self.mm(self.ps[pb][:], self.bps[pb], [(self.onesb[:], sq[:, sl])], [self.b_onesb, bsq])
            S.op("act", lambda e, pb=pb, sl=sl: e.activation(out=rs[:, sl], in_=self.ps[pb][:], func=AF.Sqrt, bias=self.eps_t[:], scale=1.0 / 128.0),
                 r=[self.bps[pb], self.b_eps], w=[brs])
        S.op("dve", lambda e: e.reciprocal(out=rs[:], in_=rs[:]), r=[brs], w=[brs])
        S.op("dve", lambda e: e.tensor_tensor(out=rs[:], in0=rs[:], in1=o[:], op=ALU.mult), r=[brs, bo], w=[brs])
        S.op("dve", lambda e: e.tensor_tensor(out=zdst, in0=rs[:], in1=gate[:], op=ALU.mult), r=[brs, bgate], w=[bz])

    def ph_even(self):
        S = self.S
        with ExitStack() as ph:
            zT, _ = self.sb(ph, "zT", [128, 8, NT], BF16)
            bz = [[S.buf("z%d_%d" % (gi, h)) for h in range(8)] for gi in range(2)]
            lbr, b_lbr = self.sb(ph, "lbr", [128, 3, 4], F32)
            S.dma("sp", lbr[:], self.dram["hg_lb"].rearrange("r (h p) -> p r h", p=128), w=[b_lbr], allow_slow_non_contiguous=True)
            lb, b_lb = self.sb(ph, "lb", [128, 4], F32)
            oml, b_oml = self.sb(ph, "oml", [128, 4], F32)
            S.op("act", lambda e: e.activation(out=lbr[:], in_=lbr[:], func=AF.Exp), r=[b_lbr], w=[b_lbr])
            S.op("dve", lambda e: e.tensor_tensor(out=oml[:], in0=lbr[:, 1, :], in1=lbr[:, 2, :], op=ALU.add), r=[b_lbr], w=[b_oml])
            S.op("dve", lambda e: e.tensor_tensor(out=lb[:], in0=oml[:], in1=lbr[:, 0, :], op=ALU.add), r=[b_lbr, b_oml], w=[b_lb])
            S.op("dve", lambda e: e.reciprocal(out=lb[:], in_=lb[:]), r=[b_lb], w=[b_lb])
            S.op("dve", lambda e: e.tensor_tensor(out=oml[:], in0=oml[:], in1=lb[:], op=ALU.mult), r=[b_oml, b_lb], w=[b_oml])
            S.op("dve", lambda e: e.tensor_tensor(out=lb[:], in0=lbr[:, 0, :], in1=lb[:], op=ALU.mult), r=[b_lbr, b_lb], w=[b_lb])
            nmf, b_nmf = self.sb(ph, "nmf", [128, 128], F32)
            nmb, b_nmb = self.sb(ph, "nmb", [128, 128], F32)
            mf64, b_mf64 = self.sb(ph, "mf64", [128, 128], I32)
            mb64, b_mb64 = self.sb(ph, "mb64", [128, 128], I32)
            with ExitStack() as ms:
                io, b_io = self.sb(ms, "io2", [128, 128], I32)
                ion, b_ion = self.sb(ms, "ion2", [128, 128], I32)
                S.op("pool", lambda e: e.iota(io[:], pattern=[[1, 128]], base=0, channel_multiplier=-1), w=[b_io])
                S.op("pool", lambda e: e.iota(ion[:], pattern=[[-1, 128]], base=0, channel_multiplier=1), w=[b_ion])
                S.op("dve", lambda e: e.tensor_scalar(out=mf64[:], in0=io[:], scalar1=0, scalar2=None, op0=ALU.is_ge), r=[b_io], w=[b_mf64])
                S.op("dve", lambda e: e.tensor_scalar(out=mb64[:], in0=ion[:], scalar1=0, scalar2=None, op0=ALU.is_ge), r=[b_ion], w=[b_mb64])
                S.op("dve", lambda e: e.tensor_scalar(out=nmf[:], in0=io[:], scalar1=0, scalar2=30000.0, op0=ALU.is_ge, op1=ALU.mult), r=[b_io], w=[b_nmf])
                S.op("dve", lambda e: e.tensor_scalar(out=nmb[:], in0=ion[:], scalar1=0, scalar2=30000.0, op0=ALU.is_ge, op1=ALU.mult), r=[b_ion], w=[b_nmb])
                S.op("dve", lambda e: e.tensor_scalar(out=nmf[:], in0=nmf[:], scalar1=-30000.0, scalar2=None, op0=ALU.add), r=[b_nmf], w=[b_nmf])
                S.op("dve", lambda e: e.tensor_scalar(out=nmb[:], in0=nmb[:], scalar1=-30000.0, scalar2=None, op0=ALU.add), r=[b_nmb], w=[b_nmb])
                S.op("pool", lambda e: e.memset(mf64[0:64, 64:128], 0), r=[], w=[b_mf64])
                S.op("pool", lambda e: e.memset(mb64[64:128, 0:64], 0), r=[], w=[b_mb64])
                S.end_phase(keep=[x for x in S.phase_bufs if x is not b_io and x is not b_ion])
            self.ev = dict(zT=zT, bz=bz, lb=lb, b_lb=b_lb, oml=oml, b_oml=b_oml, mf64=mf64, b_mf64=b_mf64, mb64=mb64, b_mb64=b_mb64,
                           nmf=nmf, b_nmf=b_nmf, nmb=nmb, b_nmb=b_nmb)
            keep = list(S.phase_bufs)
            for g in GROUPS:
                if "no_hgrn" not in self.plan:
                    self.hgrn_group(g, keep)
                if "no_mlstm" not in self.plan:
                    self.mlstm_group(g, keep)
            self.out_proj(ph, zT, [b for gb in bz for b in gb], "ev_w_out", 0, 2, keep)
            S.end_phase(keep=())

    def out_proj(self, ph, zT, bzs, wname, l, piece, keep):
        S = self.S
        with ExitStack() as os_:
            mp = self.mod_pieces(os_, l, [piece], ["g1"])
            wts = [self.wload(os_, "wo%d" % hf, self.dram[wname], hf * 512, 512) for hf in range(2)]
            tmps = [self.sb(os_, "otmp%d" % i, [128, 512], F32) for i in range(3)]
            cnt = 0
            for tb in range(16):
                gi = 0 if tb < 8 else 1
                g1t, bg1 = mp[(piece, gi)]
                for hf in range(2):
                    wt, bw = wts[hf]
                    pb = 4 + (cnt % 2)
                    tmp, btmp = tmps[cnt % 3]
                    cnt += 1
                    self.mm(self.ps[pb][:], self.bps[pb], [(zT[:, kc, tb * 128:(tb + 1) * 128], wt[:, kc, :]) for kc in range(8)], [bw] + bzs)
                    S.op("dve", lambda e, tmp=tmp, pb=pb, g1t=g1t, hf=hf: e.tensor_tensor(out=tmp[:], in0=self.ps[pb][:], in1=g1t[:, hf * 512:(hf + 1) * 512], op=ALU.mult),
                         r=[self.bps[pb], bg1], w=[btmp])
                    xs = self.x_tok[:, tb, hf * 512:(hf + 1) * 512]
                    S.op("pool", lambda e, xs=xs, tmp=tmp: e.tensor_tensor(out=xs, in0=xs, in1=tmp[:], op=ALU.add), r=[btmp], w=[self.bx[tb]])
            S.end_phase(keep=keep)

    def hgrn_group(self, g, keep):
        S = self.S
        ev = self.ev
        T = g.T
        nb = T // 128
        nck = T // 64
        ncs = g.L // 64
        w_in = self.dram["ev_w_in"]
        with ExitStack() as gs:
            cmask, b_cmask = self.sb(gs, "cmask", [128, T], F32)
            S.op("pool", lambda e: e.memset(cmask[:], 1.0), w=[b_cmask])
            S.op("pool", lambda e: e.memset(cmask[:].rearrange("p (c k) -> p c k", k=64)[:, :, 0:1], 0.0), w=[b_cmask])
            attn = {}
            for d in range(2):
                for blk in range(nb):
                    attn[(d, blk)] = self.sb(gs, "attn%d_%d" % (d, blk), [128, 128], BF16)
                    S.op("pool", lambda e, t=attn[(d, blk)][0]: e.memset(t[:], 0.0), w=[attn[(d, blk)][1]])
            gkeep = keep + list(S.phase_bufs[len(keep):])
            for h in range(4):
                with ExitStack() as hs:
                    wq = self.wload(hs, "hwq", w_in, 0 + h * 128, 128)
                    wi = self.wload(hs, "hwi", w_in, 512 + h * 128, 128)
                    wg = self.wload(hs, "hwg", w_in, 1024 + h * 128, 128)
                    wf = [self.wload(hs, "hwf%d" % d, w_in, 1536 + d * 512 + h * 128, 128) for d in range(2)]
                    q32, bq32 = self.sb(hs, "q32", [128, T], F32)
                    sg, bsg = self.sb(hs, "sg", [128, T], F32)
                    vtok, bvtok = self.sb(hs, "vtok", [128, nb, 128], BF16)
                    self.proj_fm(g, [(lambda kc: wq[0][:, kc, :], 0)], 128,
                                 lambda ps, bp, lt0, n: S.op("act", lambda e: e.copy(out=q32[:, lt0:lt0 + n], in_=ps), r=[bp], w=[bq32]), wbufs=[wq[1]])
                    self.proj_fm(g, [(lambda kc: wg[0][:, kc, :], 0)], 128,
                                 lambda ps, bp, lt0, n: S.op("act", lambda e: e.activation(out=sg[:, lt0:lt0 + n], in_=ps, func=AF.Silu), r=[bp], w=[bsg]), wbufs=[wg[1]])
                    self.proj_tm(g, [(lambda kc: wi[0][:, kc, :], 0)], 128,
                                 lambda ps, bp, lb_: S.op("dve", lambda e: e.tensor_copy(out=vtok[:, lb_, :], in_=ps), r=[bp], w=[bvtok]), wbufs=[wi[1]])
                    ktT, qtT, ktok, dc = {}, {}, {}, {}
                    f_, bf_ = self.sb(hs, "f_", [128, T], F32)
                    lf, blf = self.sb(hs, "lf", [128, T], F32)
                    bb, bbb = self.sb(hs, "bb", [128, T], F32)
                    E, bE = self.sb(hs, "E", [128, T], F32)
                    for d in range(2):
                        ktT[d] = self.sb(hs, "ktT%d" % d, [128, T], BF16)
                        qtT[d] = self.sb(hs, "qtT%d" % d, [128, T], BF16)
                        ktok[d] = self.sb(hs, "ktok%d" % d, [128, nb, 128], BF16)
                        dc[d] = self.sb(hs, "dc%d" % d, [128, nck], F32)
                        self.proj_fm(g, [(lambda kc, d=d: wf[d][0][:, kc, :], 0)], 128,
                                     lambda ps, bp, lt0, n: S.op("act", lambda e: e.activation(out=f_[:, lt0:lt0 + n], in_=ps, func=AF.Sigmoid), r=[bp], w=[bf_]), wbufs=[wf[d][1]])
                        S.op("dve", lambda e: e.tensor_scalar(out=f_[:], in0=f_[:], scalar1=ev["oml"][:, h:h + 1], scalar2=ev["lb"][:, h:h + 1], op0=ALU.mult, op1=ALU.add),
                             r=[bf_, ev["b_oml"], ev["b_lb"]], w=[bf_])
                        S.op("act", lambda e: e.activation(out=lf[:], in_=f_[:], func=AF.Ln), r=[bf_], w=[blf])
                        S.op("pool", lambda e: e.tensor_scalar(out=f_[:], in0=f_[:], scalar1=-1.0, scalar2=1.0, op0=ALU.mult, op1=ALU.add), r=[bf_], w=[bf_])
                        S.op("dve", lambda e: e.tensor_tensor_scan(out=bb[:], data0=cmask[:], data1=lf[:], initial=0.0, op0=ALU.mult, op1=ALU.add),
                             r=[b_cmask, blf], w=[bbb])
                        bv = bb[:].rearrange("p (c k) -> p c k", k=64)
                        S.op("act", lambda e, d=d: e.activation(out=dc[d][0][:], in_=bv[:, :, 63], func=AF.Exp), r=[bbb], w=[dc[d][1]])
                        if d == 0:
                            S.op("dve", lambda e: e.tensor_tensor(out=E[:].rearrange("p (c k) -> p c k", k=64), in0=bv[:, :, 63:64].to_broadcast([128, nck, 64]), in1=bv, op=ALU.subtract),
                                 r=[bbb], w=[bE])
                        else:
                            S.op("dve", lambda e: e.tensor_tensor(out=E[:], in0=bb[:], in1=lf[:], op=ALU.subtract), r=[bbb, blf], w=[bE])
                        S.op("act", lambda e: e.activation(out=lf[:], in_=E[:], func=AF.Exp), r=[bE], w=[blf])
                        S.op("dve", lambda e, d=d: e.tensor_tensor(out=ktT[d][0][:], in0=f_[:], in1=lf[:], op=ALU.mult), r=[bf_, blf], w=[ktT[d][1]])
                        S.op("act", lambda e: e.activation(out=lf[:], in_=E[:], func=AF.Exp, scale=-1.0), r=[bE], w=[blf])
                        S.op("dve", lambda e, d=d: e.tensor_tensor(out=qtT[d][0][:], in0=q32[:], in1=lf[:], op=ALU.mult), r=[bq32, blf], w=[qtT[d][1]])
                        pb = 5
                        psb = self.ps[pb][:].bitcast(BF16)
                        for blk in range(nb):
                            S.op("pe", lambda e, blk=blk, d=d: e.transpose(psb[:, blk * 128:(blk + 1) * 128], ktT[d][0][:, blk * 128:(blk + 1) * 128], self.identb[:]),
                                 r=[ktT[d][1], self.b_identb], w=[self.bps[pb]], inc=(blk == nb - 1))
                        S.op("act", lambda e, d=d: e.copy(out=ktok[d][0][:], in_=psb[:, 0:nb * 128].rearrange("p (b c) -> p b c", c=128)), r=[self.bps[pb]], w=[ktok[d][1]])
                        msk, bmsk = (ev["mf64"], ev["b_mf64"]) if d == 0 else (ev["mb64"], ev["b_mb64"])
                        for blk in range(nb):
                            pb2 = 6 + (blk % 2)
                            sl = slice(blk * 128, (blk + 1) * 128)
                            self.mm(self.ps[pb2][:, 0:128], self.bps[pb2], [(ktT[d][0][:, sl], qtT[d][0][:, sl])], [ktT[d][1], qtT[d][1]])
                            at, bat = attn[(d, blk)]
                            S.op("dve", lambda e, at=at, pb2=pb2, msk=msk: e.copy_predicated(out=at[:], mask=msk[:], data=self.ps[pb2][:, 0:128]),
                                 r=[self.bps[pb2], bmsk], w=[bat])
                    S32 = {}
                    for d in range(2):
                        for j in range(g.nseq):
                            S32[(d, j)] = self.sb(hs, "S32_%d_%d" % (d, j), [128, 128], F32)
                            if g.gi == 0:
                                S.op("pool", lambda e, t=S32[(d, j)][0]: e.memset(t[:], 0.0), w=[S32[(d, j)][1]])
                            else:
                                S.dma("sp", S32[(d, j)][0][:], self.dram["st_hg"][d, h], w=[S32[(d, j)][1]])
                    Sps = [self.sb(hs, "Sp%d" % i, [128, 128], BF16) for i in range(4)]
                    oT = {0: (f_, bf_), 1: (lf, blf)}
                    scnt = 0
                    for c in range(ncs):
                        for d in range(2):
                            for j in range(g.nseq):
                                cl = c if d == 0 else ncs - 1 - c
                                cg = j * ncs + cl
                                blk = cg // 2
                                half = cg % 2
                                p0 = 64 * half
                                t0 = cg * 64
                                St, bSt = S32[(d, j)]
                                Sp, bSp = Sps[scnt % 4]
                                scnt += 1
                                S.op("act", lambda e, Sp=Sp, St=St, d=d, cg=cg: e.activation(out=Sp[:], in_=St[:], func=AF.Copy, scale=dc[d][0][:, cg:cg + 1]),
                                     r=[bSt, dc[d][1]], w=[bSp])
                                ob = d * 2 + (cg // 8)
                                oc = (cg % 8) * 64
                                at, bat = attn[(d, blk)]
                                self.mm(self.ps[ob][:, oc:oc + 64], self.bps[ob],
                                        [(Sp[:], qtT[d][0][:, t0:t0 + 64]),
                                         (vtok[p0:p0 + 64, blk, :], at[p0:p0 + 64, p0:p0 + 64])],
                                        [bSp, qtT[d][1], bvtok, bat])
                                pbs = 4 + (scnt % 2)
                                self.mm(self.ps[pbs][:, 0:128], self.bps[pbs], [(ktok[d][0][p0:p0 + 64, blk, :], vtok[p0:p0 + 64, blk, :])], [ktok[d][1], bvtok])
                                S.op("dve", lambda e, St=St, d=d, cg=cg, pbs=pbs: e.scalar_tensor_tensor(out=St[:], in0=St[:], scalar=dc[d][0][:, cg:cg + 1], in1=self.ps[pbs][:, 0:128], op0=ALU.mult, op1=ALU.add),
                                     r=[bSt, dc[d][1], self.bps[pbs]], w=[bSt])
                    for d in range(2):
                        for i in range(T // 512):
                            ob = d * 2 + i
                            S.op("act", lambda e, d=d, i=i, ob=ob: e.copy(out=oT[d][0][:, i * 512:(i + 1) * 512], in_=self.ps[ob][:]), r=[self.bps[ob]], w=[oT[d][1]])
                        if g.gi == 0:
                            for j in range(g.nseq):
                                self.out_dma(self.dram["o_hg"][j, d, h], S32[(d, j)][0][:], [S32[(d, j)][1]])
                    S.op("pool", lambda e: e.tensor_tensor(out=oT[0][0][:], in0=oT[0][0][:], in1=oT[1][0][:], op=ALU.add), r=[oT[0][1], oT[1][1]], w=[oT[0][1]])
                    self.head_rms_gate(hs, oT[0][0], oT[0][1], sg, bsg, ev["zT"][:, h, g.tok0:g.tok0 + T], ev["bz"][g.gi][h], T)
                    S.end_phase(keep=gkeep)
            S.end_phase(keep=keep)

    def mlstm_group(self, g, keep):
        S = self.S
        ev = self.ev
        T = g.T
        nb = T // 128
        nbs = g.L // 128
        L = g.L
        w_in = self.dram["ev_w_in"]
        ISQ = float(128 ** -0.5)
        with ExitStack() as gs:
            wgt, bwgt = self.wload(gs, "mwg", w_in, 4608, 16)
            gb, bgb = self.sb(gs, "gb", [4, 4], F32)
            S.dma("sp", gb[:], self.dram["ev_gate_b"].rearrange("(g h) -> h g", h=4), w=[bgb], allow_slow_non_contiguous=True)
            ngb, bngb = self.sb(gs, "ngb", [4, 4], F32)
            S.op("dve", lambda e: e.tensor_scalar(out=ngb[:], in0=gb[:], scalar1=-1.0, scalar2=None, op0=ALU.mult), r=[bgb], w=[bngb])
            sel, bsel = self.sb(gs, "sel", [4, 4, 128], F32)
            m0 = None
            if g.gi == 1:
                m0, bm0 = self.sb(gs, "m0", [4, 2], F32)
                S.dma("sp", m0[:], self.dram["st_m"].rearrange("d h -> h d"), w=[bm0], allow_slow_non_contiguous=True)
            GV = {}
            PRE = {}
            for d in range(2):
                PRE[d] = dict(col=self.sb(gs, "col%d" % d, [4, T], F32), enm=self.sb(gs, "enm%d" % d, [4, T], F32),
                              inter=(self.sb(gs, "inter%d" % d, [4, T], F32) if g.gi == 1 else (None, None)),
                              rowT=self.sb(gs, "rowT%d" % d, [128, nb, 4], F32))
            with ExitStack() as ts:
                zer, bzer = self.sb(ts, "zer", [4, L], F32)
                S.op("pool", lambda e: e.memset(zer[:], 0.0), w=[bzer])
                seli, bseli = self.sb(ts, "seli", [4, 4, 128], I32)
                S.op("pool", lambda e: e.iota(seli[:], pattern=[[1, 4], [0, 128]], base=0, channel_multiplier=-1), w=[bseli])
                S.op("dve", lambda e: e.tensor_scalar(out=sel[:], in0=seli[:], scalar1=0, scalar2=None, op0=ALU.is_equal), r=[bseli], w=[bsel])
                ig, big = self.sb(ts, "ig", [4, T], F32)
                lf, blf = self.sb(ts, "mlf", [4, T], F32)
                bcs, bbcs = self.sb(ts, "bcs", [4, T], F32)
                mm_, bmm = self.sb(ts, "mm", [4, T], F32)
                for d in range(2):
                    col, bcol = PRE[d]["col"]
                    enm, benm = PRE[d]["enm"]
                    inter, binter = PRE[d]["inter"]
                    rowT, browT = PRE[d]["rowT"]
                    self.proj_fm(g, [(lambda kc, d=d: wgt[:, kc, 8 * d:8 * d + 4], 0)], 4,
                                 lambda ps, bp, lt0, n: S.op("act", lambda e: e.activation(out=ig[:, lt0:lt0 + n], in_=ps, func=AF.Identity, bias=gb[:, 2 * d:2 * d + 1], scale=1.0), r=[bp, bgb], w=[big]),
                                 wbufs=[bwgt])
                    self.proj_fm(g, [(lambda kc, d=d: wgt[:, kc, 8 * d + 4:8 * d + 8], 0)], 4,
                                 lambda ps, bp, lt0, n: S.op("act", lambda e: e.activation(out=lf[:, lt0:lt0 + n], in_=ps, func=AF.Exp, bias=ngb[:, 2 * d + 1:2 * d + 2], scale=-1.0), r=[bp, bngb], w=[blf]),
                                 wbufs=[bwgt])
                    S.op("act", lambda e: e.activation(out=lf[:], in_=lf[:], func=AF.Ln, bias=1.0, scale=1.0), r=[blf], w=[blf])
                    S.op("dve", lambda e: e.tensor_scalar(out=lf[:], in0=lf[:], scalar1=-1.0, scalar2=None, op0=ALU.mult), r=[blf], w=[blf])
                    for j in range(g.nseq):
                        sl = slice(j * L, (j + 1) * L)
                        if d == 0:
                            v = lambda t: t[:, sl]
                        else:
                            v = lambda t: t[:, sl][:, ::-1]
                        init = 0.0 if g.gi == 0 else m0[:, d:d + 1]
                        S.op("dve", lambda e: e.tensor_tensor_scan(out=v(bcs), data0=v(lf), data1=(zer[:, :] if d == 0 else zer[:, ::-1]), initial=0.0, op0=ALU.add, op1=ALU.add),
                             r=[blf, bzer], w=[bbcs])
                        S.op("dve", lambda e: e.tensor_tensor_scan(out=v(mm_), data0=v(lf), data1=v(ig), initial=init, op0=ALU.add, op1=ALU.max),
                             r=[blf, big] + ([bm0] if g.gi == 1 else []), w=[bmm])
                    S.op("dve", lambda e: e.tensor_tensor(out=col[:], in0=bcs[:], in1=mm_[:], op=ALU.subtract), r=[bbcs, bmm], w=[bcol])
                    S.op("dve", lambda e: e.tensor_tensor(out=ig[:], in0=ig[:], in1=bcs[:], op=ALU.subtract), r=[big, bbcs], w=[big])
                    if g.gi == 1:
                        S.op("act", lambda e: e.activation(out=inter[:], in_=col[:], func=AF.Exp, bias=m0[:, d:d + 1], scale=1.0), r=[bcol, bm0], w=[binter])
                    S.op("act", lambda e: e.activation(out=enm[:], in_=mm_[:], func=AF.Exp, scale=-1.0), r=[bmm], w=[benm])
                    for blk in range(nb):
                        S.op("pe", lambda e, blk=blk: e.transpose(self.ps[7][:, blk * 4:(blk + 1) * 4], ig[0:4, blk * 128:(blk + 1) * 128], self.ident[0:4, 0:4]),
                             r=[big, self.b_ident], w=[self.bps[7]], inc=(blk == nb - 1))
                    S.op("act", lambda e: e.copy(out=rowT[:], in_=self.ps[7][:, 0:nb * 4].rearrange("p (b h) -> p b h", h=4)), r=[self.bps[7]], w=[browT])
                    GV[d] = dict(col=(col, bcol), inter=(inter, binter), enm=(enm, benm), rowT=(rowT, browT))
                    if g.gi == 0:
                        for j in range(g.nseq):
                            tl = (j + 1) * L - 1 if d == 0 else j * L
                            self.out_dma(self.dram["o_m"][j, d].rearrange("(h o) -> h o", o=1), mm_[0:4, tl:tl + 1], [bmm])
                S.end_phase(keep=keep + [x for x in S.phase_bufs if x not in (big, blf, bbcs, bmm, bzer, bseli)])
            gkeep = list(S.phase_bufs)
            for h in range(4):
                with ExitStack() as hs:
                    cw, bcw = self.sb(hs, "cw", [128, 3, 2, 128], F32)
                    for jj in range(3):
                        for qi in range(2):
                            cc = qi * 512 + h * 128
                            S.dma("sp", cw[:, jj, qi, :], self.dram["ev_conv"][jj:jj + 1, cc:cc + 128].partition_broadcast(128), w=[bcw])
                    w32, bw32 = self.sb(hs, "w32", [128, 8, 128], F32)
                    tapt = [self.sb(hs, "wt%d" % jj, [128, 8, 128], BF16) for jj in range(3)]

                    def mk_taps(qi, c0):
                        S.dma("sp", w32[:], w_in.rearrange("(k p) n -> p k n", p=128)[:, :, c0:c0 + 128], w=[bw32])
                        for jj in range(3):
                            wt, bwt = tapt[jj]
                            S.op("dve", lambda e, wt=wt, jj=jj: e.tensor_tensor(out=wt[:], in0=w32[:], in1=cw[:, jj, qi, :].unsqueeze(1).to_broadcast([128, 8, 128]), op=ALU.mult),
                                 r=[bw32, bcw], w=[bwt])
                    tapl = [(lambda kc, jj=jj: tapt[jj][0][:, kc, :], jj - 1) for jj in range(3)]
                    tapb = [tapt[jj][1] for jj in range(3)]
                    wv = self.wload(hs, "mwv", w_in, 3584 + h * 128, 128)
                    wo = self.wload(hs, "mwo", w_in, 4096 + h * 128, 128)
                    qT, bqT = self.sb(hs, "qT", [128, T], BF16)
                    kT, bkT = self.sb(hs, "kT", [128, T], BF16)
                    vtok, bvtok = self.sb(hs, "mvtok", [128, nb, 129], BF16)
                    S.op("pool", lambda e: e.memset(vtok[:, :, 128:129], 1.0), w=[bvtok])
                    mk_taps(0, 2560 + h * 128)
                    self.proj_fm(g, tapl, 128, lambda ps, bp, lt0, n: S.op("act", lambda e: e.activation(out=qT[:, lt0:lt0 + n], in_=ps, func=AF.Silu), r=[bp], w=[bqT]), wbufs=tapb)
                    mk_taps(1, 3072 + h * 128)
                    self.proj_fm(g, tapl, 128, lambda ps, bp, lt0, n: S.op("act", lambda e: e.activation(out=kT[:, lt0:lt0 + n], in_=ps, func=AF.Silu), r=[bp], w=[bkT]), wbufs=tapb)
                    S.op("pool", lambda e: e.tensor_scalar(out=kT[:], in0=kT[:], scalar1=ISQ, scalar2=None, op0=ALU.mult), r=[bkT], w=[bkT])
                    self.proj_tm(g, [(lambda kc: wv[0][:, kc, :], 0)], 128,
                                 lambda ps, bp, lb_: S.op("dve", lambda e: e.tensor_copy(out=vtok[:, lb_, 0:128], in_=ps), r=[bp], w=[bvtok]), wbufs=[wv[1]])
                    if g.gi == 0:
                        ktok, bktok = self.sb(hs, "mktok", [128, nb, 128], BF16)
                        psbk = self.ps[5][:].bitcast(BF16)
                        for blk in range(nb):
                            S.op("pe", lambda e, blk=blk: e.transpose(psbk[:, blk * 128:(blk + 1) * 128], kT[:, blk * 128:(blk + 1) * 128], self.identb[:]),
                                 r=[bkT, self.b_identb], w=[self.bps[5]], inc=(blk == nb - 1))
                        S.op("act", lambda e: e.copy(out=ktok[:], in_=psbk[:, 0:nb * 128].rearrange("p (b c) -> p b c", c=128)), r=[self.bps[5]], w=[bktok])
                    hT0, bhT0 = self.sb(hs, "mh0", [128, T], F32)
                    colbc, bcolbc = self.sb(hs, "colbc", [128, T], F32)
                    enmbc, benmbc = self.sb(hs, "enmbc", [128, T], F32)
                    TW = min(512, L)
                    DT2 = [self.sb(hs, "DT%d" % i, [128, TW], F32) for i in range(2)]
                    dtmp2 = [self.sb(hs, "dtmp%d" % i, [128, 128], F32) for i in range(2)]
                    PT2 = [self.sb(hs, "PT%d" % i, [128, TW], BF16) for i in range(2)]
                    if g.gi == 1:
                        qsc, bqsc = self.sb(hs, "qsc", [128, T], BF16)
                        c0b, bc0b = self.sb(hs, "c0b", [128, 128], BF16)
                        n0, bn0 = self.sb(hs, "n0", [128, 1], F32)
                        n0r, bn0r = self.sb(hs, "n0r", [128, 128], BF16)
                    else:
                        wv_, bwv_ = self.sb(hs, "wv_", [128, 1], F32)
                        kw, bkw = self.sb(hs, "kw", [128, 128], BF16)
                        cn, bcn = self.sb(hs, "cn", [128, 129], F32)
                    pcnt = 0
                    for d in range(2):
                        gv = GV[d]
                        nm_, bnm_ = (ev["nmf"], ev["b_nmf"]) if d == 0 else (ev["nmb"], ev["b_nmb"])
                        for (src, bsrc), (dst, bdst) in ((gv["col"], (colbc, bcolbc)), (gv["enm"], (enmbc, benmbc))):
                            for i in range(T // 512):
                                pb = 6 + (i % 2)
                                self.mm(self.ps[pb][:], self.bps[pb], [(sel[0:4, h, :], src[0:4, i * 512:(i + 1) * 512])], [bsel, bsrc])
                                S.op("act", lambda e, dst=dst, i=i, pb=pb: e.copy(out=dst[:, i * 512:(i + 1) * 512], in_=self.ps[pb][:]), r=[self.bps[pb]], w=[bdst])
                        if g.gi == 1:
                            src, bsrc = gv["inter"]
                            for i in range(T // 512):
                                pb = 6 + (i % 2)
                                self.mm(self.ps[pb][:], self.bps[pb], [(sel[0:4, h, :], src[0:4, i * 512:(i + 1) * 512])], [bsel, bsrc])
                                S.op("dve", lambda e, i=i, pb=pb: e.tensor_tensor(out=qsc[:, i * 512:(i + 1) * 512], in0=qT[:, i * 512:(i + 1) * 512], in1=self.ps[pb][:], op=ALU.mult),
                                     r=[self.bps[pb], bqT], w=[bqsc])
                            S.dma("pool", c0b[:], self.dram["st_c"][d, h], w=[bc0b])
                            S.dma("sp", n0[:], self.dram["st_n"][d, h].rearrange("(p o) -> p o", o=1), w=[bn0])
                            S.op("dve", lambda e: e.tensor_copy(out=n0r[:], in_=n0[:].to_broadcast([128, 128])), r=[bn0], w=[bn0r])
                        rowT, browT = gv["rowT"]
                        tts = g.ttiles()

                        def tile_gen(ti, lt0, n):
                            slot = ti % 2
                            DT, bDT = DT2[slot]
                            dtmp, bdtmp = dtmp2[slot]
                            PT, bPT = PT2[slot]
                            dd, bdd = DT2[slot]
                            j = lt0 // L
                            pn, pd, pst = slot, 2 + slot, 4 + slot
                            tb0, tb1 = lt0 // 128, (lt0 + n) // 128
                            sb_lo, sb_hi = j * nbs, (j + 1) * nbs
                            if d == 0:
                                sblocks = [s for s in range(sb_lo, sb_hi) if s < tb1]
                            else:
                                sblocks = [s for s in range(sb_hi - 1, sb_lo - 1, -1) if s >= tb0]
                            first = True
                            if g.gi == 1:
                                self.mm(self.ps[pn][:, 0:n], self.bps[pn], [(c0b[:], qsc[:, lt0:lt0 + n])], [bc0b, bqsc], start=True, stop=False)
                                self.mm(self.ps[pd][:, 0:n], self.bps[pd], [(n0r[:], qsc[:, lt0:lt0 + n])], [bn0r, bqsc], start=True, stop=False)
                                first = False
                                yield
                            for si, s in enumerate(sblocks):
                                last = (si == len(sblocks) - 1)
                                if d == 0:
                                    a, b_ = max(tb0, s), tb1
                                else:
                                    a, b_ = tb0, min(tb1, s + 1)
                                c_a, c_n = a * 128, (b_ - a) * 128
                                self.mm(self.ps[pst][:, 0:c_n], self.bps[pst], [(kT[:, s * 128:(s + 1) * 128], qT[:, c_a:c_a + c_n])], [bkT, bqT])
                                has_diag = (a <= s < b_)
                                if has_diag:
                                    dcol = s * 128
                                    S.op("dve", lambda e: e.tensor_tensor(out=dtmp[:], in0=colbc[:, dcol:dcol + 128], in1=nm_[:], op=ALU.add), r=[bcolbc, bnm_], w=[bdtmp])
                                    yield
                                    S.op("act", lambda e: e.activation(out=DT[:, dcol - c_a:dcol - c_a + 128], in_=dtmp[:], func=AF.Exp, bias=rowT[:, s, h:h + 1], scale=1.0),
                                         r=[bdtmp, browT], w=[bDT])
                                    if d == 0:
                                        r_a, r_n = dcol + 128, c_a + c_n - (dcol + 128)
                                    else:
                                        r_a, r_n = c_a, dcol - c_a
                                else:
                                    r_a, r_n = c_a, c_n
                                if r_n > 0:
                                    S.op("act", lambda e: e.activation(out=DT[:, r_a - c_a:r_a - c_a + r_n], in_=colbc[:, r_a:r_a + r_n], func=AF.Exp, bias=rowT[:, s, h:h + 1], scale=1.0),
                                         r=[bcolbc, browT], w=[bDT])
                                yield
                                S.op("dve", lambda e: e.tensor_tensor(out=PT[:, 0:c_n], in0=self.ps[pst][:, 0:c_n], in1=DT[:, 0:c_n], op=ALU.mult),
                                     r=[self.bps[pst], bDT], w=[bPT])
                                yield
                                o0 = c_a - lt0
                                self.mm(self.ps[pn][:, o0:o0 + c_n], self.bps[pn], [(vtok[:, s, 0:128], PT[:, 0:c_n])], [bvtok, bPT], start=first, stop=last)
                                self.mm(self.ps[pd][:, o0:o0 + c_n], self.bps[pd], [(self.onesb[:], PT[:, 0:c_n])], [self.b_onesb, bPT], start=first, stop=last)
                                first = False
                                yield
                            S.op("act", lambda e: e.activation(out=dd[:, 0:n], in_=self.ps[pd][:, 0:n], func=AF.Abs), r=[self.bps[pd]], w=[bdd])
                            yield
                            S.op("dve", lambda e: e.tensor_tensor(out=dd[:, 0:n], in0=dd[:, 0:n], in1=enmbc[:, lt0:lt0 + n], op=ALU.max),
                                 r=[bdd, benmbc], w=[bdd])
                            yield
                            S.op("dve", lambda e: e.reciprocal(out=dd[:, 0:n], in_=dd[:, 0:n]), r=[bdd], w=[bdd])
                            yield
                            if d == 0:
                                S.op("dve", lambda e: e.tensor_tensor(out=hT0[:, lt0:lt0 + n], in0=self.ps[pn][:, 0:n], in1=dd[:, 0:n], op=ALU.mult),
                                     r=[self.bps[pn], bdd], w=[bhT0])
                            else:
                                S.op("dve", lambda e: e.tensor_tensor(out=dd[:, 0:n], in0=self.ps[pn][:, 0:n], in1=dd[:, 0:n], op=ALU.mult),
                                     r=[self.bps[pn], bdd], w=[bdd])
                                yield
                                S.op("pool", lambda e: e.tensor_tensor(out=hT0[:, lt0:lt0 + n], in0=hT0[:, lt0:lt0 + n], in1=dd[:, 0:n], op=ALU.add),
                                     r=[bdd, bhT0], w=[bhT0])

                        for p0 in range(0, len(tts), 2):
                            gens = [tile_gen(ti, tts[ti][0], tts[ti][1]) for ti in range(p0, min(p0 + 2, len(tts)))]
                            while gens:
                                for gn in list(gens):
                                    try:
                                        next(gn)
                                    except StopIteration:
                                        gens.remove(gn)
                        if g.gi == 0:
                            for j in range(g.nseq):
                                tl = (j + 1) * L - 1 if d == 0 else j * L
                                for bi in range(nbs):
                                    s = j * nbs + bi
                                    S.op("act", lambda e, s=s, tl=tl: e.activation(out=wv_[:], in_=rowT[:, s, h:h + 1], func=AF.Exp, bias=colbc[:, tl:tl + 1], scale=1.0),
                                         r=[browT, bcolbc], w=[bwv_])
                                    S.op("dve", lambda e, s=s: e.tensor_scalar(out=kw[:], in0=ktok[:, s, :], scalar1=wv_[:, 0:1], scalar2=None, op0=ALU.mult), r=[bktok, bwv_], w=[bkw])
                                    self.mm(self.ps[7][:, 0:129], self.bps[7], [(kw[:], vtok[:, s, 0:129])], [bkw, bvtok], start=(bi == 0), stop=(bi == nbs - 1))
                                S.op("act", lambda e: e.copy(out=cn[:], in_=self.ps[7][:, 0:129]), r=[self.bps[7]], w=[bcn])
                                self.out_dma(self.dram["o_c"][j, d, h], cn[:, 0:128], [bcn])
                                self.out_dma(self.dram["o_n"][j, d, h].rearrange("(p o) -> p o", o=1), cn[:, 128:129], [bcn])
                    og, bog = enmbc, benmbc
                    self.proj_fm(g, [(lambda kc: wo[0][:, kc, :], 0)], 128,
                                 lambda ps, bp, lt0, n: S.op("act", lambda e: e.activation(out=og[:, lt0:lt0 + n], in_=ps, func=AF.Sigmoid), r=[bp], w=[bog]), wbufs=[wo[1]])
                    self.head_rms_gate(hs, hT0, bhT0, og, bog, ev["zT"][:, 4 + h, g.tok0:g.tok0 + T], ev["bz"][g.gi][4 + h], T,
                                       scratch=((kT, bkT), (colbc, bcolbc)))
                    S.end_phase(keep=gkeep)
            S.end_phase(keep=keep)


    def ph_hyena(self):
        S = self.S
        with ExitStack() as ph:
            zT, _ = self.sb(ph, "hzT", [128, 8, NT], BF16)
            bz = [[S.buf("hz%d_%d" % (gi, c)) for c in range(8)] for gi in range(2)]
            ones32, b_ones32 = self.sb(ph, "ones32", [128, 128], F32)
            S.op("pool", lambda e: e.memset(ones32[:], 1.0), w=[b_ones32])
            self.hy = dict(zT=zT, bz=bz, ones32=ones32, b_ones32=b_ones32)
            keep = list(S.phase_bufs)
            for g in GROUPS:
                self.hyena_group(g, keep)
            self.out_proj(ph, zT, [b for gb in bz for b in gb], "hy_w_out", 1, 2, keep)
            S.end_phase(keep=())

    def sin_rr(self, st, x, bx, n, P_):
        S = self.S
        PI = float(np.pi)
        nx, bnx = self.sb(st, "rr_nx", [P_, n], F32)
        y, by = self.sb(st, "rr_y", [P_, n], F32)
        tmp, btmp = self.sb(st, "rr_t", [P_, n], F32)
        S.op("dve", lambda e: e.tensor_scalar(out=nx[:], in0=x[:], scalar1=-1.0, scalar2=None, op0=ALU.mult), r=[bx], w=[bnx])
        S.op("dve", lambda e: e.tensor_copy(out=y[:], in_=x[:]), r=[bx], w=[by])
        for (src, bsrc, thr, delta) in ((x, bx, PI, -2 * PI), (x, bx, 3 * PI, -2 * PI), (nx, bnx, PI, 2 * PI), (nx, bnx, 3 * PI, 2 * PI)):
            S.op("dve", lambda e, src=src, thr=thr, delta=delta: e.tensor_scalar(out=tmp[:], in0=src[:], scalar1=thr, scalar2=delta, op0=ALU.is_ge, op1=ALU.mult), r=[bsrc], w=[btmp])
            S.op("dve", lambda e: e.tensor_tensor(out=y[:], in0=y[:], in1=tmp[:], op=ALU.add), r=[by, btmp], w=[by])
        S.op("act", lambda e: e.activation(out=x[:], in_=y[:], func=AF.Sin), r=[by], w=[bx])

    def hyena_group(self, g, keep):
        S = self.S
        hy = self.hy
        gi = g.gi
        L = g.L
        T = g.T
        nb = T // 128
        nbl = L // 128
        CW = 512 if gi == 0 else 256
        dftc, dfts = self.dram["dftc%d" % gi], self.dram["dfts%d" % gi]
        idftc, idfts = self.dram["idftc%d" % gi], self.dram["idfts%d" % gi]
        w_in = self.dram["hy_w_in"]
        with ExitStack() as gs:
            a2, ba2 = self.sb(gs, "a2T", [64, L], F32)
            tn, btn = self.sb(gs, "tn", [128, nbl], F32)
            S.dma("sp", tn[:], self.dram["tn%d" % gi], w=[btn])
            pmask, bpmask = self.sb(gs, "pmask", [128, nbl], F32)
            S.op("pool", lambda e: e.memset(pmask[:], 1.0), w=[bpmask])
            S.op("pool", lambda e: e.memset(pmask[0:1, 0:1], 0.0), w=[bpmask])
            gk0 = list(S.phase_bufs)
            with ExitStack() as fs:
                zf, bzf = self.sb(fs, "zf", [33, L], F32)
                S.dma("sp", zf[:], self.dram["zfeat%d" % gi], w=[bzf])
                w1, bw1 = self.sb(fs, "hw1", [33, 64], F32)
                S.dma("sp", w1[:], self.dram["hy_w1"], w=[bw1])
                w2, bw2 = self.sb(fs, "hw2", [64, 64], F32)
                S.dma("sp", w2[:], self.dram["hy_w2"], w=[bw2])
                fr, bfr = self.sb(fs, "hfr", [64, 2], F32)
                S.dma("sp", fr[:], self.dram["hy_freq"].rearrange("r h -> h r"), w=[bfr], allow_slow_non_contiguous=True)
                bb_, bbb_ = self.sb(fs, "hbb", [64, 2], F32)
                S.dma("sp", bb_[:, 0:1], self.dram["hy_b1"].rearrange("(h o) -> h o", o=1), w=[bbb_])
                S.dma("sp", bb_[:, 1:2], self.dram["hy_b2"].rearrange("(h o) -> h o", o=1), w=[bbb_])
                S.op("dve", lambda e: e.tensor_tensor(out=bb_[:], in0=bb_[:], in1=fr[:], op=ALU.mult), r=[bbb_, bfr], w=[bbb_])
                a1, ba1 = self.sb(fs, "a1T", [64, L], F32)
                for (dst, bdst, wt, bwt, src, bsrc, li) in ((a1, ba1, w1, bw1, zf, bzf, 0), (a2, ba2, w2, bw2, a1, ba1, 1)):
                    for i in range(0, L, 512):
                        n = min(512, L - i)
                        pb = 6 + ((i // 512) % 2)
                        self.mm(self.ps[pb][0:64, 0:n], self.bps[pb], [(wt[:], src[:, i:i + n])], [bwt, bsrc])
                        S.op("act", lambda e, dst=dst, i=i, n=n, pb=pb, li=li: e.activation(out=dst[:, i:i + n], in_=self.ps[pb][0:64, 0:n], func=AF.Identity, bias=bb_[:, li:li + 1], scale=fr[:, li:li + 1]),
                             r=[self.bps[pb], bbb_, bfr], w=[bdst])
                    self.sin_rr(fs, dst, bdst, L, 64)
                S.end_phase(keep=gk0)
            gkeep = list(S.phase_bufs)
            for p in range(1024 // CW):
                cq = p * CW
                with ExitStack() as pss:
                    Cs = {}
                    for o in range(2):
                        Cs[o] = (self.sb(pss, "C%d" % o, [128, nbl, CW], BF16), self.sb(pss, "D%d" % o, [128, nbl, CW], BF16))
                    pk = list(S.phase_bufs)
                    with ExitStack() as fs:
                        hs, bhs = self.sb(fs, "hs", [128, nbl, CW], BF16)
                        hd, bhd = self.sb(fs, "hd", [128, nbl, CW], BF16)
                        w3 = [self.sb(fs, "w3_%d" % dr, [64, CW], F32) for dr in range(2)]
                        b3 = [self.sb(fs, "b3_%d" % dr, [128, CW], F32) for dr in range(2)]
                        rt = [self.sb(fs, "rt_%d" % dr, [128, CW], F32) for dr in range(2)]
                        fr_ = [self.sb(fs, "fraw%d" % dr, [128, CW], F32) for dr in range(2)]
                        dec, bdec = self.sb(fs, "dec", [128, CW], F32)
                        sq, bsq = self.sb(fs, "fsq", [128, CW], F32)
                        rn, brn = self.sb(fs, "rn", [128, CW], F32)
                        bia, bbia = self.sb(fs, "bia", [128, CW], F32)
                        fcs = [self.sb(fs, "fc%d" % i, [128, nbl, 128], BF16) for i in range(2)]
                        fss = [self.sb(fs, "fs%d" % i, [128, nbl, 128], BF16) for i in range(2)]
                        for o in range(2):
                            (C, bC), (Dd, bD) = Cs[o]
                            for dr in range(2):
                                c0 = dr * 2048 + o * 1024 + cq
                                S.dma("sp", w3[dr][0][:], self.dram["hy_w3"][:, c0:c0 + CW], w=[w3[dr][1]])
                                S.dma("sp", b3[dr][0][:], self.dram["hy_b3"][0:1, c0:c0 + CW].partition_broadcast(128), w=[b3[dr][1]])
                                S.dma("sp", rt[dr][0][:], self.dram["hy_log_rate"][0:1, c0:c0 + CW].partition_broadcast(128), w=[rt[dr][1]])
                                S.op("act", lambda e, dr=dr: e.activation(out=rt[dr][0][:], in_=rt[dr][0][:], func=AF.Exp), r=[rt[dr][1]], w=[rt[dr][1]])
                            S.dma("sp", bia[:], self.dram["hy_bias"][o:o + 1, cq:cq + CW].partition_broadcast(128), w=[bbia])
                            for pb_ in range(nbl):
                                for dr in range(2):
                                    pbk = 6 + dr
                                    self.mm(self.ps[pbk][:, 0:CW], self.bps[pbk], [(a2[:, pb_ * 128:(pb_ + 1) * 128], w3[dr][0][:])], [ba2, w3[dr][1]])
                                    f, bf = fr_[dr]
                                    S.op("dve", lambda e, f=f, pbk=pbk, dr=dr: e.tensor_tensor(out=f[:], in0=self.ps[pbk][:, 0:CW], in1=b3[dr][0][:], op=ALU.add), r=[self.bps[pbk], b3[dr][1]], w=[bf])
                                    S.op("act", lambda e, dr=dr, pb_=pb_: e.activation(out=dec[:], in_=rt[dr][0][:], func=AF.Exp, scale=tn[:, pb_:pb_ + 1]), r=[rt[dr][1], btn], w=[bdec])
                                    S.op("dve", lambda e, f=f: e.tensor_tensor(out=f[:], in0=f[:], in1=dec[:], op=ALU.mult), r=[bf, bdec], w=[bf])
                                    S.op("act", lambda e, f=f: e.activation(out=sq[:], in_=f[:], func=AF.Square), r=[bf], w=[bsq])
                                    self.mm(self.ps[5][:, 0:CW], self.bps[5], [(hy["ones32"][:], sq[:])], [hy["b_ones32"], bsq],
                                            start=(pb_ == 0 and dr == 0), stop=(pb_ == nbl - 1 and dr == 1))
                                S.op("dve", lambda e, pb_=pb_: e.tensor_scalar(out=fr_[1][0][:], in0=fr_[1][0][:], scalar1=pmask[:, pb_:pb_ + 1], scalar2=None, op0=ALU.mult), r=[fr_[1][1], bpmask], w=[fr_[1][1]])
                                S.op("dve", lambda e, pb_=pb_: e.tensor_tensor(out=hs[:, pb_, :], in0=fr_[0][0][:], in1=fr_[1][0][:], op=ALU.add), r=[fr_[0][1], fr_[1][1]], w=[bhs])
                                S.op("dve", lambda e, pb_=pb_: e.tensor_tensor(out=hd[:, pb_, :], in0=fr_[0][0][:], in1=fr_[1][0][:], op=ALU.subtract), r=[fr_[0][1], fr_[1][1]], w=[bhd])
                            S.op("act", lambda e: e.activation(out=rn[:], in_=self.ps[5][:, 0:CW], func=AF.Sqrt), r=[self.bps[5]], w=[brn])
                            S.op("dve", lambda e: e.reciprocal(out=rn[:], in_=rn[:]), r=[brn], w=[brn])
                            for kc in range(nbl):
                                fc, bfc = fcs[kc % 2]
                                fs_, bfs = fss[kc % 2]
                                S.dma("sp", fc[:], dftc[kc], w=[bfc])
                                S.dma("sp", fs_[:], dfts[kc], w=[bfs])
                                self.mm(self.ps[6][:, 0:CW], self.bps[6], [(fc[:, sb_, :], hs[:, sb_, :]) for sb_ in range(nbl)], [bfc, bhs])
                                self.mm(self.ps[7][:, 0:CW], self.bps[7], [(fs_[:, sb_, :], hd[:, sb_, :]) for sb_ in range(nbl)], [bfs, bhd])
                                S.op("dve", lambda e: e.tensor_tensor(out=sq[:], in0=self.ps[6][:, 0:CW], in1=rn[:], op=ALU.mult), r=[self.bps[6], brn], w=[bsq])
                                S.op("pool", lambda e, kc=kc, C=C: e.tensor_tensor(out=C[:, kc, :], in0=sq[:], in1=bia[:], op=ALU.add), r=[bsq, bbia], w=[bC])
                                S.op("dve", lambda e, kc=kc, Dd=Dd: e.tensor_tensor(out=Dd[:, kc, :], in0=self.ps[7][:, 0:CW], in1=rn[:], op=ALU.mult), r=[self.bps[7], brn], w=[bD])
                        S.end_phase(keep=pk)
                    vt, bvt = self.sb(pss, "vt", [128, nb, CW], BF16)
                    x1, bx1 = self.sb(pss, "x1", [128, nb, CW], BF16)
                    x2, bx2 = self.sb(pss, "x2", [128, nb, CW], BF16)
                    PQ, bPQ = self.sb(pss, "PQ", [128, 2 * nbl, CW], BF16)
                    pk2 = list(S.phase_bufs)
                    with ExitStack() as ws:
                        w32s = [self.sb(ws, "hw32_%d" % i, [128, 4, CW], F32) for i in range(2 if gi == 1 else 1)]
                        wcnt_ = 0
                        cwb, bcwb = self.sb(ws, "hcw", [128, 3, CW], F32)
                        tapt = [self.sb(ws, "hwt%d" % jj, [128, 8, CW], BF16) for jj in range(3)]
                        tapl = [(lambda kc, jj=jj: tapt[jj][0][:, kc, :], jj - 1) for jj in range(3)]
                        tapb = [tapt[jj][1] for jj in range(3)]
                        for wi, (dst, bdst) in enumerate(((vt, bvt), (x1, bx1), (x2, bx2))):
                            c0 = wi * 1024 + cq
                            for jj in range(3):
                                S.dma("sp", cwb[:, jj, :], self.dram["hy_conv"][jj:jj + 1, c0:c0 + CW].partition_broadcast(128), w=[bcwb])
                            for hk in range(2):
                                w32, bw32 = w32s[wcnt_ % len(w32s)]
                                wcnt_ += 1
                                S.dma("sp", w32[:], w_in.rearrange("(k p) n -> p k n", p=128)[:, hk * 4:hk * 4 + 4, c0:c0 + CW], w=[bw32])
                                for jj in range(3):
                                    wt, bwt = tapt[jj]
                                    eng = "dve" if jj != 1 else "pool"
                                    S.op(eng, lambda e, wt=wt, jj=jj, hk=hk: e.tensor_tensor(out=wt[:, hk * 4:hk * 4 + 4, :], in0=w32[:], in1=cwb[:, jj, :].unsqueeze(1).to_broadcast([128, 4, CW]), op=ALU.mult),
                                         r=[bw32, bcwb], w=[bwt])
                            self.proj_tm(g, tapl, CW, lambda ps, bp, lb_, dst=dst, bdst=bdst: S.op("act", lambda e: e.copy(out=dst[:, lb_, :], in_=ps), r=[bp], w=[bdst]), wbufs=tapb)
                        S.end_phase(keep=pk2)
                    with ExitStack() as cs:
                        fcs = [self.sb(cs, "sfc%d" % i, [128, nbl, 128], BF16) for i in range(2)]
                        fss = [self.sb(cs, "sfs%d" % i, [128, nbl, 128], BF16) for i in range(2)]
                        ics = [self.sb(cs, "ic%d" % i, [128, nbl, 128], BF16) for i in range(2)]
                        iss = [self.sb(cs, "is%d" % i, [128, nbl, 128], BF16) for i in range(2)]
                        t1s = [self.sb(cs, "ct1_%d" % i, [128, CW], F32) for i in range(2)]
                        t2s = [self.sb(cs, "ct2_%d" % i, [128, CW], F32) for i in range(2)]
                        z2, bz2 = self.sb(cs, "z2", [128, CW], BF16)
                        cnt = 0
                        for o in range(2):
                            (C, bC), (Dd, bD) = Cs[o]
                            gate, bgate = (x1, bx1) if o == 0 else (x2, bx2)
                            for j in range(g.nseq):
                                for kc in range(nbl):
                                    fc, bfc = fcs[cnt % 2]
                                    fs_, bfs = fss[cnt % 2]
                                    t1, bt1 = t1s[cnt % 2]
                                    t2, bt2 = t2s[cnt % 2]
                                    cnt += 1
                                    S.dma("sp", fc[:], dftc[kc], w=[bfc])
                                    S.dma("sp", fs_[:], dfts[kc], w=[bfs])
                                    pa, pb2 = (0, 1) if cnt % 2 else (2, 3)
                                    self.mm(self.ps[pa][:, 0:CW], self.bps[pa], [(fc[:, sb_, :], vt[:, j * nbl + sb_, :]) for sb_ in range(nbl)], [bfc, bvt])
                                    self.mm(self.ps[pb2][:, 0:CW], self.bps[pb2], [(fs_[:, sb_, :], vt[:, j * nbl + sb_, :]) for sb_ in range(nbl)], [bfs, bvt])
                                    S.op("dve", lambda e, t1=t1, pa=pa, kc=kc, C=C: e.tensor_tensor(out=t1[:], in0=self.ps[pa][:, 0:CW], in1=C[:, kc, :], op=ALU.mult), r=[self.bps[pa], bC], w=[bt1])
                                    S.op("dve", lambda e, t2=t2, pb2=pb2, kc=kc, Dd=Dd: e.tensor_tensor(out=t2[:], in0=self.ps[pb2][:, 0:CW], in1=Dd[:, kc, :], op=ALU.mult), r=[self.bps[pb2], bD], w=[bt2])
                                    S.op("pool", lambda e, t1=t1, t2=t2, kc=kc: e.tensor_tensor(out=PQ[:, kc, :], in0=t1[:], in1=t2[:], op=ALU.subtract), r=[bt1, bt2], w=[bPQ])
                                    S.op("dve", lambda e, t1=t1, pa=pa, kc=kc, Dd=Dd: e.tensor_tensor(out=t1[:], in0=self.ps[pa][:, 0:CW], in1=Dd[:, kc, :], op=ALU.mult), r=[self.bps[pa], bD], w=[bt1])
                                    S.op("dve", lambda e, t2=t2, pb2=pb2, kc=kc, C=C: e.tensor_tensor(out=t2[:], in0=self.ps[pb2][:, 0:CW], in1=C[:, kc, :], op=ALU.mult), r=[self.bps[pb2], bC], w=[bt2])
                                    S.op("pool", lambda e, t1=t1, t2=t2, kc=kc: e.tensor_tensor(out=PQ[:, nbl + kc, :], in0=t1[:], in1=t2[:], op=ALU.add), r=[bt1, bt2], w=[bPQ])
                                for tb in range(nbl):
                                    ic, bic = ics[tb % 2]
                                    is_, bis = iss[tb % 2]
                                    S.dma("sp", ic[:], idftc[tb], w=[bic])
                                    S.dma("sp", is_[:], idfts[tb], w=[bis])
                                    pb3 = 4 + (tb % 2)
                                    pairs = [(ic[:, kc, :], PQ[:, kc, :]) for kc in range(nbl)] + [(is_[:, kc, :], PQ[:, nbl + kc, :]) for kc in range(nbl)]
                                    self.mm(self.ps[pb3][:, 0:CW], self.bps[pb3], pairs, [bic, bis, bPQ])
                                    blk = j * nbl + tb
                                    if o == 0:
                                        S.op("dve", lambda e, blk=blk, pb3=pb3: e.tensor_tensor(out=vt[:, blk, :], in0=self.ps[pb3][:, 0:CW], in1=gate[:, blk, :], op=ALU.mult),
                                             r=[self.bps[pb3], bgate], w=[bvt])
                                    else:
                                        S.op("dve", lambda e, blk=blk, pb3=pb3: e.tensor_tensor(out=z2[:], in0=self.ps[pb3][:, 0:CW], in1=gate[:, blk, :], op=ALU.mult),
                                             r=[self.bps[pb3], bgate], w=[bz2])
                                        psb = self.ps[6 + (tb % 2)][:].bitcast(BF16)
                                        pbt = 6 + (tb % 2)
                                        for q in range(CW // 128):
                                            S.op("pe", lambda e, q=q, psb=psb: e.transpose(psb[:, q * 128:(q + 1) * 128], z2[:, q * 128:(q + 1) * 128], self.identb[:]),
                                                 r=[bz2, self.b_identb], w=[self.bps[pbt]], inc=(q == CW // 128 - 1))
                                        tok = g.tok0 + blk * 128
                                        c8 = cq // 128
                                        S.op("act", lambda e, psb=psb, tok=tok, c8=c8: e.copy(out=hy["zT"][:, c8:c8 + CW // 128, tok:tok + 128], in_=psb[:, 0:CW].rearrange("p (q c) -> p q c", c=128)),
                                             r=[self.bps[pbt]], w=[hy["bz"][gi][c8]])
                        S.end_phase(keep=pk2)
                    S.end_phase(keep=gkeep)
            S.end_phase(keep=keep)


def _pos_table():
    n_tok, d, gw = 1024, 1024, 64
    rows = n_tok // gw
    r, col = np.meshgrid(np.arange(rows, dtype=np.float32), np.arange(gw, dtype=np.float32), indexing="ij")
    r = r.reshape(-1)
    col = col.reshape(-1)
    quarter = d // 4
    inv = (1.0 / (np.float32(10000.0) ** (np.arange(quarter, dtype=np.float32) / np.float32(quarter)))).astype(np.float32)
    ar = r[:, None] * inv[None]
    ac = col[:, None] * inv[None]
    return np.concatenate([np.sin(ar), np.cos(ar), np.sin(ac), np.cos(ac)], axis=-1).astype(np.float32)


_NC_CACHE = {}


def get_nc(plan):
    key = tuple(plan)
    if key not in _NC_CACHE:
        b = Builder(list(plan))
        nc = b.build()
        _NC_CACHE[key] = (b, nc)
    return _NC_CACHE[key]


FULL_PLAN = ["load", "norm:0:0", "even", "norm:0:1", "moe:0", "norm:1:0", "hyena", "norm:1:1", "moe:1", "final"]


def make_inputs(inputs, plan=None):
    f = lambda a: np.ascontiguousarray(np.asarray(a, dtype=np.float32))
    x_prompt = f(inputs["x_prompt"])
    x_sample = f(inputs["x_sample"])
    w_gu = f(inputs["w_gu"])
    gu_l = np.ascontiguousarray(
        w_gu.reshape(2, NE, 8, 128, 8, 128, 2).transpose(0, 1, 4, 3, 6, 2, 5)).reshape(2, NE, 8, 128, 2 * 8 * 128)
    wd_l = np.ascontiguousarray(
        f(inputs["w_down"]).reshape(2, NE, 8, 128, 2, 512).transpose(0, 1, 4, 3, 2, 5)).reshape(2, NE, 2, 128, 8 * 512)
    bgu_l = np.ascontiguousarray(
        f(inputs["b_gu"]).reshape(2, NE, 8, 128, 2).transpose(0, 3, 1, 4, 2)).reshape(2, 128, NE * 16)
    pos = _pos_table().reshape(8, 128, 1024)
    common = {
        "pos": pos,
        "w_mod": f(inputs["w_mod"]), "b_mod": f(inputs["b_mod"]), "norm_g": f(inputs["norm_g"]),
        "final_g": f(inputs["final_g"]).reshape(1, 1024),
        "w_router": f(inputs["w_router"]), "b_router": f(inputs["b_router"]),
        "gu_l": gu_l, "wd_l": wd_l, "bgu_l": bgu_l, "b_down": f(inputs["b_down"]),
        "ev_w_in": f(inputs["ev_w_in"])[0], "ev_gate_b": f(inputs["ev_gate_b"])[0], "ev_conv": f(inputs["ev_conv"])[0],
        "hg_lb": f(inputs["hg_lb"]), "ev_w_out": f(inputs["ev_w_out"])[0],
        "hy_w_in": f(inputs["hy_w_in"])[0], "hy_conv": f(inputs["hy_conv"])[0], "hy_w1": f(inputs["hy_w1"])[0],
        "hy_b1": f(inputs["hy_b1"])[0], "hy_w2": f(inputs["hy_w2"])[0], "hy_b2": f(inputs["hy_b2"])[0],
        "hy_w3": f(inputs["hy_w3"])[0], "hy_b3": f(inputs["hy_b3"]), "hy_freq": f(inputs["hy_freq"])[0],
        "hy_log_rate": f(inputs["hy_log_rate"]), "hy_bias": f(inputs["hy_bias"])[0], "hy_w_out": f(inputs["hy_w_out"])[0],
    }
    common.update(_hyena_consts())
    maps = []
    for c in range(NCORES):
        m = dict(common)
        xin = np.concatenate([x_prompt[4 * c:4 * c + 4].reshape(1024, 1024), x_sample[c]], axis=0)
        m["x_in"] = np.ascontiguousarray(xin.reshape(16, 128, 1024))
        cond2 = np.stack([f(inputs["c_ctx"]), f(inputs["c"])[c]], axis=0)
        m["cond_l"] = np.ascontiguousarray(cond2.reshape(2, 8, 128).transpose(2, 1, 0))
        m["st_hg"] = f(inputs["state_hgrn"])[c, 0]
        m["st_c"] = f(inputs["state_mlstm_c"])[c, 0]
        m["st_n"] = f(inputs["state_mlstm_n"])[c, 0]
        m["st_m"] = f(inputs["state_mlstm_m"])[c, 0]
        maps.append(m)
    return maps


def _hyena_consts():
    import ml_dtypes
    out = {}
    for gi, L in enumerate((256, 1024)):
        t = np.arange(L, dtype=np.float32)
        t_norm = (t / np.float32(L - 1)).astype(np.float32)
        bands = np.linspace(1e-4, 15, 16, dtype=np.float32)
        ang = (np.float32(2.0 * np.pi / L) * t[:, None] * bands[None, :]).astype(np.float32)
        z = np.concatenate([t_norm[:, None], np.cos(ang), np.sin(ang)], axis=-1).astype(np.float32)
        out["zfeat%d" % gi] = np.ascontiguousarray(z.T)
        out["tn%d" % gi] = np.ascontiguousarray((-t_norm).reshape(L // 128, 128).T)
        n = 2 * L
        k = np.arange(L, dtype=np.float64)
        w = 2.0 * np.pi * (k + 0.5) / n
        s = np.arange(L, dtype=np.float64)
        ang2 = s[:, None] * w[None, :]
        nbk = L // 128

        def chunked(m):
            return np.ascontiguousarray(m.reshape(nbk, 128, nbk, 128).transpose(2, 1, 0, 3)).astype(ml_dtypes.bfloat16)
        out["dftc%d" % gi] = chunked(np.cos(ang2))
        out["dfts%d" % gi] = chunked(np.sin(ang2))
        out["idftc%d" % gi] = chunked((2.0 / n) * np.cos(ang2.T))
        out["idfts%d" % gi] = chunked((2.0 / n) * np.sin(ang2.T))
    return out


def run_plan(inputs, plan):
    b, nc = get_nc(plan)
    maps = make_inputs(inputs)
    used = set(b.dram.keys())
    maps = [{k: v for k, v in m.items() if k in used} for m in maps]
    res = run_bass_kernel_spmd(nc, maps, core_ids=list(range(NCORES)))
    return res.results


def kernel(**inputs):
    rs = run_plan(inputs, FULL_PLAN)
    y = np.stack([r["y_out"].reshape(2048, 1024) for r in rs], axis=0)
    y_prompt = np.ascontiguousarray(y[:, :1024].reshape(32, 256, 1024)).astype(np.float32)
    y_sample = np.ascontiguousarray(y[:, 1024:]).astype(np.float32)
    o_hg = np.concatenate([r["o_hg"] for r in rs], axis=0).reshape(32, 1, 2, 4, 128, 128).astype(np.float32)
    o_c = np.concatenate([r["o_c"] for r in rs], axis=0).reshape(32, 1, 2, 4, 128, 128).astype(np.float32)
    o_n = np.concatenate([r["o_n"] for r in rs], axis=0).reshape(32, 1, 2, 4, 128).astype(np.float32)
    o_m = np.concatenate([r["o_m"] for r in rs], axis=0).reshape(32, 1, 2, 4).astype(np.float32)
    return (y_prompt, y_sample, o_hg, o_c, o_n, o_m)
```

```python
import numpy as np
from contextlib import ExitStack
import concourse.bass as bass
import concourse.mybir as mybir
from concourse.bass_utils import run_bass_kernel_spmd

F32 = mybir.dt.float32
BF16 = mybir.dt.bfloat16
I32 = mybir.dt.int32
U8 = mybir.dt.uint8
ALU = mybir.AluOpType
AF = mybir.ActivationFunctionType
AX = mybir.AxisListType

NCORES = 8
D = 1024
NT = 2048
NE = 32
EPS = 1e-6


class Buf:
    __slots__ = ("name", "w", "r", "sem")

    def __init__(self, name):
        self.name = name
        self.w = None
        self.r = {}
        self.sem = None


class Sched:
    ENG = ("pe", "act", "dve", "pool", "sp")

    def __init__(self, nc, stack):
        self.nc = nc
        self.stack = stack
        self.eng = {"pe": nc.tensor, "act": nc.scalar, "dve": nc.vector, "pool": nc.gpsimd, "sp": nc.sync}
        self.sems = {}
        self.cnt = {}
        self.seen = {}
        for e in self.ENG:
            self.sems[e] = stack.enter_context(nc.semaphore("s_" + e))
            self.cnt[e] = 0
            self.seen[e] = {}
        self.dma_cnt = {}
        self.dma_free = {}
        self.nsem = 0
        self.n_inst = 0
        self.n_wait = 0
        self.phase_bufs = []

    def buf(self, name):
        b = Buf(name)
        self.phase_bufs.append(b)
        return b

    def _dma_sem(self, buf, q):
        kind = "sw" if q == "pool" else "hw"
        if buf.sem is None:
            buf.sem = {}
        if kind not in buf.sem:
            free = self.dma_free.setdefault(kind, [])
            if free:
                buf.sem[kind] = free.pop()
            else:
                self.nsem += 1
                key = "d%d" % self.nsem
                self.sems[key] = self.stack.enter_context(self.nc.semaphore("sd%s_%d" % (kind, self.nsem)))
                self.dma_cnt[key] = 0
                buf.sem[kind] = key
        return buf.sem[kind]

    def _waits(self, e, r, w):
        deps = {}

        def add(k, v):
            if deps.get(k, 0) < v:
                deps[k] = v
        for b in r:
            if b.w is not None:
                add(*b.w)
        for b in w:
            if b.w is not None:
                add(*b.w)
            for k, v in b.r.items():
                add(k, v)
        seen = self.seen[e]
        for k, v in deps.items():
            if k == e and e == "pe":
                continue
            if seen.get(k, 0) >= v:
                continue
            if k == e and v > self.cnt[e]:
                raise RuntimeError("self-wait on future event %s %d" % (e, v))
            self.eng[e].wait_ge(self.sems[k], v)
            self.n_wait += 1
            seen[k] = v

    def op(self, e, fn, r=(), w=(), inc=True):
        self._waits(e, r, w)
        inst = fn(self.eng[e])
        self.n_inst += 1
        if inc:
            inst.then_inc(self.sems[e], 1)
            self.cnt[e] += 1
            ev = (e, self.cnt[e])
        else:
            ev = (e, self.cnt[e] + 1)
        for b in r:
            if b.r.get(ev[0], 0) < ev[1]:
                b.r[ev[0]] = ev[1]
        for b in w:
            b.w = ev
            b.r = {}
        return inst

    def dma(self, q, out, in_, r=(), w=(), **kw):
        assert len(w) == 1
        self._waits(q, r, w)
        b = w[0]
        key = self._dma_sem(b, q)
        inst = self.eng[q].dma_start(out=out, in_=in_, **kw)
        inst.then_inc(self.sems[key], 16)
        self.n_inst += 1
        self.dma_cnt[key] += 16
        ev = (key, self.dma_cnt[key])
        for x in r:
            if x.r.get(key, 0) < ev[1]:
                x.r[key] = ev[1]
        b.w = ev
        b.r = {}
        return inst

    def barrier(self, extra=()):
        evs = {}
        for e in self.ENG:
            if self.cnt[e] > 0:
                evs[e] = self.cnt[e]
        for k, v in self.dma_cnt.items():
            if v > 0:
                evs[k] = v
        for e in self.ENG:
            seen = self.seen[e]
            for k, v in evs.items():
                if k == e:
                    continue
                if seen.get(k, 0) >= v:
                    continue
                self.eng[e].wait_ge(self.sems[k], v)
                self.n_wait += 1
                seen[k] = v

    def end_phase(self, keep=()):
        self.barrier()
        keepset = set(id(b) for b in keep)
        rest = []
        for b in self.phase_bufs:
            if id(b) in keepset:
                rest.append(b)
                continue
            if b.sem is not None:
                for kind, key in b.sem.items():
                    self.dma_free.setdefault(kind, []).append(key)
                b.sem = None
            b.w = None
            b.r = {}
        self.phase_bufs = rest


class GroupInfo:
    def __init__(self, gi, nseq, L, tok0, col0):
        self.gi = gi
        self.nseq = nseq
        self.L = L
        self.tok0 = tok0
        self.col0 = col0
        self.T = nseq * L
        self.cond = gi

    def col(self, j, i):
        return self.col0 + j * (self.L + 2) + 1 + i

    def lcol(self, lt):
        return self.col(lt // self.L, lt % self.L)

    def ttiles(self):
        out = []
        n = min(self.L, 512)
        for j in range(self.nseq):
            for i0 in range(0, self.L, n):
                out.append((j * self.L + i0, n, self.col(j, i0)))
        return out


GROUPS = [GroupInfo(0, 4, 256, 0, 0), GroupInfo(1, 1, 1024, 1024, 4 * 258)]
HTW = 4 * 258 + 1026


IN_SHAPES = {
    "x_in": ([16, 128, 1024], F32),
    "pos": ([8, 128, 1024], F32),
    "cond_l": ([128, 8, 2], F32),
    "w_mod": ([2, 1024, 6144], F32),
    "b_mod": ([2, 6144], F32),
    "norm_g": ([2, 2, 1024], F32),
    "final_g": ([1, 1024], F32),
    "w_router": ([2, 1024, 32], F32),
    "b_router": ([2, 32], F32),
    "gu_l": ([2, 32, 8, 128, 2 * 8 * 128], F32),
    "wd_l": ([2, 32, 2, 128, 8 * 512], F32),
    "bgu_l": ([2, 128, 32 * 16], F32),
    "b_down": ([2, 32, 1024], F32),
    "ev_w_in": ([1024, 4624], F32),
    "ev_gate_b": ([16], F32),
    "ev_conv": ([3, 1024], F32),
    "hg_lb": ([3, 512], F32),
    "ev_w_out": ([1024, 1024], F32),
    "st_hg": ([2, 4, 128, 128], F32),
    "st_c": ([2, 4, 128, 128], F32),
    "st_n": ([2, 4, 128], F32),
    "st_m": ([2, 4], F32),
    "hy_w_in": ([1024, 3072], F32),
    "hy_conv": ([3, 3072], F32),
    "hy_w1": ([33, 64], F32),
    "hy_b1": ([64], F32),
    "hy_w2": ([64, 64], F32),
    "hy_b2": ([64], F32),
    "hy_w3": ([64, 4096], F32),
    "hy_b3": ([1, 4096], F32),
    "hy_freq": ([2, 64], F32),
    "hy_log_rate": ([1, 4096], F32),
    "hy_bias": ([2, 1024], F32),
    "hy_w_out": ([1024, 1024], F32),
    "zfeat0": ([33, 256], F32),
    "zfeat1": ([33, 1024], F32),
    "tn0": ([128, 2], F32),
    "tn1": ([128, 8], F32),
    "dftc0": ([2, 128, 2, 128], BF16),
    "dfts0": ([2, 128, 2, 128], BF16),
    "dftc1": ([8, 128, 8, 128], BF16),
    "dfts1": ([8, 128, 8, 128], BF16),
    "idftc0": ([2, 128, 2, 128], BF16),
    "idfts0": ([2, 128, 2, 128], BF16),
    "idftc1": ([8, 128, 8, 128], BF16),
    "idfts1": ([8, 128, 8, 128], BF16),
}


class LazyDram(dict):
    def __init__(self, nc):
        super().__init__()
        self.nc = nc

    def __missing__(self, name):
        sh, dt = IN_SHAPES[name]
        t = self.nc.dram_tensor(name, list(sh), dt, kind="ExternalInput").ap()
        self[name] = t
        return t


class Builder:
    def __init__(self, plan):
        self.plan = plan
        self.nc = bass.Bass("TRN2", target_bir_lowering=False)
        self.stack = ExitStack()
        self.S = Sched(self.nc, self.stack)
        self.dram = LazyDram(self.nc)
        self.outs = []

    def din(self, name, shape, dt=F32):
        t = self.nc.dram_tensor(name, list(shape), dt, kind="ExternalInput").ap()
        self.dram[name] = t
        return t

    def dout(self, name, shape, dt=F32):
        t = self.nc.dram_tensor(name, list(shape), dt, kind="ExternalOutput").ap()
        self.dram[name] = t
        return t

    def sb(self, st, name, shape, dt):
        self._uid = getattr(self, "_uid", 0) + 1
        name = "%s_u%d" % (name, self._uid)
        t = st.enter_context(self.nc.sbuf_tensor(name, list(shape), dt))
        return t, self.S.buf(name)

    def out_dma(self, dst, src_ap, src_bufs, q="sp"):
        if len(self.outs) < 12:
            self.outs.append(Buf("out%d" % len(self.outs)))
        self._oc = getattr(self, "_oc", 0) + 1
        b = self.outs[self._oc % len(self.outs)]
        self.S.dma(q, dst, src_ap, r=src_bufs, w=[b])

    def mm(self, ps_ap, ps_buf, pairs, rbufs, start=True, stop=True):
        n = len(pairs)
        for i, (l, r) in enumerate(pairs):
            self.S.op("pe", lambda e, l=l, r=r, i=i: e.matmul(ps_ap, lhsT=l, rhs=r, start=(start and i == 0), stop=(stop and i == n - 1)),
                      r=rbufs, w=[ps_buf], inc=(i == n - 1))

    def build(self):
        nc, S = self.nc, self.S
        st = self.stack
        P = self.plan
        x_in = self.dram["x_in"]
        pos = self.dram["pos"]
        cond_l = self.dram["cond_l"]
        y_out = self.dout("y_out", [16, 128, 1024])
        self.dout("o_hg", [4, 2, 4, 128, 128])
        self.dout("o_c", [4, 2, 4, 128, 128])
        self.dout("o_n", [4, 2, 4, 128])
        self.dout("o_m", [4, 2, 4])
        if "dbg_h" in P:
            self.dout("dbg_h", [128, 8, HTW])
        if "dbg_x" in P:
            self.dout("dbg_x", [16, 128, 1024])

        self.x_tok, self.b_x = self.sb(st, "x_tok", [128, 16, 1024], F32)
        self.bx = [Buf("x%d" % i) for i in range(16)]
        self.hT, _ = self.sb(st, "hT", [128, 8, HTW], BF16)
        self.bh = [Buf("h0"), Buf("h1")]
        self.ident, self.b_ident = self.sb(st, "ident", [128, 128], F32)
        self.identb, self.b_identb = self.sb(st, "identb", [128, 128], BF16)
        self.onesb, self.b_onesb = self.sb(st, "onesb", [128, 128], BF16)
        self.eps_t, self.b_eps = self.sb(st, "eps", [128, 1], F32)
        self.gates, self.b_gates = self.sb(st, "gates", [128, 16, NE], F32)
        self.ps = []
        self.bps = []
        for i in range(8):
            t = st.enter_context(nc.psum_tensor("ps%d" % i, [128, 512], F32))
            self.ps.append(t)
            self.bps.append(Buf("ps%d" % i))

        with ExitStack() as ph:
            io, b_io = self.sb(ph, "io", [128, 128], I32)
            S.op("pool", lambda e: e.iota(io[:], pattern=[[1, 128]], base=0, channel_multiplier=-1), w=[b_io])
            S.op("dve", lambda e: e.tensor_scalar(out=self.ident[:], in0=io[:], scalar1=0, scalar2=None, op0=ALU.is_equal), r=[b_io], w=[self.b_ident])
            S.op("dve", lambda e: e.tensor_copy(out=self.identb[:], in_=self.ident[:]), r=[self.b_ident], w=[self.b_identb])
            S.op("pool", lambda e: e.memset(self.onesb[:], 1.0), w=[self.b_onesb])
            S.op("pool", lambda e: e.memset(self.eps_t[:], EPS), w=[self.b_eps])
            S.op("pool", lambda e: e.memset(self.hT[:], 0.0), w=self.bh)
            S.end_phase()
        if "load" in P:
            for tb in range(16):
                S.dma("sp", self.x_tok[:, tb, :], x_in[tb], w=[self.bx[tb]])
        self.ph_modrows()

        for step in P:
            if step == "load":
                self.ph_load(x_in, pos)
            elif step.startswith("norm"):
                _, l, n = step.split(":")
                self.ph_norm(int(l), int(n))
            elif step.startswith("moe"):
                sp = step.split(":")
                self.ph_moe(int(sp[1]), nex=(int(sp[2]) if len(sp) > 2 else NE))
            elif step == "even":
                self.ph_even()
            elif step == "hyena":
                self.ph_hyena()
            elif step == "final":
                self.ph_final(y_out)
            elif step == "dbg_h":
                with ExitStack() as ph:
                    for kc in range(8):
                        t32, b32 = self.sb(ph, "dbgh%d" % kc, [128, HTW], F32)
                        S.op("dve", lambda e: e.tensor_copy(out=t32[:], in_=self.hT[:, kc, :]), r=self.bh, w=[b32])
                        self.out_dma(self.dram["dbg_h"][:, kc, :], t32[:], [b32])
                    S.end_phase()
            elif step == "dbg_x":
                for tb in range(16):
                    self.out_dma(self.dram["dbg_x"][tb], self.x_tok[:, tb, :], [self.bx[tb]])
        S._waits("sp", self.outs, ())
        S.barrier()
        self.stack.close()
        return nc

    def ph_load(self, x_in, pos):
        S = self.S
        with ExitStack() as ph:
            pts = [self.sb(ph, "pos%d" % i, [128, 1024], F32) for i in range(2)]
            for i in range(8):
                pt, bp = pts[i % 2]
                S.dma("sp", pt[:], pos[i], w=[bp])
                tb = 8 + i
                S.op("dve", lambda e, pt=pt, tb=tb: e.tensor_tensor(out=self.x_tok[:, tb, :], in0=self.x_tok[:, tb, :], in1=pt[:], op=ALU.add),
                     r=[bp], w=[self.bx[tb]])
            S.end_phase(keep=())

    def ph_modrows(self):
        S = self.S
        self.modrow = self.nc.dram_tensor("modrow", [2, 2, 6144], F32).ap()
        self.b_modrow = [Buf("modrow0"), Buf("modrow1")]
        w_mod = self.dram["w_mod"]
        b_mod = self.dram["b_mod"]
        with ExitStack() as ph:
            c32, b_c32 = self.sb(ph, "c32", [128, 8, 2], F32)
            S.dma("sp", c32[:], self.dram["cond_l"], w=[b_c32])
            S.op("act", lambda e: e.activation(out=c32[:], in_=c32[:], func=AF.Silu), r=[b_c32], w=[b_c32])
            cs2, b_cs2 = self.sb(ph, "cs2", [128, 8, 2], BF16)
            S.op("dve", lambda e: e.tensor_copy(out=cs2[:], in_=c32[:]), r=[b_c32], w=[b_cs2])
            wts = [self.sb(ph, "modw%d" % i, [128, 8, 512], BF16) for i in range(3)]
            bts = [self.sb(ph, "modb%d" % i, [2, 512], F32) for i in range(3)]
            rows = [self.sb(ph, "modr%d" % i, [2, 512], F32) for i in range(3)]
            cnt = 0
            for l in range(2):
                for blk in range(12):
                    c0 = blk * 512
                    wt, bw = wts[cnt % 3]
                    bt, bb = bts[cnt % 3]
                    row, brow = rows[cnt % 3]
                    pb = cnt % 4
                    cnt += 1
                    S.dma("pool", wt[:], w_mod[l].rearrange("(k p) n -> p k n", p=128)[:, :, c0:c0 + 512], w=[bw])
                    S.dma("sp", bt[:], b_mod[l:l + 1, c0:c0 + 512].partition_broadcast(2), w=[bb])
                    self.mm(self.ps[pb][0:2, :], self.bps[pb], [(cs2[:, kc, :], wt[:, kc, :]) for kc in range(8)], [b_cs2, bw])
                    S.op("dve", lambda e, row=row, pb=pb, bt=bt: e.tensor_tensor(out=row[:], in0=self.ps[pb][0:2, :], in1=bt[:], op=ALU.add),
                         r=[self.bps[pb], bb], w=[brow])
                    S.dma("sp", self.modrow[l, :, c0:c0 + 512], row[:], r=[brow], w=[self.b_modrow[l]])
            S.end_phase(keep=())

    def mod_pieces(self, ph, l, pieces, names):
        S = self.S
        res = {}
        for pi, k in enumerate(pieces):
            for cond in range(2):
                t, b = self.sb(ph, "%s_%d" % (names[pi], cond), [128, 1024], F32)
                S.dma("sp", t[:], self.modrow[l, cond:cond + 1, k * 1024:(k + 1) * 1024].partition_broadcast(128), r=[self.b_modrow[l]], w=[b])
                res[(k, cond)] = (t, b)
        return res

    def ph_norm(self, l, n):
        S = self.S
        with ExitStack() as ph:
            mp = self.mod_pieces(ph, l, [3 * n, 3 * n + 1], ["sh", "sc"])
            g_bc, b_g = self.sb(ph, "g_bc", [128, 1024], F32)
            S.dma("sp", g_bc[:], self.dram["norm_g"][l, n:n + 1, :].partition_broadcast(128), w=[b_g])
            for cond in range(2):
                sc, bsc = mp[(3 * n + 1, cond)]
                S.op("dve", lambda e, sc=sc: e.scalar_tensor_tensor(out=sc[:], in0=sc[:], scalar=1.0, in1=g_bc[:], op0=ALU.add, op1=ALU.mult),
                     r=[bsc, b_g], w=[bsc])
            if n == 0:
                for g in GROUPS:
                    for j in range(g.nseq):
                        for cpad in (g.col(j, 0) - 1, g.col(j, g.L - 1) + 1):
                            S.op("pool", lambda e, cpad=cpad: e.memset(self.hT[:, :, cpad:cpad + 1], 0.0), w=[self.bh[g.gi]])
                self.norm_blocks(ph, lambda cond: mp[(3 * n + 1, cond)], lambda cond: mp[(3 * n, cond)], router=None)
            else:
                mp2 = self.mod_pieces(ph, l, [5], ["g2n"])
                self.norm_blocks(ph, lambda cond: mp[(3 * n + 1, cond)], lambda cond: mp[(3 * n, cond)], router=l, g2=[mp2[(5, c)] for c in range(2)])
            S.end_phase(keep=())

    def norm_blocks(self, ph, a_of, sh_of, router=None, final_out=None, g2=None):
        S = self.S
        junk, b_junk = self.sb(ph, "junk", [128, 1024], F32)
        tmps = [self.sb(ph, "ntmp%d" % i, [128, 1024], F32) for i in range(2)]
        xns = [self.sb(ph, "xn%d" % i, [128, 1024], F32) for i in range(2)]
        ss, b_ss = self.sb(ph, "ss", [128, 16], F32)
        if router is not None:
            wr, b_wr = self.sb(ph, "wr", [128, 8, 32], F32)
            S.dma("sp", wr[:], self.dram["w_router"][router].rearrange("(k p) n -> p k n", p=128), w=[b_wr])
            br, b_br = self.sb(ph, "br", [128, 32], F32)
            S.dma("sp", br[:], self.dram["b_router"][router:router + 1, :].partition_broadcast(128), w=[b_br])
            xT32s = [self.sb(ph, "xT32_%d" % i, [128, 8, 128], F32) for i in range(2)]
            lg, b_lg = self.sb(ph, "lg", [128, 32], F32)
            t8, b_t8 = self.sb(ph, "t8", [128, 8], F32)
            msk, b_msk = self.sb(ph, "msk", [128, 32], F32)
            ex, b_ex = self.sb(ph, "ex", [128, 32], F32)
            sm, b_sm = self.sb(ph, "sm", [128, 2], F32)
            bd32, b_bd32 = self.sb(ph, "bd32", [NE, 1024], F32)
            S.dma("sp", bd32[:], self.dram["b_down"][router], w=[b_bd32])
            gT, b_gT = self.sb(ph, "gT", [NE, 128], F32)
            btmp_t = [self.sb(ph, "bdt%d" % i, [128, 512], F32) for i in range(2)]
        for tb in range(16):
            gi = 0 if tb < 8 else 1
            g = GROUPS[gi]
            a, ba = a_of(gi)
            x = self.x_tok[:, tb, :]
            tmp, btmp = tmps[tb % 2]
            xn, bxn = xns[tb % 2]
            S.op("act", lambda e, x=x, tb=tb: e.activation(out=junk[:], in_=x, func=AF.Square, accum_out=ss[:, tb:tb + 1]),
                 r=[self.bx[tb]], w=[b_junk, b_ss])
            S.op("act", lambda e, tb=tb: e.activation(out=ss[:, tb:tb + 1], in_=ss[:, tb:tb + 1], func=AF.Sqrt, bias=self.eps_t[:], scale=1.0 / 1024.0),
                 r=[b_ss, self.b_eps], w=[b_ss])
            S.op("dve", lambda e, tb=tb: e.reciprocal(out=ss[:, tb:tb + 1], in_=ss[:, tb:tb + 1]), r=[b_ss], w=[b_ss])
            S.op("dve", lambda e, x=x, tb=tb, tmp=tmp, a=a: e.scalar_tensor_tensor(out=tmp[:], in0=x, scalar=ss[:, tb:tb + 1], in1=a[:], op0=ALU.mult, op1=ALU.mult),
                 r=[self.bx[tb], b_ss, ba], w=[btmp])
            if final_out is not None:
                self.out_dma(final_out[tb], tmp[:], [btmp])
                continue
            sh, bsh = sh_of(gi)
            S.op("dve", lambda e, xn=xn, tmp=tmp, sh=sh: e.tensor_tensor(out=xn[:], in0=tmp[:], in1=sh[:], op=ALU.add),
                 r=[btmp, bsh], w=[bxn])
            lt0 = tb * 128 - g.tok0
            col = g.lcol(lt0) if router is None else tb * 128
            for hb in range(2):
                pb = 2 * (tb % 2) + hb
                for q in range(4):
                    kc = hb * 4 + q
                    S.op("pe", lambda e, pb=pb, q=q, kc=kc, xn=xn: e.transpose(self.ps[pb][:, q * 128:(q + 1) * 128], xn[:, kc * 128:(kc + 1) * 128], self.ident[:]),
                         r=[bxn, self.b_ident], w=[self.bps[pb]], inc=(q == 3))
                src = self.ps[pb][:].rearrange("p (q c) -> p q c", c=128)
                if router is None:
                    S.op("act", lambda e, hb=hb, col=col, src=src: e.copy(out=self.hT[:, hb * 4:hb * 4 + 4, col:col + 128], in_=src),
                         r=[self.bps[pb]], w=[self.bh[gi]])
                else:
                    xT32, bxT = xT32s[tb % 2]
                    S.op("act", lambda e, hb=hb, src=src, xT32=xT32: e.copy(out=xT32[:, hb * 4:hb * 4 + 4, :], in_=src),
                         r=[self.bps[pb]], w=[bxT])
                    S.op("dve", lambda e, hb=hb, col=col, xT32=xT32: e.tensor_copy(out=self.hT[:, hb * 4:hb * 4 + 4, col:col + 128], in_=xT32[:, hb * 4:hb * 4 + 4, :]),
                         r=[bxT], w=[self.bh[gi]])
            if router is not None:
                xT32, bxT = xT32s[tb % 2]
                pb = 4 + (tb % 2)
                self.mm(self.ps[pb][:, 0:32], self.bps[pb], [(xT32[:, kc, :], wr[:, kc, :]) for kc in range(8)], [bxT, b_wr])
                S.op("dve", lambda e, pb=pb: e.tensor_tensor(out=lg[:], in0=self.ps[pb][:, 0:32], in1=br[:], op=ALU.add), r=[self.bps[pb], b_br], w=[b_lg])
                S.op("dve", lambda e: e.max(out=t8[:], in_=lg[:]), r=[b_lg], w=[b_t8])
                S.op("dve", lambda e: e.tensor_scalar(out=msk[:], in0=lg[:], scalar1=t8[:, 3:4], scalar2=None, op0=ALU.is_ge), r=[b_lg, b_t8], w=[b_msk])
                S.op("dve", lambda e: e.tensor_scalar(out=sm[:, 0:1], in0=t8[:, 0:1], scalar1=-1.0, scalar2=None, op0=ALU.mult), r=[b_t8], w=[b_sm])
                S.op("act", lambda e: e.activation(out=ex[:], in_=lg[:], func=AF.Exp, bias=sm[:, 0:1], scale=1.0), r=[b_lg, b_sm], w=[b_ex])
                S.op("dve", lambda e: e.tensor_tensor(out=ex[:], in0=ex[:], in1=msk[:], op=ALU.mult), r=[b_ex, b_msk], w=[b_ex])
                S.op("dve", lambda e: e.reduce_sum(out=sm[:, 1:2], in_=ex[:], axis=AX.X), r=[b_ex], w=[b_sm])
                S.op("dve", lambda e: e.reciprocal(out=sm[:, 1:2], in_=sm[:, 1:2]), r=[b_sm], w=[b_sm])
                S.op("dve", lambda e, tb=tb: e.tensor_scalar(out=self.gates[:, tb, :], in0=ex[:], scalar1=sm[:, 1:2], scalar2=None, op0=ALU.mult),
                     r=[b_ex, b_sm], w=[self.b_gates])
                S.op("pe", lambda e, tb=tb: e.transpose(self.ps[6][0:NE, 0:128], self.gates[:, tb, :], self.ident[:]), r=[self.b_gates, self.b_ident], w=[self.bps[6]])
                S.op("act", lambda e: e.copy(out=gT[:], in_=self.ps[6][0:NE, 0:128]), r=[self.bps[6]], w=[b_gT])
                for half in range(2):
                    pbb = 6 + half
                    self.mm(self.ps[pbb][:], self.bps[pbb], [(gT[:], bd32[:, half * 512:(half + 1) * 512])], [b_gT, b_bd32])
                    bt_, bbt_ = btmp_t[half]
                    g2t, bg2 = g2[gi]
                    S.op("dve", lambda e, bt_=bt_, pbb=pbb, g2t=g2t, half=half: e.tensor_tensor(out=bt_[:], in0=self.ps[pbb][:], in1=g2t[:, half * 512:(half + 1) * 512], op=ALU.mult),
                         r=[self.bps[pbb], bg2], w=[bbt_])
                    xs = self.x_tok[:, tb, half * 512:(half + 1) * 512]
                    S.op("pool", lambda e, xs=xs, bt_=bt_: e.tensor_tensor(out=xs, in0=xs, in1=bt_[:], op=ALU.add), r=[bbt_], w=[self.bx[tb]])

    def ph_final(self, y_out):
        S = self.S
        with ExitStack() as ph:
            g_bc, b_g = self.sb(ph, "fg_bc", [128, 1024], F32)
            S.dma("sp", g_bc[:], self.dram["final_g"].partition_broadcast(128), w=[b_g])
            self.norm_blocks(ph, lambda cond: (g_bc, b_g), None, final_out=y_out)
            S.end_phase(keep=())

    def ph_moe(self, l, nex=NE):
        S = self.S
        nc = self.nc
        with ExitStack() as ph:
            mp = self.mod_pieces(ph, l, [5], ["g2"])
            g2 = [mp[(5, c)] for c in range(2)]
            actT, _ = self.sb(ph, "actT", [128, 8, NT], BF16)
            b_act = [[S.buf("act%d_%d" % (c, tt)) for tt in range(4)] for c in range(8)]
            NGU, NWD = 5, 3
            gus = [self.sb(ph, "gu%d" % i, [128, 2, 8, 128], BF16) for i in range(NGU)]
            wds = [self.sb(ph, "wd%d" % i, [128, 8, 512], BF16) for i in range(NWD)]
            bgu, b_bgu = self.sb(ph, "bgu", [128, NE, 2, 8], F32)
            S.dma("sp", bgu[:], self.dram["bgu_l"][l].rearrange("p (e g c) -> p e g c", e=NE, g=2), w=[b_bgu])
            NTMP = 3
            Gt = [self.sb(ph, "Gt%d" % i, [128, 512], F32) for i in range(NTMP)]
            St = [self.sb(ph, "St%d" % i, [128, 512], BF16) for i in range(NTMP)]
            Ut = [self.sb(ph, "Ut%d" % i, [128, 512], F32) for i in range(NTMP)]
            Yt = [self.sb(ph, "Yt%d" % i, [128, 512], F32) for i in range(3)]
            gu_l = self.dram["gu_l"]
            wd_l = self.dram["wd_l"]
            b_down = self.dram["b_down"]
            ucnt = 0
            wcnt = 0
            ecnt = 0
            ycnt = 0
            deferred = []
            for ex in range(nex):
                for c in range(8):
                    gu, b_gu = gus[ucnt % NGU]
                    ucnt += 1
                    S.dma("pool", gu[:], gu_l[l, ex, c].rearrange("p (g k f) -> p g k f", g=2, k=8), w=[b_gu])
                    for tt in range(4):
                        gi = 0 if tt < 2 else 1
                        g = GROUPS[gi]
                        lt0 = tt * 512 - g.tok0
                        segs = [(0, 512, tt * 512)]
                        pg, pu = (ecnt % 2) * 2, (ecnt % 2) * 2 + 1
                        for which, pb in ((0, pg), (1, pu)):
                            for (o0, n, col) in segs:
                                self.mm(self.ps[pb][:, o0:o0 + n], self.bps[pb],
                                        [(gu[:, which, kc, :], self.hT[:, kc, col:col + n]) for kc in range(8)],
                                        [b_gu, self.bh[gi]])
                        G, bG = Gt[ecnt % NTMP]
                        Sg, bS = St[ecnt % NTMP]
                        U, bU = Ut[ecnt % NTMP]
                        ecnt += 1
                        S.op("dve", lambda e, G=G, pg=pg, ex=ex, c=c: e.tensor_scalar(out=G[:], in0=self.ps[pg][:], scalar1=bgu[:, ex, 0, c:c + 1], scalar2=7.0, op0=ALU.add, op1=ALU.min),
                             r=[self.bps[pg], b_bgu], w=[bG])
                        S.op("act", lambda e, U=U, pu=pu, ex=ex, c=c: e.activation(out=U[:], in_=self.ps[pu][:], func=AF.Identity, bias=bgu[:, ex, 1, c:c + 1], scale=1.0),
                             r=[self.bps[pu], b_bgu], w=[bU])
                        S.op("act", lambda e, G=G, Sg=Sg: e.activation(out=Sg[:], in_=G[:], func=AF.Sigmoid, scale=1.702), r=[bG], w=[bS])
                        S.op("dve", lambda e, U=U: e.tensor_scalar(out=U[:], in0=U[:], scalar1=7.0, scalar2=-7.0, op0=ALU.min, op1=ALU.max), r=[bU], w=[bU])
                        for fn in deferred:
                            fn()
                        deferred = []

                        def tail(G=G, bG=bG, Sg=Sg, bS=bS, U=U, bU=bU, c=c, tt=tt):
                            S.op("dve", lambda e: e.tensor_tensor(out=G[:], in0=G[:], in1=Sg[:], op=ALU.mult), r=[bG, bS], w=[bG])
                            S.op("dve", lambda e: e.scalar_tensor_tensor(out=actT[:, c, tt * 512:(tt + 1) * 512], in0=U[:], scalar=1.0, in1=G[:], op0=ALU.add, op1=ALU.mult),
                                 r=[bU, bG], w=[b_act[c][tt]])
                        deferred.append(tail)
                for fn in deferred:
                    fn()
                deferred = []
                for half in range(2):
                    wd, b_wd = wds[wcnt % NWD]
                    wcnt += 1
                    S.dma("pool", wd[:], wd_l[l, ex, half].rearrange("p (c n) -> p c n", c=8), w=[b_wd])
                    for tb in range(16):
                        gi = 0 if tb < 8 else 1
                        pb = 4 + (ycnt % 2)
                        tt = tb // 4
                        pairs = [(actT[:, c, tb * 128:(tb + 1) * 128], wd[:, c, :]) for c in range(8)]
                        self.mm(self.ps[pb][:], self.bps[pb], pairs, [b_wd] + [b_act[c][tt] for c in range(8)])
                        Y, bY = Yt[ycnt % 3]
                        ycnt += 1
                        S.op("act", lambda e, Y=Y, pb=pb, tb=tb, ex=ex: e.activation(out=Y[:], in_=self.ps[pb][:], func=AF.Copy, scale=self.gates[:, tb, ex:ex + 1]),
                             r=[self.bps[pb], self.b_gates], w=[bY])
                        g2t, bg2 = g2[gi]
                        S.op("dve", lambda e, Y=Y, g2t=g2t, half=half: e.tensor_tensor(out=Y[:], in0=Y[:], in1=g2t[:, half * 512:(half + 1) * 512], op=ALU.mult),
                             r=[bY, bg2], w=[bY])
                        xs = self.x_tok[:, tb, half * 512:(half + 1) * 512]
                        S.op("dve", lambda e, Y=Y, xs=xs: e.tensor_tensor(out=xs, in0=xs, in1=Y[:], op=ALU.add), r=[bY], w=[self.bx[tb]])
            S.end_phase(keep=())


    def proj_fm(self, g, taps, M, evac, banks=(6, 7), wbufs=()):
        for i, (lt0, n, col) in enumerate(g.ttiles()):
            pb = banks[i % len(banks)]
            pairs = []
            for (wt, shift) in taps:
                for kc in range(8):
                    pairs.append((wt(kc), self.hT[:, kc, col + shift:col + shift + n]))
            self.mm(self.ps[pb][0:M, 0:n], self.bps[pb], pairs, [self.bh[g.gi]] + list(wbufs))
            evac(self.ps[pb][0:M, 0:n], self.bps[pb], lt0, n)

    def proj_tm(self, g, taps, N, evac, banks=(6, 7), wbufs=()):
        for lb_ in range(g.T // 128):
            pb = banks[lb_ % len(banks)]
            col = g.lcol(lb_ * 128)
            pairs = []
            for (wt, shift) in taps:
                for kc in range(8):
                    pairs.append((self.hT[:, kc, col + shift:col + shift + 128], wt(kc)))
            self.mm(self.ps[pb][:, 0:N], self.bps[pb], pairs, [self.bh[g.gi]] + list(wbufs))
            evac(self.ps[pb][:, 0:N], self.bps[pb], lb_)

    def wload(self, st, name, src, c0, n, q="pool", dt=BF16):
        t, b = self.sb(st, name, [128, 8, n], dt)
        self.S.dma(q, t[:], src.rearrange("(k p) n -> p k n", p=128)[:, :, c0:c0 + n], w=[b])
        return t, b

    def head_rms_gate(self, hs, o, bo, gate, bgate, zdst, bz, T, scratch=None):
        S = self.S
        if scratch is None:
            sq, bsq = self.sb(hs, "hr_sq", [128, T], BF16)
            rs, brs = self.sb(hs, "hr_rs", [128, T], F32)
        else:
            (sq, bsq), (rs, brs) = scratch
        S.op("act", lambda e: e.activation(out=sq[:], in_=o[:], func=AF.Square), r=[bo], w=[bsq])
        for i in range(T // 512):
            pb = 6 + (i % 2)
            sl = slice(i * 512, (i + 1) * 512)
            self.mm(self.ps[pb][:], self.bps[pb], [(self.onesb[:], sq[:, sl])], [self.b_onesb, bsq])
            S.op("act", lambda e, pb=pb, sl=sl: e.activation(out=rs[:, sl], in_=self.ps[pb][:], func=AF.Sqrt, bias=self.eps_t[:], scale=1.0 / 128.0),
                 r=[self.bps[pb], self.b_eps], w=[brs])
        S.op("dve", lambda e: e.reciprocal(out=rs[:], in_=rs[:]), r=[brs], w=[brs])
        S.op("dve", lambda e: e.tensor_tensor(out=rs[:], in0=rs[:], in1=o[:], op=ALU.mult), r=[brs, bo], w=[brs])
        S.op("dve", lambda e: e.tensor_tensor(out=zdst, in0=rs[:], in1=gate[:], op=ALU.mult), r=[brs, bgate], w=[bz])

    def ph_even(self):
        S = self.S
        with ExitStack() as ph:
            zT, _ = self.sb(ph, "zT", [128, 8, NT], BF16)
            bz = [[S.buf("z%d_%d" % (gi, h)) for h in range(8)] for gi in range(2)]
            lbr, b_lbr = self.sb(ph, "lbr", [128, 3, 4], F32)
            S.dma("sp", lbr[:], self.dram["hg_lb"].rearrange("r (h p) -> p r h", p=128), w=[b_lbr], allow_slow_non_contiguous=True)
            lb, b_lb = self.sb(ph, "lb", [128, 4], F32)
            oml, b_oml = self.sb(ph, "oml", [128, 4], F32)
            S.op("act", lambda e: e.activation(out=lbr[:], in_=lbr[:], func=AF.Exp), r=[b_lbr], w=[b_lbr])
            S.op("dve", lambda e: e.tensor_tensor(out=oml[:], in0=lbr[:, 1, :], in1=lbr[:, 2, :], op=ALU.add), r=[b_lbr], w=[b_oml])
            S.op("dve", lambda e: e.tensor_tensor(out=lb[:], in0=oml[:], in1=lbr[:, 0, :], op=ALU.add), r=[b_lbr, b_oml], w=[b_lb])
            S.op("dve", lambda e: e.reciprocal(out=lb[:], in_=lb[:]), r=[b_lb], w=[b_lb])
            S.op("dve", lambda e: e.tensor_tensor(out=oml[:], in0=oml[:], in1=lb[:], op=ALU.mult), r=[b_oml, b_lb], w=[b_oml])
            S.op("dve", lambda e: e.tensor_tensor(out=lb[:], in0=lbr[:, 0, :], in1=lb[:], op=ALU.mult), r=[b_lbr, b_lb], w=[b_lb])
            nmf, b_nmf = self.sb(ph, "nmf", [128, 128], F32)
            nmb, b_nmb = self.sb(ph, "nmb", [128, 128], F32)
            mf64, b_mf64 = self.sb(ph, "mf64", [128, 128], I32)
            mb64, b_mb64 = self.sb(ph, "mb64", [128, 128], I32)
            with ExitStack() as ms:
                io, b_io = self.sb(ms, "io2", [128, 128], I32)
                ion, b_ion = self.sb(ms, "ion2", [128, 128], I32)
                S.op("pool", lambda e: e.iota(io[:], pattern=[[1, 128]], base=0, channel_multiplier=-1), w=[b_io])
                S.op("pool", lambda e: e.iota(ion[:], pattern=[[-1, 128]], base=0, channel_multiplier=1), w=[b_ion])
                S.op("dve", lambda e: e.tensor_scalar(out=mf64[:], in0=io[:], scalar1=0, scalar2=None, op0=ALU.is_ge), r=[b_io], w=[b_mf64])
                S.op("dve", lambda e: e.tensor_scalar(out=mb64[:], in0=ion[:], scalar1=0, scalar2=None, op0=ALU.is_ge), r=[b_ion], w=[b_mb64])
                S.op("dve", lambda e: e.tensor_scalar(out=nmf[:], in0=io[:], scalar1=0, scalar2=30000.0, op0=ALU.is_ge, op1=ALU.mult), r=[b_io], w=[b_nmf])
                S.op("dve", lambda e: e.tensor_scalar(out=nmb[:], in0=ion[:], scalar1=0, scalar2=30000.0, op0=ALU.is_ge, op1=ALU.mult), r=[b_ion], w=[b_nmb])
                S.op("dve", lambda e: e.tensor_scalar(out=nmf[:], in0=nmf[:], scalar1=-30000.0, scalar2=None, op0=ALU.add), r=[b_nmf], w=[b_nmf])
                S.op("dve", lambda e: e.tensor_scalar(out=nmb[:], in0=nmb[:], scalar1=-30000.0, scalar2=None, op0=ALU.add), r=[b_nmb], w=[b_nmb])
                S.op("pool", lambda e: e.memset(mf64[0:64, 64:128], 0), r=[], w=[b_mf64])
                S.op("pool", lambda e: e.memset(mb64[64:128, 0:64], 0), r=[], w=[b_mb64])
                S.end_phase(keep=[x for x in S.phase_bufs if x is not b_io and x is not b_ion])
            self.ev = dict(zT=zT, bz=bz, lb=lb, b_lb=b_lb, oml=oml, b_oml=b_oml, mf64=mf64, b_mf64=b_mf64, mb64=mb64, b_mb64=b_mb64,
                           nmf=nmf, b_nmf=b_nmf, nmb=nmb, b_nmb=b_nmb)
            keep = list(S.phase_bufs)
            for g in GROUPS:
                if "no_hgrn" not in self.plan:
                    self.hgrn_group(g, keep)
                if "no_mlstm" not in self.plan:
                    self.mlstm_group(g, keep)
            self.out_proj(ph, zT, [b for gb in bz for b in gb], "ev_w_out", 0, 2, keep)
            S.end_phase(keep=())

    def out_proj(self, ph, zT, bzs, wname, l, piece, keep):
        S = self.S
        with ExitStack() as os_:
            mp = self.mod_pieces(os_, l, [piece], ["g1"])
            wts = [self.wload(os_, "wo%d" % hf, self.dram[wname], hf * 512, 512) for hf in range(2)]
            tmps = [self.sb(os_, "otmp%d" % i, [128, 512], F32) for i in range(3)]
            cnt = 0
            for tb in range(16):
                gi = 0 if tb < 8 else 1
                g1t, bg1 = mp[(piece, gi)]
                for hf in range(2):
                    wt, bw = wts[hf]
                    pb = 4 + (cnt % 2)
                    tmp, btmp = tmps[cnt % 3]
                    cnt += 1
                    self.mm(self.ps[pb][:], self.bps[pb], [(zT[:, kc, tb * 128:(tb + 1) * 128], wt[:, kc, :]) for kc in range(8)], [bw] + bzs)
                    S.op("dve", lambda e, tmp=tmp, pb=pb, g1t=g1t, hf=hf: e.tensor_tensor(out=tmp[:], in0=self.ps[pb][:], in1=g1t[:, hf * 512:(hf + 1) * 512], op=ALU.mult),
                         r=[self.bps[pb], bg1], w=[btmp])
                    xs = self.x_tok[:, tb, hf * 512:(hf + 1) * 512]
                    S.op("pool", lambda e, xs=xs, tmp=tmp: e.tensor_tensor(out=xs, in0=xs, in1=tmp[:], op=ALU.add), r=[btmp], w=[self.bx[tb]])
            S.end_phase(keep=keep)

    def hgrn_group(self, g, keep):
        S = self.S
        ev = self.ev
        T = g.T
        nb = T // 128
        nck = T // 64
        ncs = g.L // 64
        w_in = self.dram["ev_w_in"]
        with ExitStack() as gs:
            cmask, b_cmask = self.sb(gs, "cmask", [128, T], F32)
            S.op("pool", lambda e: e.memset(cmask[:], 1.0), w=[b_cmask])
            S.op("pool", lambda e: e.memset(cmask[:].rearrange("p (c k) -> p c k", k=64)[:, :, 0:1], 0.0), w=[b_cmask])
            attn = {}
            for d in range(2):
                for blk in range(nb):
                    attn[(d, blk)] = self.sb(gs, "attn%d_%d" % (d, blk), [128, 128], BF16)
                    S.op("pool", lambda e, t=attn[(d, blk)][0]: e.memset(t[:], 0.0), w=[attn[(d, blk)][1]])
            gkeep = keep + list(S.phase_bufs[len(keep):])
            for h in range(4):
                with ExitStack() as hs:
                    wq = self.wload(hs, "hwq", w_in, 0 + h * 128, 128)
                    wi = self.wload(hs, "hwi", w_in, 512 + h * 128, 128)
                    wg = self.wload(hs, "hwg", w_in, 1024 + h * 128, 128)
                    wf = [self.wload(hs, "hwf%d" % d, w_in, 1536 + d * 512 + h * 128, 128) for d in range(2)]
                    q32, bq32 = self.sb(hs, "q32", [128, T], F32)
                    sg, bsg = self.sb(hs, "sg", [128, T], F32)
                    vtok, bvtok = self.sb(hs, "vtok", [128, nb, 128], BF16)
                    self.proj_fm(g, [(lambda kc: wq[0][:, kc, :], 0)], 128,
                                 lambda ps, bp, lt0, n: S.op("act", lambda e: e.copy(out=q32[:, lt0:lt0 + n], in_=ps), r=[bp], w=[bq32]), wbufs=[wq[1]])
                    self.proj_fm(g, [(lambda kc: wg[0][:, kc, :], 0)], 128,
                                 lambda ps, bp, lt0, n: S.op("act", lambda e: e.activation(out=sg[:, lt0:lt0 + n], in_=ps, func=AF.Silu), r=[bp], w=[bsg]), wbufs=[wg[1]])
                    self.proj_tm(g, [(lambda kc: wi[0][:, kc, :], 0)], 128,
                                 lambda ps, bp, lb_: S.op("dve", lambda e: e.tensor_copy(out=vtok[:, lb_, :], in_=ps), r=[bp], w=[bvtok]), wbufs=[wi[1]])
                    ktT, qtT, ktok, dc = {}, {}, {}, {}
                    f_, bf_ = self.sb(hs, "f_", [128, T], F32)
                    lf, blf = self.sb(hs, "lf", [128, T], F32)
                    bb, bbb = self.sb(hs, "bb", [128, T], F32)
                    E, bE = self.sb(hs, "E", [128, T], F32)
                    for d in range(2):
                        ktT[d] = self.sb(hs, "ktT%d" % d, [128, T], BF16)
                        qtT[d] = self.sb(hs, "qtT%d" % d, [128, T], BF16)
                        ktok[d] = self.sb(hs, "ktok%d" % d, [128, nb, 128], BF16)
                        dc[d] = self.sb(hs, "dc%d" % d, [128, nck], F32)
                        self.proj_fm(g, [(lambda kc, d=d: wf[d][0][:, kc, :], 0)], 128,
                                     lambda ps, bp, lt0, n: S.op("act", lambda e: e.activation(out=f_[:, lt0:lt0 + n], in_=ps, func=AF.Sigmoid), r=[bp], w=[bf_]), wbufs=[wf[d][1]])
                        S.op("dve", lambda e: e.tensor_scalar(out=f_[:], in0=f_[:], scalar1=ev["oml"][:, h:h + 1], scalar2=ev["lb"][:, h:h + 1], op0=ALU.mult, op1=ALU.add),
                             r=[bf_, ev["b_oml"], ev["b_lb"]], w=[bf_])
                        S.op("act", lambda e: e.activation(out=lf[:], in_=f_[:], func=AF.Ln), r=[bf_], w=[blf])
                        S.op("pool", lambda e: e.tensor_scalar(out=f_[:], in0=f_[:], scalar1=-1.0, scalar2=1.0, op0=ALU.mult, op1=ALU.add), r=[bf_], w=[bf_])
                        S.op("dve", lambda e: e.tensor_tensor_scan(out=bb[:], data0=cmask[:], data1=lf[:], initial=0.0, op0=ALU.mult, op1=ALU.add),
                             r=[b_cmask, blf], w=[bbb])
                        bv = bb[:].rearrange("p (c k) -> p c k", k=64)
                        S.op("act", lambda e, d=d: e.activation(out=dc[d][0][:], in_=bv[:, :, 63], func=AF.Exp), r=[bbb], w=[dc[d][1]])
                        if d == 0:
                            S.op("dve", lambda e: e.tensor_tensor(out=E[:].rearrange("p (c k) -> p c k", k=64), in0=bv[:, :, 63:64].to_broadcast([128, nck, 64]), in1=bv, op=ALU.subtract),
                                 r=[bbb], w=[bE])
                        else:
                            S.op("dve", lambda e: e.tensor_tensor(out=E[:], in0=bb[:], in1=lf[:], op=ALU.subtract), r=[bbb, blf], w=[bE])
                        S.op("act", lambda e: e.activation(out=lf[:], in_=E[:], func=AF.Exp), r=[bE], w=[blf])
                        S.op("dve", lambda e, d=d: e.tensor_tensor(out=ktT[d][0][:], in0=f_[:], in1=lf[:], op=ALU.mult), r=[bf_, blf], w=[ktT[d][1]])
                        S.op("act", lambda e: e.activation(out=lf[:], in_=E[:], func=AF.Exp, scale=-1.0), r=[bE], w=[blf])
                        S.op("dve", lambda e, d=d: e.tensor_tensor(out=qtT[d][0][:], in0=q32[:], in1=lf[:], op=ALU.mult), r=[bq32, blf], w=[qtT[d][1]])
                        pb = 5
                        psb = self.ps[pb][:].bitcast(BF16)
                        for blk in range(nb):
                            S.op("pe", lambda e, blk=blk, d=d: e.transpose(psb[:, blk * 128:(blk + 1) * 128], ktT[d][0][:, blk * 128:(blk + 1) * 128], self.identb[:]),
                                 r=[ktT[d][1], self.b_identb], w=[self.bps[pb]], inc=(blk == nb - 1))
                        S.op("act", lambda e, d=d: e.copy(out=ktok[d][0][:], in_=psb[:, 0:nb * 128].rearrange("p (b c) -> p b c", c=128)), r=[self.bps[pb]], w=[ktok[d][1]])
                        msk, bmsk = (ev["mf64"], ev["b_mf64"]) if d == 0 else (ev["mb64"], ev["b_mb64"])
                        for blk in range(nb):
                            pb2 = 6 + (blk % 2)
                            sl = slice(blk * 128, (blk + 1) * 128)
                            self.mm(self.ps[pb2][:, 0:128], self.bps[pb2], [(ktT[d][0][:, sl], qtT[d][0][:, sl])], [ktT[d][1], qtT[d][1]])
                            at, bat = attn[(d, blk)]
                            S.op("dve", lambda e, at=at, pb2=pb2, msk=msk: e.copy_predicated(out=at[:], mask=msk[:], data=self.ps[pb2][:, 0:128]),
                                 r=[self.bps[pb2], bmsk], w=[bat])
                    S32 = {}
                    for d in range(2):
                        for j in range(g.nseq):
                            S32[(d, j)] = self.sb(hs, "S32_%d_%d" % (d, j), [128, 128], F32)
                            if g.gi == 0:
                                S.op("pool", lambda e, t=S32[(d, j)][0]: e.memset(t[:], 0.0), w=[S32[(d, j)][1]])
                            else:
                                S.dma("sp", S32[(d, j)][0][:], self.dram["st_hg"][d, h], w=[S32[(d, j)][1]])
                    Sps = [self.sb(hs, "Sp%d" % i, [128, 128], BF16) for i in range(4)]
                    oT = {0: (f_, bf_), 1: (lf, blf)}
                    scnt = 0
                    for c in range(ncs):
                        for d in range(2):
                            for j in range(g.nseq):
                                cl = c if d == 0 else ncs - 1 - c
                                cg = j * ncs + cl
                                blk = cg // 2
                                half = cg % 2
                                p0 = 64 * half
                                t0 = cg * 64
                                St, bSt = S32[(d, j)]
                                Sp, bSp = Sps[scnt % 4]
                                scnt += 1
                                S.op("act", lambda e, Sp=Sp, St=St, d=d, cg=cg: e.activation(out=Sp[:], in_=St[:], func=AF.Copy, scale=dc[d][0][:, cg:cg + 1]),
                                     r=[bSt, dc[d][1]], w=[bSp])
                                ob = d * 2 + (cg // 8)
                                oc = (cg % 8) * 64
                                at, bat = attn[(d, blk)]
                                self.mm(self.ps[ob][:, oc:oc + 64], self.bps[ob],
                                        [(Sp[:], qtT[d][0][:, t0:t0 + 64]),
                                         (vtok[p0:p0 + 64, blk, :], at[p0:p0 + 64, p0:p0 + 64])],
                                        [bSp, qtT[d][1], bvtok, bat])
                                pbs = 4 + (scnt % 2)
                                self.mm(self.ps[pbs][:, 0:128], self.bps[pbs], [(ktok[d][0][p0:p0 + 64, blk, :], vtok[p0:p0 + 64, blk, :])], [ktok[d][1], bvtok])
                                S.op("dve", lambda e, St=St, d=d, cg=cg, pbs=pbs: e.scalar_tensor_tensor(out=St[:], in0=St[:], scalar=dc[d][0][:, cg:cg + 1], in1=self.ps[pbs][:, 0:128], op0=ALU.mult, op1=ALU.add),
                                     r=[bSt, dc[d][1], self.bps[pbs]], w=[bSt])
                    for d in range(2):
                        for i in range(T // 512):
                            ob = d * 2 + i
                            S.op("act", lambda e, d=d, i=i, ob=ob: e.copy(out=oT[d][0][:, i * 512:(i + 1) * 512], in_=self.ps[ob][:]), r=[self.bps[ob]], w=[oT[d][1]])
                        if g.gi == 0:
                            for j in range(g.nseq):
                                self.out_dma(self.dram["o_hg"][j, d, h], S32[(d, j)][0][:], [S32[(d, j)][1]])
                    S.op("pool", lambda e: e.tensor_tensor(out=oT[0][0][:], in0=oT[0][0][:], in1=oT[1][0][:], op=ALU.add), r=[oT[0][1], oT[1][1]], w=[oT[0][1]])
                    self.head_rms_gate(hs, oT[0][0], oT[0][1], sg, bsg, ev["zT"][:, h, g.tok0:g.tok0 + T], ev["bz"][g.gi][h], T)
                    S.end_phase(keep=gkeep)
            S.end_phase(keep=keep)

    def mlstm_group(self, g, keep):
        S = self.S
        ev = self.ev
        T = g.T
        nb = T // 128
        nbs = g.L // 128
        L = g.L
        w_in = self.dram["ev_w_in"]
        ISQ = float(128 ** -0.5)
        with ExitStack() as gs:
            wgt, bwgt = self.wload(gs, "mwg", w_in, 4608, 16)
            gb, bgb = self.sb(gs, "gb", [4, 4], F32)
            S.dma("sp", gb[:], self.dram["ev_gate_b"].rearrange("(g h) -> h g", h=4), w=[bgb], allow_slow_non_contiguous=True)
            ngb, bngb = self.sb(gs, "ngb", [4, 4], F32)
            S.op("dve", lambda e: e.tensor_scalar(out=ngb[:], in0=gb[:], scalar1=-1.0, scalar2=None, op0=ALU.mult), r=[bgb], w=[bngb])
            sel, bsel = self.sb(gs, "sel", [4, 4, 128], F32)
            m0 = None
            if g.gi == 1:
                m0, bm0 = self.sb(gs, "m0", [4, 2], F32)
                S.dma("sp", m0[:], self.dram["st_m"].rearrange("d h -> h d"), w=[bm0], allow_slow_non_contiguous=True)
            GV = {}
            PRE = {}
            for d in range(2):
                PRE[d] = dict(col=self.sb(gs, "col%d" % d, [4, T], F32), enm=self.sb(gs, "enm%d" % d, [4, T], F32),
                              inter=(self.sb(gs, "inter%d" % d, [4, T], F32) if g.gi == 1 else (None, None)),
                              rowT=self.sb(gs, "rowT%d" % d, [128, nb, 4], F32))
            with ExitStack() as ts:
                zer, bzer = self.sb(ts, "zer", [4, L], F32)
                S.op("pool", lambda e: e.memset(zer[:], 0.0), w=[bzer])
                seli, bseli = self.sb(ts, "seli", [4, 4, 128], I32)
                S.op("pool", lambda e: e.iota(seli[:], pattern=[[1, 4], [0, 128]], base=0, channel_multiplier=-1), w=[bseli])
                S.op("dve", lambda e: e.tensor_scalar(out=sel[:], in0=seli[:], scalar1=0, scalar2=None, op0=ALU.is_equal), r=[bseli], w=[bsel])
                ig, big = self.sb(ts, "ig", [4, T], F32)
                lf, blf = self.sb(ts, "mlf", [4, T], F32)
                bcs, bbcs = self.sb(ts, "bcs", [4, T], F32)
                mm_, bmm = self.sb(ts, "mm", [4, T], F32)
                for d in range(2):
                    col, bcol = PRE[d]["col"]
                    enm, benm = PRE[d]["enm"]
                    inter, binter = PRE[d]["inter"]
                    rowT, browT = PRE[d]["rowT"]
                    self.proj_fm(g, [(lambda kc, d=d: wgt[:, kc, 8 * d:8 * d + 4], 0)], 4,
                                 lambda ps, bp, lt0, n: S.op("act", lambda e: e.activation(out=ig[:, lt0:lt0 + n], in_=ps, func=AF.Identity, bias=gb[:, 2 * d:2 * d + 1], scale=1.0), r=[bp, bgb], w=[big]),
                                 wbufs=[bwgt])
                    self.proj_fm(g, [(lambda kc, d=d: wgt[:, kc, 8 * d + 4:8 * d + 8], 0)], 4,
                                 lambda ps, bp, lt0, n: S.op("act", lambda e: e.activation(out=lf[:, lt0:lt0 + n], in_=ps, func=AF.Exp, bias=ngb[:, 2 * d + 1:2 * d + 2], scale=-1.0), r=[bp, bngb], w=[blf]),
                                 wbufs=[bwgt])
                    S.op("act", lambda e: e.activation(out=lf[:], in_=lf[:], func=AF.Ln, bias=1.0, scale=1.0), r=[blf], w=[blf])
                    S.op("dve", lambda e: e.tensor_scalar(out=lf[:], in0=lf[:], scalar1=-1.0, scalar2=None, op0=ALU.mult), r=[blf], w=[blf])
                    for j in range(g.nseq):
                        sl = slice(j * L, (j + 1) * L)
                        if d == 0:
                            v = lambda t: t[:, sl]
                        else:
                            v = lambda t: t[:, sl][:, ::-1]
                        init = 0.0 if g.gi == 0 else m0[:, d:d + 1]
                        S.op("dve", lambda e: e.tensor_tensor_scan(out=v(bcs), data0=v(lf), data1=(zer[:, :] if d == 0 else zer[:, ::-1]), initial=0.0, op0=ALU.add, op1=ALU.add),
                             r=[blf, bzer], w=[bbcs])
                        S.op("dve", lambda e: e.tensor_tensor_scan(out=v(mm_), data0=v(lf), data1=v(ig), initial=init, op0=ALU.add, op1=ALU.max),
                             r=[blf, big] + ([bm0] if g.gi == 1 else []), w=[bmm])
                    S.op("dve", lambda e: e.tensor_tensor(out=col[:], in0=bcs[:], in1=mm_[:], op=ALU.subtract), r=[bbcs, bmm], w=[bcol])
                    S.op("dve", lambda e: e.tensor_tensor(out=ig[:], in0=ig[:], in1=bcs[:], op=ALU.subtract), r=[big, bbcs], w=[big])
                    if g.gi == 1:
                        S.op("act", lambda e: e.activation(out=inter[:], in_=col[:], func=AF.Exp, bias=m0[:, d:d + 1], scale=1.0), r=[bcol, bm0], w=[binter])
                    S.op("act", lambda e: e.activation(out=enm[:], in_=mm_[:], func=AF.Exp, scale=-1.0), r=[bmm], w=[benm])
                    for blk in range(nb):
                        S.op("pe", lambda e, blk=blk: e.transpose(self.ps[7][:, blk * 4:(blk + 1) * 4], ig[0:4, blk * 128:(blk + 1) * 128], self.ident[0:4, 0:4]),
                             r=[big, self.b_ident], w=[self.bps[7]], inc=(blk == nb - 1))
                    S.op("act", lambda e: e.copy(out=rowT[:], in_=self.ps[7][:, 0:nb * 4].rearrange("p (b h) -> p b h", h=4)), r=[self.bps[7]], w=[browT])
                    GV[d] = dict(col=(col, bcol), inter=(inter, binter), enm=(enm, benm), rowT=(rowT, browT))
                    if g.gi == 0:
                        for j in range(g.nseq):
                            tl = (j + 1) * L - 1 if d == 0 else j * L
                            self.out_dma(self.dram["o_m"][j, d].rearrange("(h o) -> h o", o=1), mm_[0:4, tl:tl + 1], [bmm])
                S.end_phase(keep=keep + [x for x in S.phase_bufs if x not in (big, blf, bbcs, bmm, bzer, bseli)])
            gkeep = list(S.phase_bufs)
            for h in range(4):
                with ExitStack() as hs:
                    cw, bcw = self.sb(hs, "cw", [128, 3, 2, 128], F32)
                    for jj in range(3):
                        for qi in range(2):
                            cc = qi * 512 + h * 128
                            S.dma("sp", cw[:, jj, qi, :], self.dram["ev_conv"][jj:jj + 1, cc:cc + 128].partition_broadcast(128), w=[bcw])
                    w32, bw32 = self.sb(hs, "w32", [128, 8, 128], F32)
                    tapt = [self.sb(hs, "wt%d" % jj, [128, 8, 128], BF16) for jj in range(3)]

                    def mk_taps(qi, c0):
                        S.dma("sp", w32[:], w_in.rearrange("(k p) n -> p k n", p=128)[:, :, c0:c0 + 128], w=[bw32])
                        for jj in range(3):
                            wt, bwt = tapt[jj]
                            S.op("dve", lambda e, wt=wt, jj=jj: e.tensor_tensor(out=wt[:], in0=w32[:], in1=cw[:, jj, qi, :].unsqueeze(1).to_broadcast([128, 8, 128]), op=ALU.mult),
                                 r=[bw32, bcw], w=[bwt])
                    tapl = [(lambda kc, jj=jj: tapt[jj][0][:, kc, :], jj - 1) for jj in range(3)]
                    tapb = [tapt[jj][1] for jj in range(3)]
                    wv = self.wload(hs, "mwv", w_in, 3584 + h * 128, 128)
                    wo = self.wload(hs, "mwo", w_in, 4096 + h * 128, 128)
                    qT, bqT = self.sb(hs, "qT", [128, T], BF16)
                    kT, bkT = self.sb(hs, "kT", [128, T], BF16)
                    vtok, bvtok = self.sb(hs, "mvtok", [128, nb, 129], BF16)
                    S.op("pool", lambda e: e.memset(vtok[:, :, 128:129], 1.0), w=[bvtok])
                    mk_taps(0, 2560 + h * 128)
                    self.proj_fm(g, tapl, 128, lambda ps, bp, lt0, n: S.op("act", lambda e: e.activation(out=qT[:, lt0:lt0 + n], in_=ps, func=AF.Silu), r=[bp], w=[bqT]), wbufs=tapb)
                    mk_taps(1, 3072 + h * 128)
                    self.proj_fm(g, tapl, 128, lambda ps, bp, lt0, n: S.op("act", lambda e: e.activation(out=kT[:, lt0:lt0 + n], in_=ps, func=AF.Silu), r=[bp], w=[bkT]), wbufs=tapb)
                    S.op("pool", lambda e: e.tensor_scalar(out=kT[:], in0=kT[:], scalar1=ISQ, scalar2=None, op0=ALU.mult), r=[bkT], w=[bkT])
                    self.proj_tm(g, [(lambda kc: wv[0][:, kc, :], 0)], 128,
                                 lambda ps, bp, lb_: S.op("dve", lambda e: e.tensor_copy(out=vtok[:, lb_, 0:128], in_=ps), r=[bp], w=[bvtok]), wbufs=[wv[1]])
                    if g.gi == 0:
                        ktok, bktok = self.sb(hs, "mktok", [128, nb, 128], BF16)
                        psbk = self.ps[5][:].bitcast(BF16)
                        for blk in range(nb):
                            S.op("pe", lambda e, blk=blk: e.transpose(psbk[:, blk * 128:(blk + 1) * 128], kT[:, blk * 128:(blk + 1) * 128], self.identb[:]),
                                 r=[bkT, self.b_identb], w=[self.bps[5]], inc=(blk == nb - 1))
                        S.op("act", lambda e: e.copy(out=ktok[:], in_=psbk[:, 0:nb * 128].rearrange("p (b c) -> p b c", c=128)), r=[self.bps[5]], w=[bktok])
                    hT0, bhT0 = self.sb(hs, "mh0", [128, T], F32)
                    colbc, bcolbc = self.sb(hs, "colbc", [128, T], F32)
                    enmbc, benmbc = self.sb(hs, "enmbc", [128, T], F32)
                    TW = min(512, L)
                    DT2 = [self.sb(hs, "DT%d" % i, [128, TW], F32) for i in range(2)]
                    dtmp2 = [self.sb(hs, "dtmp%d" % i, [128, 128], F32) for i in range(2)]
                    PT2 = [self.sb(hs, "PT%d" % i, [128, TW], BF16) for i in range(2)]
                    if g.gi == 1:
                        qsc, bqsc = self.sb(hs, "qsc", [128, T], BF16)
                        c0b, bc0b = self.sb(hs, "c0b", [128, 128], BF16)
                        n0, bn0 = self.sb(hs, "n0", [128, 1], F32)
                        n0r, bn0r = self.sb(hs, "n0r", [128, 128], BF16)
                    else:
                        wv_, bwv_ = self.sb(hs, "wv_", [128, 1], F32)
                        kw, bkw = self.sb(hs, "kw", [128, 128], BF16)
                        cn, bcn = self.sb(hs, "cn", [128, 129], F32)
                    pcnt = 0
                    for d in range(2):
                        gv = GV[d]
                        nm_, bnm_ = (ev["nmf"], ev["b_nmf"]) if d == 0 else (ev["nmb"], ev["b_nmb"])
                        for (src, bsrc), (dst, bdst) in ((gv["col"], (colbc, bcolbc)), (gv["enm"], (enmbc, benmbc))):
                            for i in range(T // 512):
                                pb = 6 + (i % 2)
                                self.mm(self.ps[pb][:], self.bps[pb], [(sel[0:4, h, :], src[0:4, i * 512:(i + 1) * 512])], [bsel, bsrc])
                                S.op("act", lambda e, dst=dst, i=i, pb=pb: e.copy(out=dst[:, i * 512:(i + 1) * 512], in_=self.ps[pb][:]), r=[self.bps[pb]], w=[bdst])
                        if g.gi == 1:
                            src, bsrc = gv["inter"]
                            for i in range(T // 512):
                                pb = 6 + (i % 2)
                                self.mm(self.ps[pb][:], self.bps[pb], [(sel[0:4, h, :], src[0:4, i * 512:(i + 1) * 512])], [bsel, bsrc])
                                S.op("dve", lambda e, i=i, pb=pb: e.tensor_tensor(out=qsc[:, i * 512:(i + 1) * 512], in0=qT[:, i * 512:(i + 1) * 512], in1=self.ps[pb][:], op=ALU.mult),
                                     r=[self.bps[pb], bqT], w=[bqsc])
                            S.dma("pool", c0b[:], self.dram["st_c"][d, h], w=[bc0b])
                            S.dma("sp", n0[:], self.dram["st_n"][d, h].rearrange("(p o) -> p o", o=1), w=[bn0])
                            S.op("dve", lambda e: e.tensor_copy(out=n0r[:], in_=n0[:].to_broadcast([128, 128])), r=[bn0], w=[bn0r])
                        rowT, browT = gv["rowT"]
                        tts = g.ttiles()

                        def tile_gen(ti, lt0, n):
                            slot = ti % 2
                            DT, bDT = DT2[slot]
                            dtmp, bdtmp = dtmp2[slot]
                            PT, bPT = PT2[slot]
                            dd, bdd = DT2[slot]
                            j = lt0 // L
                            pn, pd, pst = slot, 2 + slot, 4 + slot
                            tb0, tb1 = lt0 // 128, (lt0 + n) // 128
                            sb_lo, sb_hi = j * nbs, (j + 1) * nbs
                            if d == 0:
                                sblocks = [s for s in range(sb_lo, sb_hi) if s < tb1]
                            else:
                                sblocks = [s for s in range(sb_hi - 1, sb_lo - 1, -1) if s >= tb0]
                            first = True
                            if g.gi == 1:
                                self.mm(self.ps[pn][:, 0:n], self.bps[pn], [(c0b[:], qsc[:, lt0:lt0 + n])], [bc0b, bqsc], start=True, stop=False)
                                self.mm(self.ps[pd][:, 0:n], self.bps[pd], [(n0r[:], qsc[:, lt0:lt0 + n])], [bn0r, bqsc], start=True, stop=False)
                                first = False
                                yield
                            for si, s in enumerate(sblocks):
                                last = (si == len(sblocks) - 1)
                                if d == 0:
                                    a, b_ = max(tb0, s), tb1
                                else:
                                    a, b_ = tb0, min(tb1, s + 1)
                                c_a, c_n = a * 128, (b_ - a) * 128
                                self.mm(self.ps[pst][:, 0:c_n], self.bps[pst], [(kT[:, s * 128:(s + 1) * 128], qT[:, c_a:c_a + c_n])], [bkT, bqT])
                                has_diag = (a <= s < b_)
                                if has_diag:
                                    dcol = s * 128
                                    S.op("dve", lambda e: e.tensor_tensor(out=dtmp[:], in0=colbc[:, dcol:dcol + 128], in1=nm_[:], op=ALU.add), r=[bcolbc, bnm_], w=[bdtmp])
                                    yield
                                    S.op("act", lambda e: e.activation(out=DT[:, dcol - c_a:dcol - c_a + 128], in_=dtmp[:], func=AF.Exp, bias=rowT[:, s, h:h + 1], scale=1.0),
                                         r=[bdtmp, browT], w=[bDT])
                                    if d == 0:
                                        r_a, r_n = dcol + 128, c_a + c_n - (dcol + 128)
                                    else:
                                        r_a, r_n = c_a, dcol - c_a
                                else:
                                    r_a, r_n = c_a, c_n
                                if r_n > 0:
                                    S.op("act", lambda e: e.activation(out=DT[:, r_a - c_a:r_a - c_a + r_n], in_=colbc[:, r_a:r_a + r_n], func=AF.Exp, bias=rowT[:, s, h:h + 1], scale=1.0),
                                         r=[bcolbc, browT], w=[bDT])
                                yield
                                S.op("dve", lambda e: e.tensor_tensor(out=PT[:, 0:c_n], in0=self.ps[pst][:, 0:c_n], in1=DT[:, 0:c_n], op=ALU.mult),
                                     r=[self.bps[pst], bDT], w=[bPT])
                                yield
                                o0 = c_a - lt0
                                self.mm(self.ps[pn][:, o0:o0 + c_n], self.bps[pn], [(vtok[:, s, 0:128], PT[:, 0:c_n])], [bvtok, bPT], start=first, stop=last)
                                self.mm(self.ps[pd][:, o0:o0 + c_n], self.bps[pd], [(self.onesb[:], PT[:, 0:c_n])], [self.b_onesb, bPT], start=first, stop=last)
                                first = False
                                yield
                            S.op("act", lambda e: e.activation(out=dd[:, 0:n], in_=self.ps[pd][:, 0:n], func=AF.Abs), r=[self.bps[pd]], w=[bdd])
                            yield
                            S.op("dve", lambda e: e.tensor_tensor(out=dd[:, 0:n], in0=dd[:, 0:n], in1=enmbc[:, lt0:lt0 + n], op=ALU.max),
                                 r=[bdd, benmbc], w=[bdd])
                            yield
                            S.op("dve", lambda e: e.reciprocal(out=dd[:, 0:n], in_=dd[:, 0:n]), r=[bdd], w=[bdd])
                            yield
                            if d == 0:
                                S.op("dve", lambda e: e.tensor_tensor(out=hT0[:, lt0:lt0 + n], in0=self.ps[pn][:, 0:n], in1=dd[:, 0:n], op=ALU.mult),
                                     r=[self.bps[pn], bdd], w=[bhT0])
                            else:
                                S.op("dve", lambda e: e.tensor_tensor(out=dd[:, 0:n], in0=self.ps[pn][:, 0:n], in1=dd[:, 0:n], op=ALU.mult),
                                     r=[self.bps[pn], bdd], w=[bdd])
                                yield
                                S.op("pool", lambda e: e.tensor_tensor(out=hT0[:, lt0:lt0 + n], in0=hT0[:, lt0:lt0 + n], in1=dd[:, 0:n], op=ALU.add),
                                     r=[bdd, bhT0], w=[bhT0])

                        for p0 in range(0, len(tts), 2):
                            gens = [tile_gen(ti, tts[ti][0], tts[ti][1]) for ti in range(p0, min(p0 + 2, len(tts)))]
                            while gens:
                                for gn in list(gens):
                                    try:
                                        next(gn)
                                    except StopIteration:
                                        gens.remove(gn)
                        if g.gi == 0:
                            for j in range(g.nseq):
                                tl = (j + 1) * L - 1 if d == 0 else j * L
                                for bi in range(nbs):
                                    s = j * nbs + bi
                                    S.op("act", lambda e, s=s, tl=tl: e.activation(out=wv_[:], in_=rowT[:, s, h:h + 1], func=AF.Exp, bias=colbc[:, tl:tl + 1], scale=1.0),
                                         r=[browT, bcolbc], w=[bwv_])
                                    S.op("dve", lambda e, s=s: e.tensor_scalar(out=kw[:], in0=ktok[:, s, :], scalar1=wv_[:, 0:1], scalar2=None, op0=ALU.mult), r=[bktok, bwv_], w=[bkw])
                                    self.mm(self.ps[7][:, 0:129], self.bps[7], [(kw[:], vtok[:, s, 0:129])], [bkw, bvtok], start=(bi == 0), stop=(bi == nbs - 1))
                                S.op("act", lambda e: e.copy(out=cn[:], in_=self.ps[7][:, 0:129]), r=[self.bps[7]], w=[bcn])
                                self.out_dma(self.dram["o_c"][j, d, h], cn[:, 0:128], [bcn])
                                self.out_dma(self.dram["o_n"][j, d, h].rearrange("(p o) -> p o", o=1), cn[:, 128:129], [bcn])
                    og, bog = enmbc, benmbc
                    self.proj_fm(g, [(lambda kc: wo[0][:, kc, :], 0)], 128,
                                 lambda ps, bp, lt0, n: S.op("act", lambda e: e.activation(out=og[:, lt0:lt0 + n], in_=ps, func=AF.Sigmoid), r=[bp], w=[bog]), wbufs=[wo[1]])
                    self.head_rms_gate(hs, hT0, bhT0, og, bog, ev["zT"][:, 4 + h, g.tok0:g.tok0 + T], ev["bz"][g.gi][4 + h], T,
                                       scratch=((kT, bkT), (colbc, bcolbc)))
                    S.end_phase(keep=gkeep)
            S.end_phase(keep=keep)


    def ph_hyena(self):
        S = self.S
        with ExitStack() as ph:
            zT, _ = self.sb(ph, "hzT", [128, 8, NT], BF16)
            bz = [[S.buf("hz%d_%d" % (gi, c)) for c in range(8)] for gi in range(2)]
            ones32, b_ones32 = self.sb(ph, "ones32", [128, 128], F32)
            S.op("pool", lambda e: e.memset(ones32[:], 1.0), w=[b_ones32])
            self.hy = dict(zT=zT, bz=bz, ones32=ones32, b_ones32=b_ones32)
            keep = list(S.phase_bufs)
            for g in GROUPS:
                self.hyena_group(g, keep)
            self.out_proj(ph, zT, [b for gb in bz for b in gb], "hy_w_out", 1, 2, keep)
            S.end_phase(keep=())

    def sin_rr(self, st, x, bx, n, P_):
        S = self.S
        PI = float(np.pi)
        nx, bnx = self.sb(st, "rr_nx", [P_, n], F32)
        y, by = self.sb(st, "rr_y", [P_, n], F32)
        tmp, btmp = self.sb(st, "rr_t", [P_, n], F32)
        S.op("dve", lambda e: e.tensor_scalar(out=nx[:], in0=x[:], scalar1=-1.0, scalar2=None, op0=ALU.mult), r=[bx], w=[bnx])
        S.op("dve", lambda e: e.tensor_copy(out=y[:], in_=x[:]), r=[bx], w=[by])
        for (src, bsrc, thr, delta) in ((x, bx, PI, -2 * PI), (x, bx, 3 * PI, -2 * PI), (nx, bnx, PI, 2 * PI), (nx, bnx, 3 * PI, 2 * PI)):
            S.op("dve", lambda e, src=src, thr=thr, delta=delta: e.tensor_scalar(out=tmp[:], in0=src[:], scalar1=thr, scalar2=delta, op0=ALU.is_ge, op1=ALU.mult), r=[bsrc], w=[btmp])
            S.op("dve", lambda e: e.tensor_tensor(out=y[:], in0=y[:], in1=tmp[:], op=ALU.add), r=[by, btmp], w=[by])
        S.op("act", lambda e: e.activation(out=x[:], in_=y[:], func=AF.Sin), r=[by], w=[bx])

    def hyena_group(self, g, keep):
        S = self.S
        hy = self.hy
        gi = g.gi
        L = g.L
        T = g.T
        nb = T // 128
        nbl = L // 128
        CW = 512 if gi == 0 else 256
        dftc, dfts = self.dram["dftc%d" % gi], self.dram["dfts%d" % gi]
        idftc, idfts = self.dram["idftc%d" % gi], self.dram["idfts%d" % gi]
        w_in = self.dram["hy_w_in"]
        with ExitStack() as gs:
            a2, ba2 = self.sb(gs, "a2T", [64, L], F32)
            tn, btn = self.sb(gs, "tn", [128, nbl], F32)
            S.dma("sp", tn[:], self.dram["tn%d" % gi], w=[btn])
            pmask, bpmask = self.sb(gs, "pmask", [128, nbl], F32)
            S.op("pool", lambda e: e.memset(pmask[:], 1.0), w=[bpmask])
            S.op("pool", lambda e: e.memset(pmask[0:1, 0:1], 0.0), w=[bpmask])
            gk0 = list(S.phase_bufs)
            with ExitStack() as fs:
                zf, bzf = self.sb(fs, "zf", [33, L], F32)
                S.dma("sp", zf[:], self.dram["zfeat%d" % gi], w=[bzf])
                w1, bw1 = self.sb(fs, "hw1", [33, 64], F32)
                S.dma("sp", w1[:], self.dram["hy_w1"], w=[bw1])
                w2, bw2 = self.sb(fs, "hw2", [64, 64], F32)
                S.dma("sp", w2[:], self.dram["hy_w2"], w=[bw2])
                fr, bfr = self.sb(fs, "hfr", [64, 2], F32)
                S.dma("sp", fr[:], self.dram["hy_freq"].rearrange("r h -> h r"), w=[bfr], allow_slow_non_contiguous=True)
                bb_, bbb_ = self.sb(fs, "hbb", [64, 2], F32)
                S.dma("sp", bb_[:, 0:1], self.dram["hy_b1"].rearrange("(h o) -> h o", o=1), w=[bbb_])
                S.dma("sp", bb_[:, 1:2], self.dram["hy_b2"].rearrange("(h o) -> h o", o=1), w=[bbb_])
                S.op("dve", lambda e: e.tensor_tensor(out=bb_[:], in0=bb_[:], in1=fr[:], op=ALU.mult), r=[bbb_, bfr], w=[bbb_])
                a1, ba1 = self.sb(fs, "a1T", [64, L], F32)
                for (dst, bdst, wt, bwt, src, bsrc, li) in ((a1, ba1, w1, bw1, zf, bzf, 0), (a2, ba2, w2, bw2, a1, ba1, 1)):
                    for i in range(0, L, 512):
                        n = min(512, L - i)
                        pb = 6 + ((i // 512) % 2)
                        self.mm(self.ps[pb][0:64, 0:n], self.bps[pb], [(wt[:], src[:, i:i + n])], [bwt, bsrc])
                        S.op("act", lambda e, dst=dst, i=i, n=n, pb=pb, li=li: e.activation(out=dst[:, i:i + n], in_=self.ps[pb][0:64, 0:n], func=AF.Identity, bias=bb_[:, li:li + 1], scale=fr[:, li:li + 1]),
                             r=[self.bps[pb], bbb_, bfr], w=[bdst])
                    self.sin_rr(fs, dst, bdst, L, 64)
                S.end_phase(keep=gk0)
            gkeep = list(S.phase_bufs)
            for p in range(1024 // CW):
                cq = p * CW
                with ExitStack() as pss:
                    Cs = {}
                    for o in range(2):
                        Cs[o] = (self.sb(pss, "C%d" % o, [128, nbl, CW], BF16), self.sb(pss, "D%d" % o, [128, nbl, CW], BF16))
                    pk = list(S.phase_bufs)
                    with ExitStack() as fs:
                        hs, bhs = self.sb(fs, "hs", [128, nbl, CW], BF16)
                        hd, bhd = self.sb(fs, "hd", [128, nbl, CW], BF16)
                        w3 = [self.sb(fs, "w3_%d" % dr, [64, CW], F32) for dr in range(2)]
                        b3 = [self.sb(fs, "b3_%d" % dr, [128, CW], F32) for dr in range(2)]
                        rt = [self.sb(fs, "rt_%d" % dr, [128, CW], F32) for dr in range(2)]
                        fr_ = [self.sb(fs, "fraw%d" % dr, [128, CW], F32) for dr in range(2)]
                        dec, bdec = self.sb(fs, "dec", [128, CW], F32)
                        sq, bsq = self.sb(fs, "fsq", [128, CW], F32)
                        rn, brn = self.sb(fs, "rn", [128, CW], F32)
                        bia, bbia = self.sb(fs, "bia", [128, CW], F32)
                        fcs = [self.sb(fs, "fc%d" % i, [128, nbl, 128], BF16) for i in range(2)]
                        fss = [self.sb(fs, "fs%d" % i, [128, nbl, 128], BF16) for i in range(2)]
                        for o in range(2):
                            (C, bC), (Dd, bD) = Cs[o]
                            for dr in range(2):
                                c0 = dr * 2048 + o * 1024 + cq
                                S.dma("sp", w3[dr][0][:], self.dram["hy_w3"][:, c0:c0 + CW], w=[w3[dr][1]])
                                S.dma("sp", b3[dr][0][:], self.dram["hy_b3"][0:1, c0:c0 + CW].partition_broadcast(128), w=[b3[dr][1]])
                                S.dma("sp", rt[dr][0][:], self.dram["hy_log_rate"][0:1, c0:c0 + CW].partition_broadcast(128), w=[rt[dr][1]])
                                S.op("act", lambda e, dr=dr: e.activation(out=rt[dr][0][:], in_=rt[dr][0][:], func=AF.Exp), r=[rt[dr][1]], w=[rt[dr][1]])
                            S.dma("sp", bia[:], self.dram["hy_bias"][o:o + 1, cq:cq + CW].partition_broadcast(128), w=[bbia])
                            for pb_ in range(nbl):
                                for dr in range(2):
                                    pbk = 6 + dr
                                    self.mm(self.ps[pbk][:, 0:CW], self.bps[pbk], [(a2[:, pb_ * 128:(pb_ + 1) * 128], w3[dr][0][:])], [ba2, w3[dr][1]])
                                    f, bf = fr_[dr]
                                    S.op("dve", lambda e, f=f, pbk=pbk, dr=dr: e.tensor_tensor(out=f[:], in0=self.ps[pbk][:, 0:CW], in1=b3[dr][0][:], op=ALU.add), r=[self.bps[pbk], b3[dr][1]], w=[bf])
                                    S.op("act", lambda e, dr=dr, pb_=pb_: e.activation(out=dec[:], in_=rt[dr][0][:], func=AF.Exp, scale=tn[:, pb_:pb_ + 1]), r=[rt[dr][1], btn], w=[bdec])
                                    S.op("dve", lambda e, f=f: e.tensor_tensor(out=f[:], in0=f[:], in1=dec[:], op=ALU.mult), r=[bf, bdec], w=[bf])
                                    S.op("act", lambda e, f=f: e.activation(out=sq[:], in_=f[:], func=AF.Square), r=[bf], w=[bsq])
                                    self.mm(self.ps[5][:, 0:CW], self.bps[5], [(hy["ones32"][:], sq[:])], [hy["b_ones32"], bsq],
                                            start=(pb_ == 0 and dr == 0), stop=(pb_ == nbl - 1 and dr == 1))
                                S.op("dve", lambda e, pb_=pb_: e.tensor_scalar(out=fr_[1][0][:], in0=fr_[1][0][:], scalar1=pmask[:, pb_:pb_ + 1], scalar2=None, op0=ALU.mult), r=[fr_[1][1], bpmask], w=[fr_[1][1]])
                                S.op("dve", lambda e, pb_=pb_: e.tensor_tensor(out=hs[:, pb_, :], in0=fr_[0][0][:], in1=fr_[1][0][:], op=ALU.add), r=[fr_[0][1], fr_[1][1]], w=[bhs])
                                S.op("dve", lambda e, pb_=pb_: e.tensor_tensor(out=hd[:, pb_, :], in0=fr_[0][0][:], in1=fr_[1][0][:], op=ALU.subtract), r=[fr_[0][1], fr_[1][1]], w=[bhd])
                            S.op("act", lambda e: e.activation(out=rn[:], in_=self.ps[5][:, 0:CW], func=AF.Sqrt), r=[self.bps[5]], w=[brn])
                            S.op("dve", lambda e: e.reciprocal(out=rn[:], in_=rn[:]), r=[brn], w=[brn])
                            for kc in range(nbl):
                                fc, bfc = fcs[kc % 2]
                                fs_, bfs = fss[kc % 2]
                                S.dma("sp", fc[:], dftc[kc], w=[bfc])
                                S.dma("sp", fs_[:], dfts[kc], w=[bfs])
                                self.mm(self.ps[6][:, 0:CW], self.bps[6], [(fc[:, sb_, :], hs[:, sb_, :]) for sb_ in range(nbl)], [bfc, bhs])
                                self.mm(self.ps[7][:, 0:CW], self.bps[7], [(fs_[:, sb_, :], hd[:, sb_, :]) for sb_ in range(nbl)], [bfs, bhd])
                                S.op("dve", lambda e: e.tensor_tensor(out=sq[:], in0=self.ps[6][:, 0:CW], in1=rn[:], op=ALU.mult), r=[self.bps[6], brn], w=[bsq])
                                S.op("pool", lambda e, kc=kc, C=C: e.tensor_tensor(out=C[:, kc, :], in0=sq[:], in1=bia[:], op=ALU.add), r=[bsq, bbia], w=[bC])
                                S.op("dve", lambda e, kc=kc, Dd=Dd: e.tensor_tensor(out=Dd[:, kc, :], in0=self.ps[7][:, 0:CW], in1=rn[:], op=ALU.mult), r=[self.bps[7], brn], w=[bD])
                        S.end_phase(keep=pk)
                    vt, bvt = self.sb(pss, "vt", [128, nb, CW], BF16)
                    x1, bx1 = self.sb(pss, "x1", [128, nb, CW], BF16)
                    x2, bx2 = self.sb(pss, "x2", [128, nb, CW], BF16)
                    PQ, bPQ = self.sb(pss, "PQ", [128, 2 * nbl, CW], BF16)
                    pk2 = list(S.phase_bufs)
                    with ExitStack() as ws:
                        w32s = [self.sb(ws, "hw32_%d" % i, [128, 4, CW], F32) for i in range(2 if gi == 1 else 1)]
                        wcnt_ = 0
                        cwb, bcwb = self.sb(ws, "hcw", [128, 3, CW], F32)
                        tapt = [self.sb(ws, "hwt%d" % jj, [128, 8, CW], BF16) for jj in range(3)]
                        tapl = [(lambda kc, jj=jj: tapt[jj][0][:, kc, :], jj - 1) for jj in range(3)]
                        tapb = [tapt[jj][1] for jj in range(3)]
                        for wi, (dst, bdst) in enumerate(((vt, bvt), (x1, bx1), (x2, bx2))):
                            c0 = wi * 1024 + cq
                            for jj in range(3):
                                S.dma("sp", cwb[:, jj, :], self.dram["hy_conv"][jj:jj + 1, c0:c0 + CW].partition_broadcast(128), w=[bcwb])
                            for hk in range(2):
                                w32, bw32 = w32s[wcnt_ % len(w32s)]
                                wcnt_ += 1
                                S.dma("sp", w32[:], w_in.rearrange("(k p) n -> p k n", p=128)[:, hk * 4:hk * 4 + 4, c0:c0 + CW], w=[bw32])
                                for jj in range(3):
                                    wt, bwt = tapt[jj]
                                    eng = "dve" if jj != 1 else "pool"
                                    S.op(eng, lambda e, wt=wt, jj=jj, hk=hk: e.tensor_tensor(out=wt[:, hk * 4:hk * 4 + 4, :], in0=w32[:], in1=cwb[:, jj, :].unsqueeze(1).to_broadcast([128, 4, CW]), op=ALU.mult),
                                         r=[bw32, bcwb], w=[bwt])
                            self.proj_tm(g, tapl, CW, lambda ps, bp, lb_, dst=dst, bdst=bdst: S.op("act", lambda e: e.copy(out=dst[:, lb_, :], in_=ps), r=[bp], w=[bdst]), wbufs=tapb)
                        S.end_phase(keep=pk2)
                    with ExitStack() as cs:
                        fcs = [self.sb(cs, "sfc%d" % i, [128, nbl, 128], BF16) for i in range(2)]
                        fss = [self.sb(cs, "sfs%d" % i, [128, nbl, 128], BF16) for i in range(2)]
                        ics = [self.sb(cs, "ic%d" % i, [128, nbl, 128], BF16) for i in range(2)]
                        iss = [self.sb(cs, "is%d" % i, [128, nbl, 128], BF16) for i in range(2)]
                        t1s = [self.sb(cs, "ct1_%d" % i, [128, CW], F32) for i in range(2)]
                        t2s = [self.sb(cs, "ct2_%d" % i, [128, CW], F32) for i in range(2)]
                        z2, bz2 = self.sb(cs, "z2", [128, CW], BF16)
                        cnt = 0
                        for o in range(2):
                            (C, bC), (Dd, bD) = Cs[o]
                            gate, bgate = (x1, bx1) if o == 0 else (x2, bx2)
                            for j in range(g.nseq):
                                for kc in range(nbl):
                                    fc, bfc = fcs[cnt % 2]
                                    fs_, bfs = fss[cnt % 2]
                                    t1, bt1 = t1s[cnt % 2]
                                    t2, bt2 = t2s[cnt % 2]
                                    cnt += 1
                                    S.dma("sp", fc[:], dftc[kc], w=[bfc])
                                    S.dma("sp", fs_[:], dfts[kc], w=[bfs])
                                    pa, pb2 = (0, 1) if cnt % 2 else (2, 3)
                                    self.mm(self.ps[pa][:, 0:CW], self.bps[pa], [(fc[:, sb_, :], vt[:, j * nbl + sb_, :]) for sb_ in range(nbl)], [bfc, bvt])
                                    self.mm(self.ps[pb2][:, 0:CW], self.bps[pb2], [(fs_[:, sb_, :], vt[:, j * nbl + sb_, :]) for sb_ in range(nbl)], [bfs, bvt])
                                    S.op("dve", lambda e, t1=t1, pa=pa, kc=kc, C=C: e.tensor_tensor(out=t1[:], in0=self.ps[pa][:, 0:CW], in1=C[:, kc, :], op=ALU.mult), r=[self.bps[pa], bC], w=[bt1])
                                    S.op("dve", lambda e, t2=t2, pb2=pb2, kc=kc, Dd=Dd: e.tensor_tensor(out=t2[:], in0=self.ps[pb2][:, 0:CW], in1=Dd[:, kc, :], op=ALU.mult), r=[self.bps[pb2], bD], w=[bt2])
                                    S.op("pool", lambda e, t1=t1, t2=t2, kc=kc: e.tensor_tensor(out=PQ[:, kc, :], in0=t1[:], in1=t2[:], op=ALU.subtract), r=[bt1, bt2], w=[bPQ])
                                    S.op("dve", lambda e, t1=t1, pa=pa, kc=kc, Dd=Dd: e.tensor_tensor(out=t1[:], in0=self.ps[pa][:, 0:CW], in1=Dd[:, kc, :], op=ALU.mult), r=[self.bps[pa], bD], w=[bt1])
                                    S.op("dve", lambda e, t2=t2, pb2=pb2, kc=kc, C=C: e.tensor_tensor(out=t2[:], in0=self.ps[pb2][:, 0:CW], in1=C[:, kc, :], op=ALU.mult), r=[self.bps[pb2], bC], w=[bt2])
                                    S.op("pool", lambda e, t1=t1, t2=t2, kc=kc: e.tensor_tensor(out=PQ[:, nbl + kc, :], in0=t1[:], in1=t2[:], op=ALU.add), r=[bt1, bt2], w=[bPQ])
                                for tb in range(nbl):
                                    ic, bic = ics[tb % 2]
                                    is_, bis = iss[tb % 2]
                                    S.dma("sp", ic[:], idftc[tb], w=[bic])
                                    S.dma("sp", is_[:], idfts[tb], w=[bis])
                                    pb3 = 4 + (tb % 2)
                                    pairs = [(ic[:, kc, :], PQ[:, kc, :]) for kc in range(nbl)] + [(is_[:, kc, :], PQ[:, nbl + kc, :]) for kc in range(nbl)]
                                    self.mm(self.ps[pb3][:, 0:CW], self.bps[pb3], pairs, [bic, bis, bPQ])
                                    blk = j * nbl + tb
                                    if o == 0:
                                        S.op("dve", lambda e, blk=blk, pb3=pb3: e.tensor_tensor(out=vt[:, blk, :], in0=self.ps[pb3][:, 0:CW], in1=gate[:, blk, :], op=ALU.mult),
                                             r=[self.bps[pb3], bgate], w=[bvt])
                                    else:
                                        S.op("dve", lambda e, blk=blk, pb3=pb3: e.tensor_tensor(out=z2[:], in0=self.ps[pb3][:, 0:CW], in1=gate[:, blk, :], op=ALU.mult),
                                             r=[self.bps[pb3], bgate], w=[bz2])
                                        psb = self.ps[6 + (tb % 2)][:].bitcast(BF16)
                                        pbt = 6 + (tb % 2)
                                        for q in range(CW // 128):
                                            S.op("pe", lambda e, q=q, psb=psb: e.transpose(psb[:, q * 128:(q + 1) * 128], z2[:, q * 128:(q + 1) * 128], self.identb[:]),
                                                 r=[bz2, self.b_identb], w=[self.bps[pbt]], inc=(q == CW // 128 - 1))
                                        tok = g.tok0 + blk * 128
                                        c8 = cq // 128
                                        S.op("act", lambda e, psb=psb, tok=tok, c8=c8: e.copy(out=hy["zT"][:, c8:c8 + CW // 128, tok:tok + 128], in_=psb[:, 0:CW].rearrange("p (q c) -> p q c", c=128)),
                                             r=[self.bps[pbt]], w=[hy["bz"][gi][c8]])
                        S.end_phase(keep=pk2)
                    S.end_phase(keep=gkeep)
            S.end_phase(keep=keep)


def _pos_table():
    n_tok, d, gw = 1024, 1024, 64
    rows = n_tok // gw
    r, col = np.meshgrid(np.arange(rows, dtype=np.float32), np.arange(gw, dtype=np.float32), indexing="ij")
    r = r.reshape(-1)
    col = col.reshape(-1)
    quarter = d // 4
    inv = (1.0 / (np.float32(10000.0) ** (np.arange(quarter, dtype=np.float32) / np.float32(quarter)))).astype(np.float32)
    ar = r[:, None] * inv[None]
    ac = col[:, None] * inv[None]
    return np.concatenate([np.sin(ar), np.cos(ar), np.sin(ac), np.cos(ac)], axis=-1).astype(np.float32)


_NC_CACHE = {}


def get_nc(plan):
    key = tuple(plan)
    if key not in _NC_CACHE:
        b = Builder(list(plan))
        nc = b.build()
        _NC_CACHE[key] = (b, nc)
    return _NC_CACHE[key]


FULL_PLAN = ["load", "norm:0:0", "even", "norm:0:1", "moe:0", "norm:1:0", "hyena", "norm:1:1", "moe:1", "final"]


def make_inputs(inputs, plan=None):
    f = lambda a: np.ascontiguousarray(np.asarray(a, dtype=np.float32))
    x_prompt = f(inputs["x_prompt"])
    x_sample = f(inputs["x_sample"])
    w_gu = f(inputs["w_gu"])
    gu_l = np.ascontiguousarray(
        w_gu.reshape(2, NE, 8, 128, 8, 128, 2).transpose(0, 1, 4, 3, 6, 2, 5)).reshape(2, NE, 8, 128, 2 * 8 * 128)
    wd_l = np.ascontiguousarray(
        f(inputs["w_down"]).reshape(2, NE, 8, 128, 2, 512).transpose(0, 1, 4, 3, 2, 5)).reshape(2, NE, 2, 128, 8 * 512)
    bgu_l = np.ascontiguousarray(
        f(inputs["b_gu"]).reshape(2, NE, 8, 128, 2).transpose(0, 3, 1, 4, 2)).reshape(2, 128, NE * 16)
    pos = _pos_table().reshape(8, 128, 1024)
    common = {
        "pos": pos,
        "w_mod": f(inputs["w_mod"]), "b_mod": f(inputs["b_mod"]), "norm_g": f(inputs["norm_g"]),
        "final_g": f(inputs["final_g"]).reshape(1, 1024),
        "w_router": f(inputs["w_router"]), "b_router": f(inputs["b_router"]),
        "gu_l": gu_l, "wd_l": wd_l, "bgu_l": bgu_l, "b_down": f(inputs["b_down"]),
        "ev_w_in": f(inputs["ev_w_in"])[0], "ev_gate_b": f(inputs["ev_gate_b"])[0], "ev_conv": f(inputs["ev_conv"])[0],
        "hg_lb": f(inputs["hg_lb"]), "ev_w_out": f(inputs["ev_w_out"])[0],
        "hy_w_in": f(inputs["hy_w_in"])[0], "hy_conv": f(inputs["hy_conv"])[0], "hy_w1": f(inputs["hy_w1"])[0],
        "hy_b1": f(inputs["hy_b1"])[0], "hy_w2": f(inputs["hy_w2"])[0], "hy_b2": f(inputs["hy_b2"])[0],
        "hy_w3": f(inputs["hy_w3"])[0], "hy_b3": f(inputs["hy_b3"]), "hy_freq": f(inputs["hy_freq"])[0],
        "hy_log_rate": f(inputs["hy_log_rate"]), "hy_bias": f(inputs["hy_bias"])[0], "hy_w_out": f(inputs["hy_w_out"])[0],
    }
    common.update(_hyena_consts())
    maps = []
    for c in range(NCORES):
        m = dict(common)
        xin = np.concatenate([x_prompt[4 * c:4 * c + 4].reshape(1024, 1024), x_sample[c]], axis=0)
        m["x_in"] = np.ascontiguousarray(xin.reshape(16, 128, 1024))
        cond2 = np.stack([f(inputs["c_ctx"]), f(inputs["c"])[c]], axis=0)
        m["cond_l"] = np.ascontiguousarray(cond2.reshape(2, 8, 128).transpose(2, 1, 0))
        m["st_hg"] = f(inputs["state_hgrn"])[c, 0]
        m["st_c"] = f(inputs["state_mlstm_c"])[c, 0]
        m["st_n"] = f(inputs["state_mlstm_n"])[c, 0]
        m["st_m"] = f(inputs["state_mlstm_m"])[c, 0]
        maps.append(m)
    return maps


def _hyena_consts():
    import ml_dtypes
    out = {}
    for gi, L in enumerate((256, 1024)):
        t = np.arange(L, dtype=np.float32)
        t_norm = (t / np.float32(L - 1)).astype(np.float32)
        bands = np.linspace(1e-4, 15, 16, dtype=np.float32)
        ang = (np.float32(2.0 * np.pi / L) * t[:, None] * bands[None, :]).astype(np.float32)
        z = np.concatenate([t_norm[:, None], np.cos(ang), np.sin(ang)], axis=-1).astype(np.float32)
        out["zfeat%d" % gi] = np.ascontiguousarray(z.T)
        out["tn%d" % gi] = np.ascontiguousarray((-t_norm).reshape(L // 128, 128).T)
        n = 2 * L
        k = np.arange(L, dtype=np.float64)
        w = 2.0 * np.pi * (k + 0.5) / n
        s = np.arange(L, dtype=np.float64)
        ang2 = s[:, None] * w[None, :]
        nbk = L // 128

        def chunked(m):
            return np.ascontiguousarray(m.reshape(nbk, 128, nbk, 128).transpose(2, 1, 0, 3)).astype(ml_dtypes.bfloat16)
        out["dftc%d" % gi] = chunked(np.cos(ang2))
        out["dfts%d" % gi] = chunked(np.sin(ang2))
        out["idftc%d" % gi] = chunked((2.0 / n) * np.cos(ang2.T))
        out["idfts%d" % gi] = chunked((2.0 / n) * np.sin(ang2.T))
    return out


def run_plan(inputs, plan):
    b, nc = get_nc(plan)
    maps = make_inputs(inputs)
    used = set(b.dram.keys())
    maps = [{k: v for k, v in m.items() if k in used} for m in maps]
    res = run_bass_kernel_spmd(nc, maps, core_ids=list(range(NCORES)))
    return res.results


def kernel(**inputs):
    rs = run_plan(inputs, FULL_PLAN)
    y = np.stack([r["y_out"].reshape(2048, 1024) for r in rs], axis=0)
    y_prompt = np.ascontiguousarray(y[:, :1024].reshape(32, 256, 1024)).astype(np.float32)
    y_sample = np.ascontiguousarray(y[:, 1024:]).astype(np.float32)
    o_hg = np.concatenate([r["o_hg"] for r in rs], axis=0).reshape(32, 1, 2, 4, 128, 128).astype(np.float32)
    o_c = np.concatenate([r["o_c"] for r in rs], axis=0).reshape(32, 1, 2, 4, 128, 128).astype(np.float32)
    o_n = np.concatenate([r["o_n"] for r in rs], axis=0).reshape(32, 1, 2, 4, 128).astype(np.float32)
    o_m = np.concatenate([r["o_m"] for r in rs], axis=0).reshape(32, 1, 2, 4).astype(np.float32)
    return (y_prompt, y_sample, o_hg, o_c, o_n, o_m)
```
